# Optimizing a Trainium2 kernel written in Bass

```python
import math
import jax, jax.numpy as jnp
from jax import lax
import numpy as np

D_MODEL = 4096
BATCH = 2
SEQ = 4096
DEPTH = 2

ROPE_THETA = 500000.0
Q_BLOCK = 128

GLA_HEADS = 4
GLA_DK = 192
GLA_DV = 384
GLA_GATE_RANK = 16
GLA_TAU = 16.0
GLA_CHUNK = 64

DIFF_HEADS = 10
DIFF_D = 64

DSA_HEADS = 10
DSA_KV_HEADS = 2
DSA_HEAD_DIM = 128
IDX_HEADS = 32
IDX_DIM = 64
IDX_TOPK_MAX = 256

MIX_GLA = GLA_HEADS * GLA_DV
MIX_DIFF = DIFF_HEADS * 2 * DIFF_D
MIX_DSA = DSA_HEADS * DSA_HEAD_DIM
MIX_WIDTH = MIX_GLA + MIX_DIFF + MIX_DSA

IN_SPLITS = (
    GLA_HEADS * GLA_DK, GLA_HEADS * GLA_DK, GLA_HEADS * GLA_DV, GLA_HEADS * GLA_DV, GLA_GATE_RANK,
    DIFF_HEADS * 2 * DIFF_D, DIFF_HEADS * 2 * DIFF_D, DIFF_HEADS * 2 * DIFF_D,
    DSA_HEADS * DSA_HEAD_DIM, DSA_KV_HEADS * DSA_HEAD_DIM, DSA_KV_HEADS * DSA_HEAD_DIM,
    IDX_HEADS * IDX_DIM, IDX_DIM, IDX_HEADS,
)
N_IN = sum(IN_SPLITS)
SPLIT_POINTS = tuple(int(v) for v in np.cumsum(IN_SPLITS)[:-1])

D_FF = 11008
N_EXPERTS = 8
MOE_TOP_K = 2
D_FF_EXPERT = 4096
MOE_BLOCK = 128
N_DENSE = (DEPTH + 1) // 2
N_MOE = DEPTH // 2

DEEPNORM_ALPHA = (2 * DEPTH) ** 0.25
DEEPNORM_BETA = (8 * DEPTH) ** -0.25

kernel_name = "hybrid_gla_diff_dsa_deepnorm_moe"


def layer_norm(x, g, b, eps=1e-5):
    xf = x.astype(jnp.float32)
    mu = jnp.mean(xf, -1, keepdims=True)
    var = jnp.mean(jnp.square(xf - mu), -1, keepdims=True)
    return ((xf - mu) * lax.rsqrt(var + eps) * g.astype(jnp.float32) + b.astype(jnp.float32)).astype(x.dtype)


def rms_norm(x, g, eps=1e-5):
    xf = x.astype(jnp.float32)
    return (xf * lax.rsqrt(jnp.mean(xf * xf, -1, keepdims=True) + eps) * g.astype(jnp.float32)).astype(x.dtype)


def partial_rope(x, positions):
    d = x.shape[-1]
    rot = d // 4
    half = rot // 2
    inv_freq = 1.0 / (ROPE_THETA ** (jnp.arange(half, dtype=jnp.float32) * 2.0 / rot))
    ang = positions.astype(jnp.float32)[:, :, None, None] * inv_freq
    cos, sin = jnp.cos(ang), jnp.sin(ang)
    xf = x.astype(jnp.float32)
    x1, x2, rest = xf[..., :half], xf[..., half:rot], xf[..., rot:]
    out = jnp.concatenate([x1 * cos - x2 * sin, x2 * cos + x1 * sin, rest], axis=-1)
    return out.astype(x.dtype)


def gla_mixer(q, k, v, log_a):
    B, S, H, dk = q.shape
    dv = v.shape[-1]
    C = GLA_CHUNK
    nc = S // C

    def to_chunks(t):
        return t.astype(jnp.float32).reshape(B, nc, C, H, t.shape[-1]).transpose(1, 0, 3, 2, 4)

    qc = to_chunks(q) * (dk ** -0.5)
    kc, vc, ac = to_chunks(k), to_chunks(v), to_chunks(log_a)
    causal = jnp.tril(jnp.ones((C, C), dtype=bool))

    def step(state, inp):
        qi, ki, vi, ai = inp
        b = jnp.cumsum(ai, axis=-2)
        rel = jnp.where(causal[:, :, None], b[..., :, None, :] - b[..., None, :, :], -jnp.inf)
        scores = jnp.einsum('bhid,bhjd,bhijd->bhij', qi, ki, jnp.exp(rel))
        o = (jnp.einsum('bhij,bhje->bhie', scores, vi)
             + jnp.einsum('bhid,bhde->bhie', qi * jnp.exp(b), state))
        b_last = b[..., -1:, :]
        state = (jnp.exp(b_last[..., 0, :])[..., None] * state
                 + jnp.einsum('bhjd,bhje->bhde', ki * jnp.exp(b_last - b), vi))
        return state, o

    state0 = jnp.zeros((B, H, dk, dv), jnp.float32)
    _, o = lax.scan(step, state0, (qc, kc, vc, ac))
    return o.transpose(1, 0, 3, 2, 4).reshape(B, S, H, dv)


def diff_attention(q, k, v, lam, lambda_init, norm_g):
    B, S, H, _, d = q.shape
    nb = S // Q_BLOCK
    qb = q.reshape(B, nb, Q_BLOCK, H, 2, d).swapaxes(0, 1)
    key_pos = jnp.arange(S)
    scale = d ** -0.5

    def block(args):
        qi, bi = args
        qpos = bi * Q_BLOCK + jnp.arange(Q_BLOCK)
        s = jnp.einsum('bqhcd,bkhcd->bchqk', qi, k).astype(jnp.float32) * scale
        s = jnp.where(key_pos[None, :] <= qpos[:, None], s, -jnp.inf)
        p = jax.nn.softmax(s, axis=-1)
        a = p[:, 0] - lam * p[:, 1]
        return jnp.einsum('bhqk,bkhe->bqhe', a.astype(v.dtype), v)

    o = lax.map(block, (qb, jnp.arange(nb)))
    o = o.swapaxes(0, 1).reshape(B, S, H, 2 * d)
    return rms_norm(o, norm_g) * (1.0 - lambda_init)


def dsa_attention(q, k, v, q_idx, k_idx, w_idx):
    B, S, H, dh = q.shape
    G = k.shape[2]
    R = H // G
    k_sel = min(IDX_TOPK_MAX, S // 4)
    nb = S // Q_BLOCK
    key_pos = jnp.arange(S)
    idx_scale = (IDX_DIM ** -0.5) * (IDX_HEADS ** -0.5)

    def blocks(t):
        return t.reshape(B, nb, Q_BLOCK, *t.shape[2:]).swapaxes(0, 1)

    def block(args):
        qb, qib, wib, bi = args
        qpos = bi * Q_BLOCK + jnp.arange(Q_BLOCK)
        causal = key_pos[None, :] <= qpos[:, None]
        logits = jax.nn.relu(jnp.einsum('bqhd,bsd->bqhs', qib, k_idx).astype(jnp.float32))
        score = jnp.einsum('bqh,bqhs->bqs', wib.astype(jnp.float32), logits) * idx_scale
        score = jnp.where(causal[None], score, -jnp.inf)
        _, sel = lax.top_k(score, k_sel)
        kg = jax.vmap(lambda kk, ii: kk[ii])(k, sel)
        vg = jax.vmap(lambda vv, ii: vv[ii])(v, sel)
        valid = sel <= qpos[None, :, None]
        qg = qb.reshape(B, Q_BLOCK, G, R, dh)
        s = jnp.einsum('bqgrd,bqkgd->bqgrk', qg, kg).astype(jnp.float32) * (dh ** -0.5)
        s = jnp.where(valid[:, :, None, None, :], s, -jnp.inf)
        p = jax.nn.softmax(s, axis=-1).astype(v.dtype)
        o = jnp.einsum('bqgrk,bqkgd->bqgrd', p, vg)
        return o.reshape(B, Q_BLOCK, H * dh)

    o = lax.map(block, (blocks(q), blocks(q_idx), blocks(w_idx), jnp.arange(nb)))
    return o.swapaxes(0, 1).reshape(B, S, H * dh)


def swiglu(h, w1, w3, w2):
    return (jax.nn.silu(h @ w1) * (h @ w3)) @ w2


def moe_swiglu(h, router_w, router_b, w1, w3, w2):
    B, S, D = h.shape
    N = B * S
    E = w1.shape[0]
    hf = h.reshape(N, D)
    logits = (hf @ router_w).astype(jnp.float32) + router_b.astype(jnp.float32)
    top_logit, top_e = lax.top_k(logits, MOE_TOP_K)
    gates = jax.nn.softmax(top_logit, axis=-1)
    n_assign = N * MOE_TOP_K
    flat_e = top_e.reshape(-1)
    flat_tok = jnp.repeat(jnp.arange(N, dtype=jnp.int32), MOE_TOP_K)
    flat_g = gates.reshape(-1)
    order = jnp.argsort(flat_e)
    se = flat_e[order]
    counts = jnp.bincount(flat_e, length=E)
    padded = (counts + MOE_BLOCK - 1) // MOE_BLOCK * MOE_BLOCK
    start_sorted = jnp.cumsum(counts) - counts
    end_padded = jnp.cumsum(padded)
    start_padded = end_padded - padded
    dest = start_padded[se] + jnp.arange(n_assign) - start_sorted[se]
    n_blocks = -(-(n_assign + E * MOE_BLOCK) // MOE_BLOCK)
    n_rows = n_blocks * MOE_BLOCK
    row_tok = jnp.zeros((n_rows,), jnp.int32).at[dest].set(flat_tok[order])
    row_gate = jnp.zeros((n_rows,), jnp.float32).at[dest].set(flat_g[order])
    block_start = jnp.arange(n_blocks) * MOE_BLOCK
    block_e = jnp.minimum(jnp.searchsorted(end_padded, block_start, side='right'), E - 1)

    def block(args):
        toks, e = args
        xb = hf[toks]
        return (jax.nn.silu(xb @ w1[e]) * (xb @ w3[e])) @ w2[e]

    y = lax.map(block, (row_tok.reshape(n_blocks, MOE_BLOCK), block_e))
    y = y.reshape(n_rows, D) * row_gate[:, None].astype(h.dtype)
    out = jnp.zeros((N, D), h.dtype).at[row_tok].add(y)
    return out.reshape(B, S, D)


def setup_inputs(seed: int = 0) -> dict:
    key = jax.random.key(seed)
    ks = jax.random.split(key, 24)
    n = jax.random.normal
    f32 = jnp.float32
    return {
        "x": n(ks[0], (BATCH, SEQ, D_MODEL), f32),
        "positions": jnp.broadcast_to(jnp.arange(SEQ, dtype=jnp.int32), (BATCH, SEQ)),
        "w_in": n(ks[1], (DEPTH, D_MODEL, N_IN), f32) * D_MODEL ** -0.5,
        "w_gate_up": n(ks[2], (DEPTH, GLA_GATE_RANK, GLA_HEADS * GLA_DK), f32) * GLA_GATE_RANK ** -0.5,
        "b_gate": n(ks[3], (DEPTH, GLA_HEADS * GLA_DK), f32) * 0.1,
        "gla_norm_g": 1.0 + 0.02 * n(ks[4], (DEPTH, GLA_DV), f32),
        "lambda_q1": n(ks[5], (DEPTH, DIFF_D), f32) * 0.1,
        "lambda_k1": n(ks[6], (DEPTH, DIFF_D), f32) * 0.1,
        "lambda_q2": n(ks[7], (DEPTH, DIFF_D), f32) * 0.1,
        "lambda_k2": n(ks[8], (DEPTH, DIFF_D), f32) * 0.1,
        "diff_norm_g": 1.0 + 0.02 * n(ks[9], (DEPTH, 2 * DIFF_D), f32),
        "w_out": n(ks[10], (DEPTH, MIX_WIDTH, D_MODEL), f32) * (MIX_WIDTH ** -0.5) * DEEPNORM_BETA,
        "ln1_g": 1.0 + 0.02 * n(ks[11], (DEPTH, D_MODEL), f32),
        "ln1_b": 0.02 * n(ks[12], (DEPTH, D_MODEL), f32),
        "ln2_g": 1.0 + 0.02 * n(ks[13], (DEPTH, D_MODEL), f32),
        "ln2_b": 0.02 * n(ks[14], (DEPTH, D_MODEL), f32),
        "ffn_w1": n(ks[15], (N_DENSE, D_MODEL, D_FF), f32) * D_MODEL ** -0.5,
        "ffn_w3": n(ks[16], (N_DENSE, D_MODEL, D_FF), f32) * D_MODEL ** -0.5,
        "ffn_w2": n(ks[17], (N_DENSE, D_FF, D_MODEL), f32) * (D_FF ** -0.5) * DEEPNORM_BETA,
        "router_w": n(ks[18], (N_MOE, D_MODEL, N_EXPERTS), f32) * D_MODEL ** -0.5,
        "router_b": n(ks[19], (N_MOE, N_EXPERTS), f32) * 0.01,
        "moe_w1": n(ks[20], (N_MOE, N_EXPERTS, D_MODEL, D_FF_EXPERT), f32) * D_MODEL ** -0.5,
        "moe_w3": n(ks[21], (N_MOE, N_EXPERTS, D_MODEL, D_FF_EXPERT), f32) * D_MODEL ** -0.5,
        "moe_w2": n(ks[22], (N_MOE, N_EXPERTS, D_FF_EXPERT, D_MODEL), f32) * (D_FF_EXPERT ** -0.5) * DEEPNORM_BETA,
    }


def reference(x, positions, w_in, w_gate_up, b_gate, gla_norm_g, lambda_q1, lambda_k1, lambda_q2,
              lambda_k2, diff_norm_g, w_out, ln1_g, ln1_b, ln2_g, ln2_b, ffn_w1, ffn_w3, ffn_w2,
              router_w, router_b, moe_w1, moe_w3, moe_w2):
    B, S, _ = x.shape
    for l in range(DEPTH):
        h = x @ w_in[l]
        (g_q, g_k, g_v, g_r, g_lr, d_q, d_k, d_v,
         s_q, s_k, s_v, i_q, i_k, i_w) = jnp.split(h, SPLIT_POINTS, axis=-1)

        log_a = jax.nn.log_sigmoid((g_lr @ w_gate_up[l] + b_gate[l]).astype(jnp.float32)) / GLA_TAU
        o_gla = gla_mixer(g_q.reshape(B, S, GLA_HEADS, GLA_DK), g_k.reshape(B, S, GLA_HEADS, GLA_DK),
                          g_v.reshape(B, S, GLA_HEADS, GLA_DV), log_a.reshape(B, S, GLA_HEADS, GLA_DK))
        o_gla = rms_norm(o_gla, gla_norm_g[l]).astype(x.dtype) * jax.nn.silu(g_r).reshape(B, S, GLA_HEADS, GLA_DV)

        lambda_init = 0.8 - 0.6 * math.exp(-0.3 * l)
        lam = (jnp.exp(jnp.sum((lambda_q1[l] * lambda_k1[l]).astype(jnp.float32)))
               - jnp.exp(jnp.sum((lambda_q2[l] * lambda_k2[l]).astype(jnp.float32))) + lambda_init)
        dq = partial_rope(d_q.reshape(B, S, DIFF_HEADS * 2, DIFF_D), positions).reshape(B, S, DIFF_HEADS, 2, DIFF_D)
        dk = partial_rope(d_k.reshape(B, S, DIFF_HEADS * 2, DIFF_D), positions).reshape(B, S, DIFF_HEADS, 2, DIFF_D)
        o_diff = diff_attention(dq, dk, d_v.reshape(B, S, DIFF_HEADS, 2 * DIFF_D), lam, lambda_init, diff_norm_g[l])

        sq = partial_rope(s_q.reshape(B, S, DSA_HEADS, DSA_HEAD_DIM), positions)
        sk = partial_rope(s_k.reshape(B, S, DSA_KV_HEADS, DSA_HEAD_DIM), positions)
        sv = s_v.reshape(B, S, DSA_KV_HEADS, DSA_HEAD_DIM)
        iq = partial_rope(i_q.reshape(B, S, IDX_HEADS, IDX_DIM), positions)
        ik = partial_rope(i_k.reshape(B, S, 1, IDX_DIM), positions)[:, :, 0]
        o_dsa = dsa_attention(sq, sk, sv, iq, ik, i_w)

        mix = jnp.concatenate([o_gla.reshape(B, S, MIX_GLA), o_diff.reshape(B, S, MIX_DIFF), o_dsa], axis=-1) @ w_out[l]
        x = layer_norm(DEEPNORM_ALPHA * x + mix, ln1_g[l], ln1_b[l])

        j = l // 2
        if l % 2 == 0:
            f = swiglu(x, ffn_w1[j], ffn_w3[j], ffn_w2[j])
        else:
            f = moe_swiglu(x, router_w[j], router_b[j], moe_w1[j], moe_w3[j], moe_w2[j])
        x = layer_norm(DEEPNORM_ALPHA * x + f, ln2_g[l], ln2_b[l])
    return x
```

```python
import contextlib
import numpy as np
import concourse.bass as bass
import concourse.mybir as mybir

F32 = mybir.dt.float32
BF16 = mybir.dt.bfloat16
I32 = mybir.dt.int32
U32 = mybir.dt.uint32
AF = mybir.ActivationFunctionType
ALU = mybir.AluOpType
AX = mybir.AxisListType

ENGS = ("pe", "act", "dve", "pool", "sp")
NDMASEM = 6


class Sched:
    def __init__(self, nc):
        self.nc = nc
        self.ops = []
        self.lastw = {}
        self.readers = {}
        self.es = contextlib.ExitStack()
        self._nt = 0

    def sb(self, shape, dt, name=None):
        self._nt += 1
        return self.es.enter_context(self.nc.sbuf_tensor(name or f"t{self._nt}", list(shape), dt))

    def ps(self, shape, dt=F32, name=None):
        self._nt += 1
        return self.es.enter_context(self.nc.psum_tensor(name or f"p{self._nt}", list(shape), dt))

    def add(self, eng, fn, reads=(), writes=(), dma=False):
        idx = len(self.ops)
        deps = set()
        for k in reads:
            w = self.lastw.get(k)
            if w is not None:
                deps.add(w)
        for k in writes:
            w = self.lastw.get(k)
            if w is not None:
                deps.add(w)
            for r in self.readers.get(k, {}).values():
                deps.add(r)
        deps.discard(idx)
        self.ops.append((eng, fn, sorted(deps), dma))
        for k in writes:
            self.lastw[k] = idx
            self.readers[k] = {}
        rk = ("dma", idx) if dma else eng
        for k in reads:
            self.readers.setdefault(k, {})[rk] = idx
        return idx

    def pe(self, fn, reads=(), writes=()):
        return self.add("pe", fn, reads, writes)

    def act(self, fn, reads=(), writes=()):
        return self.add("act", fn, reads, writes)

    def dve(self, fn, reads=(), writes=()):
        return self.add("dve", fn, reads, writes)

    def pool(self, fn, reads=(), writes=()):
        return self.add("pool", fn, reads, writes)

    def dma(self, q, fn, reads=(), writes=()):
        return self.add(q, fn, reads, writes, dma=True)

    def emit(self, final_wait_ops=()):
        nc = self.nc
        ops = self.ops
        n = len(ops)
        needed = [False] * n
        for (eng, fn, deps, dma) in ops:
            for d in deps:
                de, _, _, ddma = ops[d]
                if (not ddma) and de == "pe" and eng == "pe" and not dma:
                    continue
                needed[d] = True
        for d in final_wait_ops:
            needed[d] = True
        sem = {e: self.es.enter_context(nc.semaphore(f"s_{e}")) for e in ENGS if e != "sp"}
        dsem = {e: [self.es.enter_context(nc.semaphore(f"d_{e}{i}")) for i in range(NDMASEM)]
                for e in ENGS if e != "pe"}
        sig = [None] * n
        cnt = {e: 0 for e in ENGS}
        dcnt = {e: 0 for e in ENGS}
        dma_prev = {}
        dq = {e: [] for e in ENGS}
        for i, (eng, fn, deps, dma) in enumerate(ops):
            if dma:
                k = dcnt[eng]
                dcnt[eng] += 1
                sig[i] = (dsem[eng][k % NDMASEM], 16 * (k // NDMASEM + 1))
                if k >= NDMASEM:
                    dma_prev[i] = dq[eng][k - NDMASEM]
                dq[eng].append(i)
            elif needed[i]:
                cnt[eng] += 1
                sig[i] = (sem[eng], cnt[eng])
        per = {e: [] for e in ENGS}
        for i, o in enumerate(ops):
            per[o[0]].append(i)

        def run(engname, e):
            waited = {}
            for i in per[engname]:
                _, fn, deps, dma = ops[i]
                dl = list(deps)
                if dma and i in dma_prev:
                    dl.append(dma_prev[i])
                for d in dl:
                    de, _, _, ddma = ops[d]
                    if (not ddma) and de == "pe" and engname == "pe" and not dma:
                        continue
                    s, v = sig[d]
                    key = id(s)
                    if waited.get(key, 0) >= v:
                        continue
                    e.wait_ge(s, v)
                    waited[key] = v
                ins = fn(e)
                if dma:
                    s, v = sig[i]
                    ins.then_inc(s, 16)
                elif needed[i]:
                    s, v = sig[i]
                    ins.then_inc(s, 1)
            if engname == self.final_eng:
                for d in final_wait_ops:
                    s, v = sig[d]
                    e.wait_ge(s, v)

        self.final_eng = "sp"
        with nc.Block() as block:
            if per["sp"] or final_wait_ops:
                @block.sync
                def _(e):
                    run("sp", e)
            if per["pe"]:
                @block.tensor
                def _(e):
                    run("pe", e)
            if per["act"]:
                @block.scalar
                def _(e):
                    run("act", e)
            if per["dve"]:
                @block.vector
                def _(e):
                    run("dve", e)
            if per["pool"]:
                @block.gpsimd
                def _(e):
                    run("pool", e)
        self.es.close()


def _cast_eng(S, i):
    return ("dve", "pool")[i % 2]


def build_lin(K, T, Nc, x_f32, out_dt):
    nc = bass.Bass("TRN2", target_bir_lowering=False)
    KC, NT, TB = K // 128, Nc // 128, 512
    NB = T // TB
    xT = nc.dram_tensor("xT", [K, T], F32 if x_f32 else BF16, kind="ExternalInput").ap()
    W = nc.dram_tensor("W", [K, Nc], F32, kind="ExternalInput").ap()
    YT = nc.dram_tensor("YT", [Nc, T], out_dt, kind="ExternalOutput").ap()
    S = Sched(nc)
    Wb = S.sb([128, KC, Nc], BF16, "Wb")
    stg = [S.sb([128, Nc], F32, f"stg{i}") for i in range(2)]
    xb = [S.sb([128, KC, TB], BF16, f"xb{i}") for i in range(2)]
    ob = [S.sb([128, TB], out_dt, f"ob{i}") for i in range(4)]
    pb = [S.ps([128, TB], F32, f"pb{i}") for i in range(8)]
    Wv = W.rearrange("(c p) n -> p c n", p=128)
    xv = xT.rearrange("(c p) t -> p c t", p=128)
    for kc in range(KC):
        s = stg[kc % 2]
        S.dma("sp", lambda e, s=s, kc=kc: e.dma_start(out=s[:], in_=Wv[:, kc, :]), writes=[("stg", kc % 2)])
        S.add(_cast_eng(S, kc), lambda e, s=s, kc=kc: e.tensor_copy(out=Wb[:, kc, :], in_=s[:]),
              reads=[("stg", kc % 2)], writes=[("Wb", kc)])
    outs = []
    it = 0
    for tb in range(NB):
        xq = "pool" if x_f32 else "sp"
        b = xb[tb % 2]
        S.dma(xq, lambda e, b=b, tb=tb: e.dma_start(out=b[:], in_=xv[:, :, tb * TB:(tb + 1) * TB]),
              writes=[("xb", tb % 2)])
        for nt in range(NT):
            p = pb[it % 8]
            o = ob[it % 4]
            for kc in range(KC):
                S.pe(lambda e, p=p, b=b, kc=kc, nt=nt: e.matmul(p[:], lhsT=Wb[:, kc, nt * 128:(nt + 1) * 128],
                                                               rhs=b[:, kc, :], start=(kc == 0), stop=(kc == KC - 1)),
                     reads=[("xb", tb % 2), ("Wb", kc)], writes=[("pb", it % 8)])
            if it % 2 == 0:
                S.dve(lambda e, p=p, o=o: e.tensor_copy(out=o[:], in_=p[:]), reads=[("pb", it % 8)], writes=[("ob", it % 4)])
            else:
                S.act(lambda e, p=p, o=o: e.copy(out=o[:], in_=p[:]), reads=[("pb", it % 8)], writes=[("ob", it % 4)])
            outs.append(S.dma("sp", lambda e, o=o, nt=nt, tb=tb: e.dma_start(
                out=YT[nt * 128:(nt + 1) * 128, tb * TB:(tb + 1) * TB], in_=o[:]), reads=[("ob", it % 4)]))
            it += 1
    S.emit(final_wait_ops=outs[-8:])
    return nc


def build_ffn(T, F, gated):
    nc = bass.Bass("TRN2", target_bir_lowering=False)
    D = 4096
    KC, FT, TB, NO = D // 128, F // 128, 512, D // 128
    NB = T // TB
    xT = nc.dram_tensor("xT", [D, T], BF16, kind="ExternalInput").ap()
    W1 = nc.dram_tensor("W1", [D, F], F32, kind="ExternalInput").ap()
    W3 = nc.dram_tensor("W3", [D, F], F32, kind="ExternalInput").ap()
    W2 = nc.dram_tensor("W2", [F, D], F32, kind="ExternalInput").ap()
    if gated:
        gate = nc.dram_tensor("gate", [1, T], F32, kind="ExternalInput").ap()
    yT = nc.dram_tensor("yT", [D, T], BF16, kind="ExternalOutput").ap()
    W1s = nc.dram_tensor("W1s", [FT, 128, KC * 128], BF16).ap()
    W3s = nc.dram_tensor("W3s", [FT, 128, KC * 128], BF16).ap()
    W2s = nc.dram_tensor("W2s", [NO, 128, FT * 128], BF16).ap()
    S = Sched(nc)
    SW = max(F, D)
    stg = [S.sb([128, SW], F32, f"stg{i}") for i in range(1)]
    stb = [S.sb([128, SW], BF16, f"stb{i}") for i in range(2)]
    xb = [S.sb([128, KC, TB], BF16, f"xb{i}") for i in range(2)]
    h1 = S.sb([128, FT, TB], BF16, "h1")
    w1t = [S.sb([128, KC * 128], BF16, f"w1t{i}") for i in range(2)]
    w3t = [S.sb([128, KC * 128], BF16, f"w3t{i}") for i in range(2)]
    w2t = [S.sb([128, FT * 128], BF16, f"w2t{i}") for i in range(2)]
    sg = [S.sb([128, TB], F32, f"sg{i}") for i in range(2)]
    ob = [S.sb([128, TB], BF16, f"ob{i}") for i in range(4)]
    gb = [S.sb([128, TB], F32, f"gb{i}") for i in range(2)] if gated else None
    pb = [S.ps([128, TB], F32, f"pb{i}") for i in range(8)]
    n = 0
    for (Wsrc, Wdst, rows, cols, cch) in ((W1, W1s, D, F, KC), (W3, W3s, D, F, KC), (W2, W2s, F, D, FT)):
        Wv = Wsrc.rearrange("(c p) n -> p c n", p=128)
        Dv = Wdst.rearrange("n p (c j) -> p c n j", c=cch)
        for c in range(rows // 128):
            s, sb_ = stg[0], stb[n % 2]
            S.dma("sp", lambda e, s=s, Wv=Wv, c=c, cols=cols: e.dma_start(out=s[:, 0:cols], in_=Wv[:, c, :]),
                  writes=[("stg", 0)])
            S.add(_cast_eng(S, n), lambda e, s=s, sb_=sb_, cols=cols: e.tensor_copy(out=sb_[:, 0:cols], in_=s[:, 0:cols]),
                  reads=[("stg", 0)], writes=[("stb", n % 2)])
            S.dma("act", lambda e, sb_=sb_, Dv=Dv, c=c, cols=cols: e.dma_start(
                out=Dv[:, c, :, :], in_=sb_[:, 0:cols].rearrange("p (n j) -> p n j", j=128)),
                reads=[("stb", n % 2)], writes=[(id(Wdst.tensor), "scr")])
            n += 1
    keyW1, keyW3, keyW2 = (id(W1s.tensor), "scr"), (id(W3s.tensor), "scr"), (id(W2s.tensor), "scr")
    xv = xT.rearrange("(c p) t -> p c t", p=128)
    outs = []
    ip = 0
    iw = 0
    io = 0
    for tb in range(NB):
        b = xb[tb % 2]
        S.dma("sp", lambda e, b=b, tb=tb: e.dma_start(out=b[:], in_=xv[:, :, tb * TB:(tb + 1) * TB]), writes=[("xb", tb % 2)])
        if gated:
            g = gb[tb % 2]
            S.dma("sp", lambda e, g=g, tb=tb: e.dma_start(out=g[:], in_=gate[0, tb * TB:(tb + 1) * TB].partition_broadcast(128)),
                  writes=[("gb", tb % 2)])
        for ft in range(FT):
            a1, a3 = w1t[iw % 2], w3t[iw % 2]
            S.dma("sp", lambda e, a1=a1, ft=ft: e.dma_start(out=a1[:], in_=W1s[ft]), reads=[keyW1], writes=[("w1t", iw % 2)])
            S.dma("sp", lambda e, a3=a3, ft=ft: e.dma_start(out=a3[:], in_=W3s[ft]), reads=[keyW3], writes=[("w3t", iw % 2)])
            pa, pg = pb[ip % 8], pb[(ip + 1) % 8]
            for kc in range(KC):
                S.pe(lambda e, pa=pa, a1=a1, b=b, kc=kc: e.matmul(pa[:], lhsT=a1[:, kc * 128:(kc + 1) * 128], rhs=b[:, kc, :],
                                                                  start=(kc == 0), stop=(kc == KC - 1)),
                     reads=[("w1t", iw % 2), ("xb", tb % 2)], writes=[("pb", ip % 8)])
            for kc in range(KC):
                S.pe(lambda e, pg=pg, a3=a3, b=b, kc=kc: e.matmul(pg[:], lhsT=a3[:, kc * 128:(kc + 1) * 128], rhs=b[:, kc, :],
                                                                  start=(kc == 0), stop=(kc == KC - 1)),
                     reads=[("w3t", iw % 2), ("xb", tb % 2)], writes=[("pb", (ip + 1) % 8)])
            s_ = sg[ft % 2]
            S.act(lambda e, s_=s_, pa=pa: e.activation(out=s_[:], in_=pa[:], func=AF.Silu),
                  reads=[("pb", ip % 8)], writes=[("sg", ft % 2)])
            S.dve(lambda e, s_=s_, pg=pg, ft=ft: e.tensor_tensor(out=h1[:, ft, :], in0=s_[:], in1=pg[:], op=ALU.mult),
                  reads=[("sg", ft % 2), ("pb", (ip + 1) % 8)], writes=[("h1", ft)])
            ip += 2
            iw += 1
        for no in range(NO):
            a2 = w2t[no % 2]
            S.dma("sp", lambda e, a2=a2, no=no: e.dma_start(out=a2[:], in_=W2s[no]), reads=[keyW2], writes=[("w2t", no % 2)])
            po = pb[ip % 8]
            for fc in range(FT):
                S.pe(lambda e, po=po, a2=a2, fc=fc: e.matmul(po[:], lhsT=a2[:, fc * 128:(fc + 1) * 128], rhs=h1[:, fc, :],
                                                             start=(fc == 0), stop=(fc == FT - 1)),
                     reads=[("w2t", no % 2), ("h1", fc)], writes=[("pb", ip % 8)])
            o = ob[io % 4]
            if gated:
                S.dve(lambda e, o=o, po=po, g=g: e.tensor_tensor(out=o[:], in0=po[:], in1=g[:], op=ALU.mult),
                      reads=[("pb", ip % 8), ("gb", tb % 2)], writes=[("ob", io % 4)])
            elif io % 2 == 0:
                S.dve(lambda e, o=o, po=po: e.tensor_copy(out=o[:], in_=po[:]), reads=[("pb", ip % 8)], writes=[("ob", io % 4)])
            else:
                S.act(lambda e, o=o, po=po: e.copy(out=o[:], in_=po[:]), reads=[("pb", ip % 8)], writes=[("ob", io % 4)])
            outs.append(S.dma("sp", lambda e, o=o, no=no, tb=tb: e.dma_start(
                out=yT[no * 128:(no + 1) * 128, tb * TB:(tb + 1) * TB], in_=o[:]), reads=[("ob", io % 4)]))
            ip += 1
            io += 1
    S.emit(final_wait_ops=outs[-8:])
    return nc


def build_ln(Tc, n_add, add_bf16, alpha, eps=1e-5):
    nc = bass.Bass("TRN2", target_bir_lowering=False)
    D = 4096
    NTL = Tc // 128
    adt = BF16 if add_bf16 else F32
    x = nc.dram_tensor("x", [Tc, D], F32, kind="ExternalInput").ap()
    adds = [nc.dram_tensor(f"a{i}", [Tc, D], adt, kind="ExternalInput").ap() for i in range(n_add)]
    g = nc.dram_tensor("g", [1, D], F32, kind="ExternalInput").ap()
    bb = nc.dram_tensor("b", [1, D], F32, kind="ExternalInput").ap()
    y = nc.dram_tensor("y", [Tc, D], F32, kind="ExternalOutput").ap()
    yb = nc.dram_tensor("yb", [Tc, D], BF16, kind="ExternalOutput").ap()
    S = Sched(nc)
    gt = S.sb([128, D], F32, "gt")
    bt = S.sb([128, D], F32, "bt")
    xt = [S.sb([128, D], F32, f"xt{i}") for i in range(2)]
    at = [S.sb([128, D], adt, f"at{i}") for i in range(2)]
    yt = [S.sb([128, D], F32, f"yt{i}") for i in range(2)]
    ybt = [S.sb([128, D], BF16, f"ybt{i}") for i in range(2)]
    st = [S.sb([128, 8, 6], F32, f"st{i}") for i in range(2)]
    mv = [S.sb([128, 8], F32, f"mv{i}") for i in range(2)]
    S.dma("sp", lambda e: e.dma_start(out=gt[:], in_=g[0, :].partition_broadcast(128)), writes=["gt"])
    S.dma("sp", lambda e: e.dma_start(out=bt[:], in_=bb[0, :].partition_broadcast(128)), writes=["bt"])
    outs = []
    ia = 0
    for t in range(NTL):
        j = t % 2
        X, Y, YB, ST, MV = xt[j], yt[j], ybt[j], st[j], mv[j]
        rs = slice(t * 128, (t + 1) * 128)
        S.dma("sp", lambda e, X=X, rs=rs: e.dma_start(out=X[:], in_=x[rs, :]), writes=[("xt", j)])
        for i in range(n_add):
            A = at[ia % 2]
            S.dma("act" if i % 2 else "sp", lambda e, A=A, rs=rs, i=i: e.dma_start(out=A[:], in_=adds[i][rs, :]), writes=[("at", ia % 2)])
            if i == 0:
                S.dve(lambda e, X=X, A=A: e.scalar_tensor_tensor(out=X[:], in0=X[:], scalar=float(alpha), in1=A[:],
                                                                 op0=ALU.mult, op1=ALU.add),
                      reads=[("at", ia % 2), ("xt", j)], writes=[("xt", j)])
            else:
                S.dve(lambda e, X=X, A=A: e.tensor_tensor(out=X[:], in0=X[:], in1=A[:], op=ALU.add),
                      reads=[("at", ia % 2), ("xt", j)], writes=[("xt", j)])
            ia += 1
        for c in range(8):
            S.dve(lambda e, X=X, ST=ST, c=c: e.bn_stats(out=ST[:, c, :], in_=X[:, c * 512:(c + 1) * 512]),
                  reads=[("xt", j)], writes=[("st", j, c)])
        S.dve(lambda e, ST=ST, MV=MV: e.bn_aggr(out=MV[:, 0:2], in_=ST[:].rearrange("p c s -> p (c s)")),
              reads=[("st", j, c) for c in range(8)], writes=[("mv", j)])
        S.dve(lambda e, MV=MV: e.tensor_scalar(out=MV[:, 2:3], in0=MV[:, 1:2], scalar1=float(eps), scalar2=None, op0=ALU.add),
              reads=[("mv", j)], writes=[("mv", j)])
        S.act(lambda e, MV=MV: e.activation(out=MV[:, 3:4], in_=MV[:, 2:3], func=AF.Sqrt), reads=[("mv", j)], writes=[("mv", j)])
        S.dve(lambda e, MV=MV: e.reciprocal(out=MV[:, 4:5], in_=MV[:, 3:4]), reads=[("mv", j)], writes=[("mv", j)])
        S.dve(lambda e, MV=MV: e.scalar_tensor_tensor(out=MV[:, 5:6], in0=MV[:, 0:1], scalar=-1.0, in1=MV[:, 4:5],
                                                      op0=ALU.mult, op1=ALU.mult), reads=[("mv", j)], writes=[("mv", j)])
        S.act(lambda e, X=X, Y=Y, MV=MV: e.activation(out=Y[:], in_=X[:], func=AF.Identity, bias=MV[:, 5:6], scale=MV[:, 4:5]),
              reads=[("xt", j), ("mv", j)], writes=[("yt", j)])
        S.dve(lambda e, Y=Y: e.tensor_tensor(out=Y[:], in0=Y[:], in1=gt[:], op=ALU.mult), reads=[("yt", j), "gt"], writes=[("yt", j)])
        S.dve(lambda e, Y=Y: e.tensor_tensor(out=Y[:], in0=Y[:], in1=bt[:], op=ALU.add), reads=[("yt", j), "bt"], writes=[("yt", j)])
        S.pool(lambda e, Y=Y, YB=YB: e.tensor_copy(out=YB[:], in_=Y[:]), reads=[("yt", j)], writes=[("ybt", j)])
        outs.append(S.dma("sp", lambda e, Y=Y, rs=rs: e.dma_start(out=y[rs, :], in_=Y[:]), reads=[("yt", j)]))
        outs.append(S.dma("sp", lambda e, YB=YB, rs=rs: e.dma_start(out=yb[rs, :], in_=YB[:]), reads=[("ybt", j)]))
    S.emit(final_wait_ops=outs[-6:])
    return nc


def build_router(Tc):
    nc = bass.Bass("TRN2", target_bir_lowering=False)
    D, E = 4096, 8
    KC = D // 128
    NTL = Tc // 128
    xT = nc.dram_tensor("xT", [D, Tc], F32, kind="ExternalInput").ap()
    Wr = nc.dram_tensor("Wr", [D, E], F32, kind="ExternalInput").ap()
    rb = nc.dram_tensor("rb", [1, E], F32, kind="ExternalInput").ap()
    gates = nc.dram_tensor("gates", [Tc, E], F32, kind="ExternalOutput").ap()
    S = Sched(nc)
    wt = S.sb([128, KC, E], F32, "wt")
    rbt = S.sb([128, E], F32, "rbt")
    xt = [S.sb([128, KC, 128], F32, f"xt{i}") for i in range(2)]
    wk = [S.sb([128, 48], F32, f"wk{i}") for i in range(2)]
    pp = [S.ps([128, E], F32, f"pp{i}") for i in range(2)]
    S.dma("sp", lambda e: e.dma_start(out=wt[:], in_=Wr.rearrange("(c p) n -> p c n", p=128)), writes=["wt"])
    S.dma("sp", lambda e: e.dma_start(out=rbt[:], in_=rb[0, :].partition_broadcast(128)), writes=["rbt"])
    xv = xT.rearrange("(c p) t -> p c t", p=128)
    outs = []
    for t in range(NTL):
        j = t % 2
        X, P, Wk = xt[j], pp[j], wk[j]
        S.dma("sp", lambda e, X=X, t=t: e.dma_start(out=X[:], in_=xv[:, :, t * 128:(t + 1) * 128]), writes=[("xt", j)])
        for kc in range(KC):
            S.pe(lambda e, X=X, P=P, kc=kc: e.matmul(P[:], lhsT=X[:, kc, :], rhs=wt[:, kc, :], start=(kc == 0), stop=(kc == KC - 1)),
                 reads=[("xt", j), "wt"], writes=[("pp", j)])
        lg, m8, nm1, msk, ex, ssum = Wk[:, 0:8], Wk[:, 8:16], Wk[:, 16:17], Wk[:, 24:32], Wk[:, 32:40], Wk[:, 17:18]
        K = ("wk", j)
        S.dve(lambda e, P=P, lg=lg: e.tensor_tensor(out=lg, in0=P[:], in1=rbt[:], op=ALU.add), reads=[("pp", j), "rbt"], writes=[K])
        S.dve(lambda e, lg=lg, m8=m8: e.max(out=m8, in_=lg), reads=[K], writes=[K])
        S.dve(lambda e, m8=m8, nm1=nm1: e.tensor_scalar(out=nm1, in0=m8[:, 0:1], scalar1=-1.0, scalar2=None, op0=ALU.mult), reads=[K], writes=[K])
        S.dve(lambda e, lg=lg, m8=m8, msk=msk: e.tensor_scalar(out=msk, in0=lg, scalar1=m8[:, 1:2], scalar2=None, op0=ALU.is_ge),
              reads=[K], writes=[K])
        S.act(lambda e, lg=lg, ex=ex, nm1=nm1: e.activation(out=ex, in_=lg, func=AF.Exp, bias=nm1, scale=1.0), reads=[K], writes=[K])
        S.dve(lambda e, ex=ex, msk=msk: e.tensor_tensor(out=ex, in0=ex, in1=msk, op=ALU.mult), reads=[K], writes=[K])
        S.dve(lambda e, ex=ex, ssum=ssum: e.reduce_sum(out=ssum, in_=ex, axis=AX.X), reads=[K], writes=[K])
        S.dve(lambda e, ssum=ssum: e.reciprocal(out=ssum, in_=ssum), reads=[K], writes=[K])
        S.dve(lambda e, ex=ex, ssum=ssum: e.tensor_scalar(out=ex, in0=ex, scalar1=ssum, scalar2=None, op0=ALU.mult), reads=[K], writes=[K])
        outs.append(S.dma("sp", lambda e, ex=ex, t=t: e.dma_start(out=gates[t * 128:(t + 1) * 128, :], in_=ex), reads=[K]))
    S.emit(final_wait_ops=outs[-4:])
    return nc

import math
C1_2PI = 6.28125
C2_2PI = 2.0 * math.pi - 6.28125
MAGIC = 12582912.0


def build_rope(Tc):
    nc = bass.Bass("TRN2", target_bir_lowering=False)
    N16, N32 = 73, 12
    NTL = Tc // 128
    pos = nc.dram_tensor("pos", [Tc, 1], I32, kind="ExternalInput").ap()
    invf = nc.dram_tensor("invf", [1, 16], F32, kind="ExternalInput").ap()
    h16 = nc.dram_tensor("h16", [Tc, N16 * 64], BF16, kind="ExternalInput").ap()
    h32 = nc.dram_tensor("h32", [Tc, N32 * 128], BF16, kind="ExternalInput").ap()
    o16 = nc.dram_tensor("o16", [Tc, N16 * 64], BF16, kind="ExternalOutput").ap()
    o32 = nc.dram_tensor("o32", [Tc, N32 * 128], BF16, kind="ExternalOutput").ap()
    S = Sched(nc)
    ivt = S.sb([128, 16], F32, "ivt")
    S.dma("sp", lambda e: e.dma_start(out=ivt[:], in_=invf[0, :].partition_broadcast(128)), writes=["ivt"])
    A = [S.sb([128, N16 * 64], BF16, f"A{i}") for i in range(2)]
    Bt = [S.sb([128, N32 * 128], BF16, f"B{i}") for i in range(2)]
    pt = [S.sb([128, 1], I32, f"pt{i}") for i in range(2)]
    wk = [S.sb([128, 8, 16], F32, f"wk{i}") for i in range(2)]
    tmp = [[S.sb([128, N16 * 8], F32, f"tmp{i}_{k}") for k in range(4)] for i in range(2)]
    outs = []
    for t in range(NTL):
        j = t % 2
        rs = slice(t * 128, (t + 1) * 128)
        a, b, p, w, tm = A[j], Bt[j], pt[j], wk[j], tmp[j]
        S.dma("sp", lambda e, p=p, rs=rs: e.dma_start(out=p[:], in_=pos[rs, :]), writes=[("pt", j)])
        S.dma("sp", lambda e, a=a, rs=rs: e.dma_start(out=a[:], in_=h16[rs, :]), writes=[("A", j)])
        S.dma("act", lambda e, b=b, rs=rs: e.dma_start(out=b[:], in_=h32[rs, :]), writes=[("B", j)])
        K = ("wk", j)
        pf, ang, u, n_, r, ab, sn, cs = w[:, 0, 0:1], w[:, 1, :], w[:, 2, :], w[:, 3, :], w[:, 4, :], w[:, 5, :], w[:, 6, :], w[:, 7, :]
        S.dve(lambda e, pf=pf, p=p: e.tensor_copy(out=pf, in_=p[:]), reads=[("pt", j)], writes=[K])
        S.dve(lambda e, ang=ang, pf=pf: e.tensor_scalar(out=ang, in0=ivt[:], scalar1=pf, scalar2=None, op0=ALU.mult), reads=[K, "ivt"], writes=[K])
        S.dve(lambda e, u=u, ang=ang: e.tensor_scalar(out=u, in0=ang, scalar1=float(1.0 / (2 * math.pi)), scalar2=MAGIC, op0=ALU.mult, op1=ALU.add), reads=[K], writes=[K])
        S.dve(lambda e, u=u, n_=n_: e.tensor_scalar(out=n_, in0=u, scalar1=-MAGIC, scalar2=None, op0=ALU.add), reads=[K], writes=[K])
        S.dve(lambda e, r=r, n_=n_, ang=ang: e.scalar_tensor_tensor(out=r, in0=n_, scalar=-C1_2PI, in1=ang, op0=ALU.mult, op1=ALU.add), reads=[K], writes=[K])
        S.dve(lambda e, r=r, n_=n_: e.scalar_tensor_tensor(out=r, in0=n_, scalar=-C2_2PI, in1=r, op0=ALU.mult, op1=ALU.add), reads=[K], writes=[K])
        S.dve(lambda e, r=r: e.tensor_scalar(out=r, in0=r, scalar1=float(math.pi), scalar2=float(-math.pi), op0=ALU.min, op1=ALU.max), reads=[K], writes=[K])
        S.dve(lambda e, r=r, ab=ab: e.scalar_tensor_tensor(out=ab, in0=r, scalar=-1.0, in1=r, op0=ALU.mult, op1=ALU.max), reads=[K], writes=[K])
        S.dve(lambda e, ab=ab: e.tensor_scalar(out=ab, in0=ab, scalar1=-1.0, scalar2=float(math.pi / 2), op0=ALU.mult, op1=ALU.add), reads=[K], writes=[K])
        S.act(lambda e, sn=sn, r=r: e.activation(out=sn, in_=r, func=AF.Sin), reads=[K], writes=[K])
        S.act(lambda e, cs=cs, ab=ab: e.activation(out=cs, in_=ab, func=AF.Sin), reads=[K], writes=[K])
        for (buf, key, nh, hd, half, step) in ((a, ("A", j), N16, 64, 8, 2), (b, ("B", j), N32, 128, 16, 1)):
            hv = buf[:].rearrange("p (n d) -> p n d", d=hd)
            x1, x2 = hv[:, :, 0:half], hv[:, :, half:2 * half]
            cb = w[:, 7, 0:16:step].unsqueeze(1).broadcast_to([128, nh, half])
            sb_ = w[:, 6, 0:16:step].unsqueeze(1).broadcast_to([128, nh, half])
            T = [tm[k][:, 0:nh * half].rearrange("p (n d) -> p n d", d=half) for k in range(4)]
            TK = [("tmp", j, k) for k in range(4)]
            S.dve(lambda e, T=T, x1=x1, cb=cb: e.tensor_tensor(out=T[0], in0=x1, in1=cb, op=ALU.mult), reads=[key, K], writes=[TK[0]])
            S.dve(lambda e, T=T, x2=x2, sb_=sb_: e.tensor_tensor(out=T[1], in0=x2, in1=sb_, op=ALU.mult), reads=[key, K], writes=[TK[1]])
            S.dve(lambda e, T=T, x2=x2, cb=cb: e.tensor_tensor(out=T[2], in0=x2, in1=cb, op=ALU.mult), reads=[key, K], writes=[TK[2]])
            S.dve(lambda e, T=T, x1=x1, sb_=sb_: e.tensor_tensor(out=T[3], in0=x1, in1=sb_, op=ALU.mult), reads=[key, K], writes=[TK[3]])
            S.dve(lambda e, T=T, x1=x1: e.tensor_tensor(out=x1, in0=T[0], in1=T[1], op=ALU.subtract), reads=[TK[0], TK[1]], writes=[key])
            S.dve(lambda e, T=T, x2=x2: e.tensor_tensor(out=x2, in0=T[2], in1=T[3], op=ALU.add), reads=[TK[2], TK[3]], writes=[key])
        outs.append(S.dma("sp", lambda e, a=a, rs=rs: e.dma_start(out=o16[rs, :], in_=a[:]), reads=[("A", j)]))
        outs.append(S.dma("sp", lambda e, b=b, rs=rs: e.dma_start(out=o32[rs, :], in_=b[:]), reads=[("B", j)]))
    S.emit(final_wait_ops=outs[-6:])
    return nc


def build_gla(NSEG=8, eps=1e-5):
    nc = bass.Bass("TRN2", target_bir_lowering=False)
    DK, DV, C, SEG = 192, 384, 64, 512
    CPS = SEG // C
    Ttot = NSEG * SEG
    NCH = Ttot // C
    qT_d = nc.dram_tensor("qT", [DK, Ttot], BF16, kind="ExternalInput").ap()
    kT_d = nc.dram_tensor("kT", [DK, Ttot], BF16, kind="ExternalInput").ap()
    lrT_d = nc.dram_tensor("lrT", [16, Ttot], BF16, kind="ExternalInput").ap()
    Wg_d = nc.dram_tensor("Wg", [16, DK], F32, kind="ExternalInput").ap()
    bg_d = nc.dram_tensor("bg", [DK, 1], F32, kind="ExternalInput").ap()
    v_d = nc.dram_tensor("v", [C, NCH, DV], BF16, kind="ExternalInput").ap()
    gr_d = nc.dram_tensor("gr", [C, NCH, DV], BF16, kind="ExternalInput").ap()
    gn_d = nc.dram_tensor("gn", [1, DV], F32, kind="ExternalInput").ap()
    o_d = nc.dram_tensor("o", [C, NCH, DV], BF16, kind="ExternalOutput").ap()
    S = Sched(nc)
    DCS = ((0, 128), (128, 64))
    ones = S.sb([128, 128], F32, "ones")
    ident_f = S.sb([128, 128], F32, "ident_f")
    ident = S.sb([128, 128], BF16, "ident")
    umask = S.sb([64, 64], F32, "umask")
    rmask = S.sb([128, SEG], F32, "rmask")
    Wg32 = S.sb([16, DK], F32, "Wg32")
    Wg = S.sb([16, DK], BF16, "Wgb")
    nbg = [S.sb([sz, 1], F32, f"nbg{i}") for i, (o_, sz) in enumerate(DCS)]
    gn = S.sb([64, DV], F32, "gnb")
    S.pool(lambda e: e.memset(ones[:], 1.0), writes=["ones"])
    S.pool(lambda e: e.affine_select(out=ident_f[:], in_=ones[:], pattern=[[1, 128]], compare_op=ALU.is_equal, fill=0.0,
                                     base=0, channel_multiplier=-1), reads=["ones"], writes=["ident_f"])
    S.dve(lambda e: e.tensor_copy(out=ident[:], in_=ident_f[:]), reads=["ident_f"], writes=["ident"])
    S.pool(lambda e: e.affine_select(out=umask[:], in_=ones[0:64, 0:64], pattern=[[1, 64]], compare_op=ALU.is_ge, fill=0.0,
                                     base=0, channel_multiplier=-1), reads=["ones"], writes=["umask"])
    S.pool(lambda e: e.memset(rmask[:], 1.0), writes=["rmask"])
    S.pool(lambda e: e.memset(rmask[:, 0:SEG:C], 0.0), writes=["rmask"])
    S.dma("sp", lambda e: e.dma_start(out=Wg32[:], in_=Wg_d), writes=["Wg32"])
    S.dve(lambda e: e.tensor_copy(out=Wg[:], in_=Wg32[:]), reads=["Wg32"], writes=["Wg"])
    for i, (o_, sz) in enumerate(DCS):
        S.dma("sp", lambda e, i=i, o_=o_, sz=sz: e.dma_start(out=nbg[i][:], in_=bg_d[o_:o_ + sz, :]), writes=[("nbg", i)])
        S.dve(lambda e, i=i: e.tensor_scalar(out=nbg[i][:], in0=nbg[i][:], scalar1=-1.0, scalar2=None, op0=ALU.mult),
              reads=[("nbg", i)], writes=[("nbg", i)])
    S.dma("sp", lambda e: e.dma_start(out=gn[:], in_=gn_d[0, :].partition_broadcast(64)), writes=["gn"])
    st32 = [S.sb([sz, DV], F32, f"st32_{i}") for i, (o_, sz) in enumerate(DCS)]
    stbf = [S.sb([sz, DV], BF16, f"stbf_{i}") for i, (o_, sz) in enumerate(DCS)]
    def mk(shape, dt, nm):
        return [S.sb(shape, dt, f"{nm}{j}") for j in range(2)]
    qr = [mk([sz, SEG], BF16, f"qr{i}_") for i, (o_, sz) in enumerate(DCS)]
    kr = [mk([sz, SEG], BF16, f"kr{i}_") for i, (o_, sz) in enumerate(DCS)]
    qt = [mk([sz, SEG], BF16, f"qt{i}_") for i, (o_, sz) in enumerate(DCS)]
    kt = [mk([sz, SEG], BF16, f"kt{i}_") for i, (o_, sz) in enumerate(DCS)]
    kh = [mk([sz, SEG], BF16, f"kh{i}_") for i, (o_, sz) in enumerate(DCS)]
    gdec = [mk([sz, CPS], F32, f"gdec{i}_") for i, (o_, sz) in enumerate(DCS)]
    nbl = [mk([sz, CPS], F32, f"nbl{i}_") for i, (o_, sz) in enumerate(DCS)]
    lr = mk([16, SEG], BF16, "lr")
    vv = mk([C, CPS, DV], BF16, "vv")
    grr = mk([C, CPS, DV], BF16, "grr")
    sgr = mk([C, CPS, DV], BF16, "sgr")
    khat = mk([C, CPS, DK], BF16, "khat")
    sTb = mk([C, CPS, C], BF16, "sTb")
    ob = mk([C, CPS, DV], BF16, "ob")
    t_e = [S.sb([sz, SEG], F32, f"t_e{i}") for i, (o_, sz) in enumerate(DCS)]
    t_cs = [S.sb([sz, SEG], F32, f"t_cs{i}") for i, (o_, sz) in enumerate(DCS)]
    t_x = [S.sb([sz, SEG], F32, f"t_x{i}") for i, (o_, sz) in enumerate(DCS)]
    rs_t = [S.sb([C, 8], F32, f"rs{i}") for i in range(2)]
    junk = S.sb([C, DV], F32, "junk")
    pz = S.ps([128, SEG], F32, "pz")
    pS = S.ps([C, 512], F32, "pS")
    pT = S.ps([128, 512], BF16, "pT")
    pO = [S.ps([C, 512], F32, f"pO{i}") for i in range(2)]
    pD = [S.ps([128, 512], F32, f"pD{i}") for i in range(2)]
    outs = []
    INV16 = 1.0 / 16.0
    for sg in range(NSEG):
        j = sg % 2
        ts = slice(sg * SEG, (sg + 1) * SEG)
        cs0 = sg * CPS
        S.dma("sp", lambda e, j=j, ts=ts: e.dma_start(out=lr[j][:], in_=lrT_d[:, ts]), writes=[("lr", j)])
        for i, (o_, sz) in enumerate(DCS):
            S.dma("sp", lambda e, i=i, j=j, o_=o_, sz=sz, ts=ts: e.dma_start(out=qr[i][j][:], in_=qT_d[o_:o_ + sz, ts]), writes=[("qr", i, j)])
            S.dma("sp", lambda e, i=i, j=j, o_=o_, sz=sz, ts=ts: e.dma_start(out=kr[i][j][:], in_=kT_d[o_:o_ + sz, ts]), writes=[("kr", i, j)])
        S.dma("act", lambda e, j=j, cs0=cs0: e.dma_start(out=vv[j][:], in_=v_d[:, cs0:cs0 + CPS, :]), writes=[("vv", j)])
        S.dma("act", lambda e, j=j, cs0=cs0: e.dma_start(out=grr[j][:], in_=gr_d[:, cs0:cs0 + CPS, :]), writes=[("grr", j)])
        S.act(lambda e, j=j: e.activation(out=sgr[j][:], in_=grr[j][:], func=AF.Silu), reads=[("grr", j)], writes=[("sgr", j)])
        S.pool(lambda e, j=j: e.tensor_tensor(out=sgr[j][:], in0=sgr[j][:], in1=gn[:].unsqueeze(1).broadcast_to([C, CPS, DV]), op=ALU.mult),
               reads=[("sgr", j), "gn"], writes=[("sgr", j)])
        for i, (o_, sz) in enumerate(DCS):
            E, CSM, X = t_e[i], t_cs[i], t_x[i]
            kE, kC, kX = ("t_e", i), ("t_cs", i), ("t_x", i)
            S.pe(lambda e, i=i, j=j, o_=o_, sz=sz: e.matmul(pz[0:sz, :], lhsT=Wg[:, o_:o_ + sz], rhs=lr[j][:], start=True, stop=True),
                 reads=["Wg", ("lr", j)], writes=["pz"])
            S.act(lambda e, i=i, sz=sz, E=E: e.activation(out=E[:], in_=pz[0:sz, :], func=AF.Exp, bias=nbg[i][:, 0:1], scale=-1.0),
                  reads=["pz", ("nbg", i)], writes=[kE])
            S.act(lambda e, E=E: e.activation(out=E[:], in_=E[:], func=AF.Ln, bias=1.0, scale=1.0), reads=[kE], writes=[kE])
            S.dve(lambda e, E=E, CSM=CSM, sz=sz: e.tensor_tensor_scan(out=CSM[:], data0=rmask[0:sz, :], data1=E[:], initial=0.0,
                                                                       op0=ALU.mult, op1=ALU.add), reads=[kE, "rmask"], writes=[kC])
            S.dve(lambda e, i=i, j=j, CSM=CSM: e.tensor_scalar(out=nbl[i][j][:], in0=CSM[:, C - 1:SEG:C], scalar1=-INV16, scalar2=None, op0=ALU.mult),
                  reads=[kC], writes=[("nbl", i, j)])
            S.act(lambda e, i=i, j=j: e.activation(out=gdec[i][j][:], in_=nbl[i][j][:], func=AF.Exp), reads=[("nbl", i, j)], writes=[("gdec", i, j)])
            S.act(lambda e, CSM=CSM, X=X: e.activation(out=X[:], in_=CSM[:], func=AF.Exp, scale=-INV16), reads=[kC], writes=[kX])
            S.dve(lambda e, i=i, j=j, X=X: e.scalar_tensor_tensor(out=qt[i][j][:], in0=X[:], scalar=float(DK ** -0.5), in1=qr[i][j][:],
                                                                   op0=ALU.mult, op1=ALU.mult), reads=[kX, ("qr", i, j)], writes=[("qt", i, j)])
            S.act(lambda e, CSM=CSM, X=X: e.activation(out=X[:], in_=CSM[:], func=AF.Exp, scale=INV16), reads=[kC], writes=[kX])
            S.dve(lambda e, i=i, j=j, X=X: e.tensor_tensor(out=kt[i][j][:], in0=X[:], in1=kr[i][j][:], op=ALU.mult),
                  reads=[kX, ("kr", i, j)], writes=[("kt", i, j)])
            for c in range(CPS):
                S.act(lambda e, i=i, j=j, c=c, CSM=CSM, X=X: e.activation(out=X[:, c * C:(c + 1) * C], in_=CSM[:, c * C:(c + 1) * C], func=AF.Exp,
                                                                           bias=nbl[i][j][:, c:c + 1], scale=INV16),
                      reads=[kC, ("nbl", i, j)], writes=[kX])
            S.dve(lambda e, i=i, j=j, X=X: e.tensor_tensor(out=kh[i][j][:], in0=X[:], in1=kr[i][j][:], op=ALU.mult),
                  reads=[kX, ("kr", i, j)], writes=[("kh", i, j)])
        for c in range(CPS):
            cs = slice(c * C, (c + 1) * C)
            S.pe(lambda e, j=j, cs=cs: e.transpose(out=pT[0:C, 0:128], in_=kh[0][j][:, cs], identity=ident[:]),
                 reads=[("kh", 0, j), "ident"], writes=["pT"])
            S.pe(lambda e, j=j, cs=cs: e.transpose(out=pT[0:C, 128:192], in_=kh[1][j][:, cs], identity=ident[0:64, 0:64]),
                 reads=[("kh", 1, j), "ident"], writes=["pT"])
            S.act(lambda e, j=j, c=c: e.copy(out=khat[j][:, c, :], in_=pT[0:C, 0:DK]), reads=["pT"], writes=[("khat", j, c)])
            S.pe(lambda e, j=j, cs=cs: e.matmul(pS[:, 0:C], lhsT=kt[0][j][:, cs], rhs=qt[0][j][:, cs], start=True, stop=False),
                 reads=[("kt", 0, j), ("qt", 0, j)], writes=["pS"])
            S.pe(lambda e, j=j, cs=cs: e.matmul(pS[:, 0:C], lhsT=kt[1][j][:, cs], rhs=qt[1][j][:, cs], start=False, stop=True),
                 reads=[("kt", 1, j), ("qt", 1, j)], writes=["pS"])
            S.dve(lambda e, j=j, c=c: e.tensor_tensor(out=sTb[j][:, c, :], in0=pS[:, 0:C], in1=umask[:], op=ALU.mult),
                  reads=["pS", "umask"], writes=[("sTb", j, c)])
        for c in range(CPS):
            gc = cs0 + c
            cs = slice(c * C, (c + 1) * C)
            po = pO[gc % 2]
            kpo = ("pO", gc % 2)
            first = (gc == 0)
            S.pe(lambda e, j=j, c=c, po=po, first=first: e.matmul(po[:, 0:DV], lhsT=sTb[j][:, c, :], rhs=vv[j][:, c, :], start=True, stop=first),
                 reads=[("sTb", j, c), ("vv", j)], writes=[kpo])
            if not first:
                S.pe(lambda e, j=j, cs=cs, po=po: e.matmul(po[:, 0:DV], lhsT=qt[0][j][:, cs], rhs=stbf[0][:], start=False, stop=False),
                     reads=[("qt", 0, j), ("stbf", 0)], writes=[kpo])
                S.pe(lambda e, j=j, cs=cs, po=po: e.matmul(po[:, 0:DV], lhsT=qt[1][j][:, cs], rhs=stbf[1][:], start=False, stop=True),
                     reads=[("qt", 1, j), ("stbf", 1)], writes=[kpo])
            for i, (o_, sz) in enumerate(DCS):
                pd = pD[i]
                S.pe(lambda e, i=i, j=j, c=c, o_=o_, sz=sz, pd=pd: e.matmul(pd[0:sz, 0:DV], lhsT=khat[j][:, c, o_:o_ + sz], rhs=vv[j][:, c, :],
                                                                             start=True, stop=True),
                     reads=[("khat", j, c), ("vv", j)], writes=[("pD", i)])
                if first:
                    S.dve(lambda e, i=i, sz=sz, pd=pd: e.tensor_copy(out=st32[i][:], in_=pd[0:sz, 0:DV]), reads=[("pD", i)], writes=[("st32", i)])
                else:
                    S.dve(lambda e, i=i, j=j, c=c, sz=sz, pd=pd: e.scalar_tensor_tensor(out=st32[i][:], in0=st32[i][:], scalar=gdec[i][j][:, c:c + 1],
                                                                                         in1=pd[0:sz, 0:DV], op0=ALU.mult, op1=ALU.add),
                          reads=[("pD", i), ("st32", i), ("gdec", i, j)], writes=[("st32", i)])
                S.act(lambda e, i=i: e.copy(out=stbf[i][:], in_=st32[i][:]), reads=[("st32", i)], writes=[("stbf", i)])
            R = rs_t[gc % 2]
            kR = ("rs", gc % 2)
            S.act(lambda e, po=po, R=R: e.activation(out=junk[:], in_=po[:, 0:DV], func=AF.Square, accum_out=R[:, 0:1]),
                  reads=[kpo], writes=[kR, "junk"])
            S.dve(lambda e, R=R: e.tensor_scalar(out=R[:, 1:2], in0=R[:, 0:1], scalar1=float(1.0 / DV), scalar2=float(eps), op0=ALU.mult, op1=ALU.add),
                  reads=[kR], writes=[kR])
            S.act(lambda e, R=R: e.activation(out=R[:, 2:3], in_=R[:, 1:2], func=AF.Sqrt), reads=[kR], writes=[kR])
            S.dve(lambda e, R=R: e.reciprocal(out=R[:, 3:4], in_=R[:, 2:3]), reads=[kR], writes=[kR])
            S.dve(lambda e, j=j, c=c, po=po, R=R: e.scalar_tensor_tensor(out=ob[j][:, c, :], in0=po[:, 0:DV], scalar=R[:, 3:4], in1=sgr[j][:, c, :],
                                                                          op0=ALU.mult, op1=ALU.mult), reads=[kpo, kR, ("sgr", j)], writes=[("ob", j)])
        outs.append(S.dma("sp", lambda e, j=j, cs0=cs0: e.dma_start(out=o_d[:, cs0:cs0 + CPS, :], in_=ob[j][:]), reads=[("ob", j)]))
    S.emit(final_wait_ops=outs[-3:])
    return nc

NEG = -1.0e9


def _softmax_pv_consts(S):
    ones = S.sb([128, 128], F32, "c_ones")
    ident_f = S.sb([128, 128], F32, "c_identf")
    ident = S.sb([128, 128], BF16, "c_ident")
    S.pool(lambda e: e.memset(ones[:], 1.0), writes=["c_ones"])
    S.pool(lambda e: e.affine_select(out=ident_f[:], in_=ones[:], pattern=[[1, 128]], compare_op=ALU.is_equal, fill=0.0,
                                     base=0, channel_multiplier=-1), reads=["c_ones"], writes=["c_identf"])
    S.dve(lambda e: e.tensor_copy(out=ident[:], in_=ident_f[:]), reads=["c_identf"], writes=["c_ident"])
    return ident


def build_diff(NSLOT, SB, NH, lam_init, eps=1e-5):
    nc = bass.Bass("TRN2", target_bir_lowering=False)
    D, DV = 64, 128
    TK = NSLOT * SB * 128
    TQ = NSLOT * 128
    NKBT = TK // 128
    qT_d = nc.dram_tensor("qT", [NH, 128, TQ], BF16, kind="ExternalInput").ap()
    kT_d = nc.dram_tensor("kT", [NH, 128, TK], BF16, kind="ExternalInput").ap()
    v_d = nc.dram_tensor("v", [NH, 128, NKBT, DV], BF16, kind="ExternalInput").ap()
    msk_d = nc.dram_tensor("msk", [NSLOT, 128, SB * 128], F32, kind="ExternalInput").ap()
    lam_d = nc.dram_tensor("lamv", [4, D], F32, kind="ExternalInput").ap()
    g_d = nc.dram_tensor("g", [1, DV], F32, kind="ExternalInput").ap()
    o_d = nc.dram_tensor("o", [TQ, NH, DV], BF16, kind="ExternalOutput").ap()
    S = Sched(nc)
    ident = _softmax_pv_consts(S)
    lv = S.sb([128, 4, D], F32, "lv")
    lw = S.sb([128, 8], F32, "lw")
    junk = S.sb([128, DV], F32, "junk")
    gt = S.sb([128, DV], F32, "gt")
    for i in range(4):
        S.dma("sp", lambda e, i=i: e.dma_start(out=lv[:, i, :], in_=lam_d[i, :].partition_broadcast(128)), writes=[("lv", i)])
    S.dma("sp", lambda e: e.dma_start(out=gt[:], in_=g_d[0, :].partition_broadcast(128)), writes=["gt"])
    S.dve(lambda e: e.tensor_scalar(out=gt[:], in0=gt[:], scalar1=float(1.0 - lam_init), scalar2=None, op0=ALU.mult), reads=["gt"], writes=["gt"])
    for i in range(2):
        S.dve(lambda e, i=i: e.tensor_tensor(out=junk[:, 0:D], in0=lv[:, 2 * i, :], in1=lv[:, 2 * i + 1, :], op=ALU.mult),
              reads=[("lv", 2 * i), ("lv", 2 * i + 1)], writes=["junk"])
        S.dve(lambda e, i=i: e.reduce_sum(out=lw[:, i:i + 1], in_=junk[:, 0:D], axis=AX.X), reads=["junk"], writes=["lw"])
    S.act(lambda e: e.activation(out=lw[:, 2:4], in_=lw[:, 0:2], func=AF.Exp), reads=["lw"], writes=["lw"])
    S.dve(lambda e: e.tensor_tensor(out=lw[:, 4:5], in0=lw[:, 3:4], in1=lw[:, 2:3], op=ALU.subtract), reads=["lw"], writes=["lw"])
    S.dve(lambda e: e.tensor_scalar(out=lw[:, 5:6], in0=lw[:, 4:5], scalar1=float(-lam_init), scalar2=None, op0=ALU.add), reads=["lw"], writes=["lw"])
    nlam = lw[:, 5:6]
    mk_ = S.sb([128, NSLOT, SB * 128], F32, "mk_sb")
    S.dma("sp", lambda e: e.dma_start(out=mk_[:], in_=msk_d.rearrange("s p k -> p s k")), writes=["mk"])
    qt = [S.sb([128, TQ], BF16, f"qt{i}") for i in range(2)]
    kt = [S.sb([128, TK], BF16, f"kt{i}") for i in range(2)]
    vt = [S.sb([128, NKBT, DV], BF16, f"vt{i}") for i in range(2)]
    Sb = [[S.sb([128, TK], F32, f"S{c}_{i}") for i in range(2)] for c in range(2)]
    Ab = [S.sb([128, TK], BF16, f"A{i}") for i in range(2)]
    AT = [S.sb([128, NKBT, 128], BF16, f"AT{i}") for i in range(2)]
    st = [S.sb([128, 64], F32, f"stt{i}") for i in range(2)]
    ob = [S.sb([128, DV], BF16, f"ob{i}") for i in range(4)]
    pS = [S.ps([128, 512], F32, f"pS{i}") for i in range(4)]
    pT = [S.ps([128, 8, 128], BF16, f"pT{i}") for i in range(2)]
    pO = [S.ps([128, 512], F32, f"pO{i}") for i in range(2)]
    outs = []
    it = 0
    ips = 0
    ipt = 0
    for h in range(NH):
        hj = h % 2
        S.dma("sp", lambda e, h=h, hj=hj: e.dma_start(out=qt[hj][:], in_=qT_d[h]), writes=[("qt", hj)])
        S.dma("sp", lambda e, h=h, hj=hj: e.dma_start(out=kt[hj][:], in_=kT_d[h]), writes=[("kt", hj)])
        S.dma("act", lambda e, h=h, hj=hj: e.dma_start(out=vt[hj][:], in_=v_d[h]), writes=[("vt", hj)])
        for k in range(NSLOT):
            j = it % 2
            L = SB * (k + 1) * 128
            NKB = L // 128
            L0 = L - SB * 128
            ST = st[j]
            kST = ("st", j)
            qs = slice(k * 128, (k + 1) * 128)
            nmx = 0
            for c in range(2):
                Sc = Sb[c][j]
                kS = ("S", c, j)
                ps_ = slice(64 * c, 64 * c + 64)
                col = 0
                while col < L:
                    w = min(512, L - col) if col >= L0 else min(512, L0 - col)
                    p = pS[ips % 4]
                    kp = ("pS", ips % 4)
                    S.pe(lambda e, p=p, hj=hj, ps_=ps_, qs=qs, col=col, w=w: e.matmul(p[:, 0:w], lhsT=qt[hj][ps_, qs], rhs=kt[hj][ps_, col:col + w],
                                                                                  start=True, stop=True),
                         reads=[("qt", hj), ("kt", hj)], writes=[kp])
                    mcol = ST[:, 16 * c + nmx % 16: 16 * c + nmx % 16 + 1]
                    if col >= L0:
                        S.dve(lambda e, p=p, Sc=Sc, col=col, w=w, k=k, L0=L0: e.tensor_tensor(
                            out=Sc[:, col:col + w], in0=p[:, 0:w], in1=mk_[:, k, col - L0:col - L0 + w], op=ALU.add),
                            reads=[kp, "mk"], writes=[kS])
                        S.dve(lambda e, Sc=Sc, col=col, w=w, mcol=mcol: e.reduce_max(out=mcol, in_=Sc[:, col:col + w], axis=AX.X),
                              reads=[kS], writes=[(kST, c, nmx)])
                    else:
                        S.dve(lambda e, p=p, Sc=Sc, col=col, w=w, mcol=mcol: e.tensor_scalar(
                            out=Sc[:, col:col + w], in0=p[:, 0:w], scalar1=1.0, scalar2=-3.0e38, op0=ALU.mult, op1=ALU.max, accum_out=mcol),
                            reads=[kp], writes=[kS, (kST, c, nmx)])
                    ips += 1
                    nmx += 1
                    col += w
                nm = nmx
                nmx = 0
                S.dve(lambda e, ST=ST, c=c, nm=nm: e.reduce_max(out=ST[:, 32 + c:33 + c], in_=ST[:, 16 * c:16 * c + nm], axis=AX.X),
                      reads=[(kST, c, i_) for i_ in range(nm)], writes=[(kST, "m", c)])
                S.dve(lambda e, ST=ST, c=c: e.tensor_scalar(out=ST[:, 34 + c:35 + c], in0=ST[:, 32 + c:33 + c], scalar1=-0.125, scalar2=None, op0=ALU.mult),
                      reads=[(kST, "m", c)], writes=[(kST, "nmb", c)])
                S.act(lambda e, Sc=Sc, ST=ST, c=c, L=L: e.activation(out=Sc[:, 0:L], in_=Sc[:, 0:L], func=AF.Exp, bias=ST[:, 34 + c:35 + c], scale=0.125,
                                                                     accum_out=ST[:, 36 + c:37 + c]),
                      reads=[kS, (kST, "nmb", c)], writes=[kS, (kST, "l", c)])
            S.dve(lambda e, ST=ST: e.reciprocal(out=ST[:, 38:40], in_=ST[:, 36:38]), reads=[(kST, "l", 0), (kST, "l", 1)], writes=[(kST, "r")])
            S.dve(lambda e, ST=ST: e.tensor_scalar(out=ST[:, 40:41], in0=ST[:, 39:40], scalar1=nlam, scalar2=None, op0=ALU.mult),
                  reads=[(kST, "r"), "lw"], writes=[(kST, "r2")])
            A = Ab[j]
            kA = ("A", j)
            S.act(lambda e, A=A, j=j, ST=ST, L=L: e.activation(out=A[:, 0:L], in_=Sb[0][j][:, 0:L], func=AF.Copy, scale=ST[:, 38:39]),
                  reads=[("S", 0, j), (kST, "r")], writes=[kA])
            S.dve(lambda e, A=A, j=j, ST=ST, L=L: e.scalar_tensor_tensor(out=A[:, 0:L], in0=Sb[1][j][:, 0:L], scalar=ST[:, 40:41], in1=A[:, 0:L],
                                                                          op0=ALU.mult, op1=ALU.add),
                  reads=[("S", 1, j), (kST, "r2"), kA], writes=[kA])
            ATj = AT[j]
            kb = 0
            while kb < NKB:
                g = min(4, NKB - kb)
                pt = pT[ipt % 2]
                kpt = ("pT", ipt % 2)
                for u in range(g):
                    S.pe(lambda e, pt=pt, A=A, kb=kb, u=u: e.transpose(out=pt[:, u, :], in_=A[:, (kb + u) * 128:(kb + u + 1) * 128], identity=ident[:]),
                         reads=[kA, "c_ident"], writes=[kpt])
                if ipt % 2 == 0:
                    S.act(lambda e, pt=pt, ATj=ATj, kb=kb, g=g: e.copy(out=ATj[:, kb:kb + g, :], in_=pt[:, 0:g, :]), reads=[kpt], writes=[("AT", j, kb)])
                else:
                    S.dve(lambda e, pt=pt, ATj=ATj, kb=kb, g=g: e.tensor_copy(out=ATj[:, kb:kb + g, :], in_=pt[:, 0:g, :]), reads=[kpt], writes=[("AT", j, kb)])
                ipt += 1
                kb += g
            po = pO[it % 2]
            kpo = ("pO", it % 2)
            for kb in range(NKB):
                S.pe(lambda e, po=po, ATj=ATj, hj=hj, kb=kb, NKB=NKB: e.matmul(po[:, 0:DV], lhsT=ATj[:, kb, :], rhs=vt[hj][:, kb, :],
                                                                               start=(kb == 0), stop=(kb == NKB - 1)),
                     reads=[("AT", j, (kb // 4) * 4), ("vt", hj)], writes=[kpo])
            S.act(lambda e, po=po, ST=ST: e.activation(out=junk[:], in_=po[:, 0:DV], func=AF.Square, accum_out=ST[:, 42:43]),
                  reads=[kpo], writes=[(kST, "ss"), "junk"])
            S.dve(lambda e, ST=ST: e.tensor_scalar(out=ST[:, 43:44], in0=ST[:, 42:43], scalar1=float(1.0 / DV), scalar2=float(eps), op0=ALU.mult, op1=ALU.add),
                  reads=[(kST, "ss")], writes=[(kST, "ms")])
            S.act(lambda e, ST=ST: e.activation(out=ST[:, 44:45], in_=ST[:, 43:44], func=AF.Sqrt), reads=[(kST, "ms")], writes=[(kST, "sq")])
            S.dve(lambda e, ST=ST: e.reciprocal(out=ST[:, 45:46], in_=ST[:, 44:45]), reads=[(kST, "sq")], writes=[(kST, "rs")])
            o = ob[it % 4]
            S.dve(lambda e, o=o, po=po, ST=ST: e.scalar_tensor_tensor(out=o[:], in0=po[:, 0:DV], scalar=ST[:, 45:46], in1=gt[:], op0=ALU.mult, op1=ALU.mult),
                  reads=[kpo, (kST, "rs"), "gt"], writes=[("ob", it % 4)])
            outs.append(S.dma("sp", lambda e, o=o, qs=qs, h=h: e.dma_start(out=o_d[qs, h, :], in_=o[:]), reads=[("ob", it % 4)]))
            it += 1
    S.emit(final_wait_ops=outs[-6:])
    return nc

NEGBIG = -1.0e30


def build_dsa(NSLOT, SB, NH=10, NG=2, NIH=32, KSEL=256):
    nc = bass.Bass("TRN2", target_bir_lowering=False)
    DH, DI = 128, 64
    TK = NSLOT * SB * 128
    TQ = NSLOT * 128
    NKBT = TK // 128
    R = NH // NG
    NPR = NIH // 2
    NRND = KSEL // 8
    scale = float(DH ** -0.5)
    sq_d = nc.dram_tensor("sqT", [NH, DH, TQ], BF16, kind="ExternalInput").ap()
    sk_d = nc.dram_tensor("skT", [NG, DH, TK], BF16, kind="ExternalInput").ap()
    sv_d = nc.dram_tensor("sv", [NG, 128, NKBT, DH], BF16, kind="ExternalInput").ap()
    iq_d = nc.dram_tensor("iqT", [NPR, 128, TQ], BF16, kind="ExternalInput").ap()
    ik_d = nc.dram_tensor("ikT2", [128, TK], BF16, kind="ExternalInput").ap()
    iw_d = nc.dram_tensor("iw", [TQ, NIH], BF16, kind="ExternalInput").ap()
    msk_d = nc.dram_tensor("msk", [NSLOT, 128, SB * 128], F32, kind="ExternalInput").ap()
    o_d = nc.dram_tensor("o", [TQ, NH, DH], BF16, kind="ExternalOutput").ap()
    S = Sched(nc)
    ident = _softmax_pv_consts(S)
    sk = S.sb([128, NG, TK], BF16, "sk_sb")
    sv = S.sb([128, NG, NKBT, DH], BF16, "sv_sb")
    ik = S.sb([128, TK], BF16, "ik_sb")
    iwb = S.sb([128, NSLOT, NIH], BF16, "iwb")
    iw = S.sb([128, NSLOT, NIH], F32, "iw32")
    mk_ = S.sb([128, NSLOT, SB * 128], F32, "mk_sb")
    for g in range(NG):
        S.dma("sp", lambda e, g=g: e.dma_start(out=sk[:, g, :], in_=sk_d[g]), writes=[("sk", g)])
        S.dma("act", lambda e, g=g: e.dma_start(out=sv[:, g, :, :], in_=sv_d[g]), writes=[("sv", g)])
    S.dma("sp", lambda e: e.dma_start(out=ik[:], in_=ik_d), writes=["ik"])
    S.dma("sp", lambda e: e.dma_start(out=iwb[:], in_=iw_d.rearrange("(s p) h -> p s h", p=128)), writes=["iwb"])
    S.dve(lambda e: e.tensor_copy(out=iw[:], in_=iwb[:]), reads=["iwb"], writes=["iw"])
    S.dma("sp", lambda e: e.dma_start(out=mk_[:], in_=msk_d.rearrange("s p k -> p s k")), writes=["mk"])
    iq = [S.sb([128, NPR, 128], BF16, f"iq{i}") for i in range(2)]
    sq = [S.sb([128, NH, 128], BF16, f"sq{i}") for i in range(2)]
    score = S.sb([128, TK], F32, "score")
    Wk = S.sb([128, TK], F32, "Wk")
    sel = S.sb([128, TK], BF16, "sel")
    Rb = [S.sb([128, 512], F32, f"Rb{i}") for i in range(4)]
    Sb = [S.sb([128, TK], F32, f"S{i}") for i in range(2)]
    Pb = [S.sb([128, TK], BF16, f"P{i}") for i in range(2)]
    AT = [S.sb([128, NKBT, 128], BF16, f"AT{i}") for i in range(2)]
    st = [S.sb([128, 32], F32, f"stt{i}") for i in range(2)]
    m8 = S.sb([128, 8], F32, "m8")
    thr = S.sb([128, 1], F32, "thr")
    ob = [S.sb([128, DH], BF16, f"ob{i}") for i in range(4)]
    pS = [S.ps([128, 512], F32, f"pS{i}") for i in range(4)]
    pT = [S.ps([128, 8, 128], BF16, f"pT{i}") for i in range(2)]
    pO = [S.ps([128, 512], F32, f"pO{i}") for i in range(2)]
    outs = []
    ips = ir = ipt = it = 0
    for k in range(NSLOT):
        kj = k % 2
        L = SB * (k + 1) * 128
        NKB = L // 128
        L0 = L - SB * 128
        qs = slice(k * 128, (k + 1) * 128)
        S.dma("sp", lambda e, kj=kj, qs=qs: e.dma_start(out=iq[kj][:], in_=iq_d[:, :, qs].rearrange("n p q -> p n q")), writes=[("iq", kj)])
        S.dma("sp", lambda e, kj=kj, qs=qs: e.dma_start(out=sq[kj][:], in_=sq_d[:, :, qs].rearrange("n p q -> p n q")), writes=[("sq", kj)])
        chunks = []
        col = 0
        while col < L:
            w = min(512, L - col)
            chunks.append((col, w))
            col += w
        for hh in range(NIH):
            pr, mem = hh // 2, hh % 2
            ps_ = slice(64 * mem, 64 * mem + 64)
            for (col, w) in chunks:
                p = pS[ips % 4]
                kp = ("pS", ips % 4)
                rb = Rb[ir % 4]
                krb = ("Rb", ir % 4)
                S.pe(lambda e, p=p, kj=kj, pr=pr, ps_=ps_, col=col, w=w: e.matmul(p[:, 0:w], lhsT=iq[kj][ps_, pr, :], rhs=ik[ps_, col:col + w],
                                                                                  start=True, stop=True),
                     reads=[("iq", kj), "ik"], writes=[kp])
                S.act(lambda e, p=p, rb=rb, w=w: e.activation(out=rb[:, 0:w], in_=p[:, 0:w], func=AF.Relu), reads=[kp], writes=[krb])
                wcol = iw[:, k, hh:hh + 1]
                if hh == 0:
                    S.dve(lambda e, rb=rb, col=col, w=w, wcol=wcol: e.tensor_scalar(out=score[:, col:col + w], in0=rb[:, 0:w], scalar1=wcol, scalar2=None,
                                                                                    op0=ALU.mult), reads=[krb, "iw"], writes=[("score", col)])
                else:
                    S.dve(lambda e, rb=rb, col=col, w=w, wcol=wcol: e.scalar_tensor_tensor(out=score[:, col:col + w], in0=rb[:, 0:w], scalar=wcol,
                                                                                           in1=score[:, col:col + w], op0=ALU.mult, op1=ALU.add),
                          reads=[krb, "iw", ("score", col)], writes=[("score", col)])
                ips += 1
                ir += 1
        allsc = [("score", c_) for (c_, w_) in chunks]
        S.dve(lambda e, k=k, L=L, L0=L0: e.tensor_tensor(out=score[:, L0:L], in0=score[:, L0:L], in1=mk_[:, k, :], op=ALU.add),
              reads=allsc + ["mk"], writes=allsc)
        S.act(lambda e, L=L: e.copy(out=Wk[:, 0:L], in_=score[:, 0:L]), reads=allsc, writes=["Wk"])
        for r in range(NRND):
            S.dve(lambda e, L=L: e.max(out=m8[:], in_=Wk[:, 0:L]), reads=["Wk"], writes=["m8"])
            if r < NRND - 1:
                S.dve(lambda e, L=L: e.match_replace(out=Wk[:, 0:L], in_to_replace=m8[:], in_values=Wk[:, 0:L], imm_value=NEGBIG),
                      reads=["Wk", "m8"], writes=["Wk"])
        S.dve(lambda e: e.tensor_scalar(out=thr[:], in0=m8[:, 7:8], scalar1=-1.0e29, scalar2=None, op0=ALU.max), reads=["m8"], writes=["thr"])
        S.dve(lambda e, L=L: e.tensor_scalar(out=sel[:, 0:L], in0=score[:, 0:L], scalar1=thr[:, 0:1], scalar2=None, op0=ALU.is_ge),
              reads=allsc + ["thr"], writes=["sel"])
        for h in range(NH):
            g = h // R
            j = it % 2
            ST = st[j]
            kST = ("st", j)
            Sc, P = Sb[j], Pb[j]
            kS, kP = ("S", j), ("P", j)
            for ci, (col, w) in enumerate(chunks):
                p = pS[ips % 4]
                kp = ("pS", ips % 4)
                S.pe(lambda e, p=p, kj=kj, h=h, g=g, col=col, w=w: e.matmul(p[:, 0:w], lhsT=sq[kj][:, h, :], rhs=sk[:, g, col:col + w], start=True, stop=True),
                     reads=[("sq", kj), ("sk", g)], writes=[kp])
                S.dve(lambda e, p=p, Sc=Sc, col=col, w=w, ST=ST, ci=ci: e.tensor_scalar(out=Sc[:, col:col + w], in0=p[:, 0:w], scalar1=1.0, scalar2=-3.0e38,
                                                                                        op0=ALU.mult, op1=ALU.max, accum_out=ST[:, ci:ci + 1]),
                      reads=[kp], writes=[kS, (kST, ci)])
                ips += 1
            nch = len(chunks)
            S.dve(lambda e, ST=ST, nch=nch: e.reduce_max(out=ST[:, 16:17], in_=ST[:, 0:nch], axis=AX.X), reads=[(kST, i_) for i_ in range(nch)], writes=[(kST, "m")])
            S.dve(lambda e, ST=ST: e.tensor_scalar(out=ST[:, 17:18], in0=ST[:, 16:17], scalar1=-scale, scalar2=None, op0=ALU.mult), reads=[(kST, "m")], writes=[(kST, "nmb")])
            S.act(lambda e, Sc=Sc, P=P, ST=ST, L=L: e.activation(out=P[:, 0:L], in_=Sc[:, 0:L], func=AF.Exp, bias=ST[:, 17:18], scale=scale),
                  reads=[kS, (kST, "nmb")], writes=[kP])
            S.dve(lambda e, P=P, ST=ST, L=L: e.scalar_tensor_tensor(out=P[:, 0:L], in0=P[:, 0:L], scalar=1.0, in1=sel[:, 0:L], op0=ALU.mult, op1=ALU.mult,
                                                                    accum_out=ST[:, 18:19]), reads=[kP, "sel"], writes=[kP, (kST, "l")])
            S.dve(lambda e, ST=ST: e.reciprocal(out=ST[:, 19:20], in_=ST[:, 18:19]), reads=[(kST, "l")], writes=[(kST, "r")])
            ATj = AT[j]
            kb = 0
            while kb < NKB:
                gsz = min(4, NKB - kb)
                pt = pT[ipt % 2]
                kpt = ("pT", ipt % 2)
                for u in range(gsz):
                    S.pe(lambda e, pt=pt, P=P, kb=kb, u=u: e.transpose(out=pt[:, u, :], in_=P[:, (kb + u) * 128:(kb + u + 1) * 128], identity=ident[:]),
                         reads=[kP, "c_ident"], writes=[kpt])
                S.act(lambda e, pt=pt, ATj=ATj, kb=kb, gsz=gsz: e.copy(out=ATj[:, kb:kb + gsz, :], in_=pt[:, 0:gsz, :]), reads=[kpt], writes=[("AT", j, kb)])
                ipt += 1
                kb += gsz
            po = pO[it % 2]
            kpo = ("pO", it % 2)
            for kb in range(NKB):
                S.pe(lambda e, po=po, ATj=ATj, g=g, kb=kb, NKB=NKB: e.matmul(po[:, 0:DH], lhsT=ATj[:, kb, :], rhs=sv[:, g, kb, :],
                                                                             start=(kb == 0), stop=(kb == NKB - 1)),
                     reads=[("AT", j, (kb // 4) * 4), ("sv", g)], writes=[kpo])
            o = ob[it % 4]
            S.dve(lambda e, o=o, po=po, ST=ST: e.tensor_scalar(out=o[:], in0=po[:, 0:DH], scalar1=ST[:, 19:20], scalar2=None, op0=ALU.mult),
                  reads=[kpo, (kST, "r")], writes=[("ob", it % 4)])
            outs.append(S.dma("sp", lambda e, o=o, qs=qs, h=h: e.dma_start(out=o_d[qs, h, :], in_=o[:]), reads=[("ob", it % 4)]))
            it += 1
    S.emit(final_wait_ops=outs[-6:])
    return nc


def build_cast(Tc):
    nc = bass.Bass("TRN2", target_bir_lowering=False)
    D = 4096
    x = nc.dram_tensor("x", [Tc, D], F32, kind="ExternalInput").ap()
    xb = nc.dram_tensor("xb", [Tc, D], BF16, kind="ExternalOutput").ap()
    S = Sched(nc)
    xt = [S.sb([128, D], F32, f"xt{i}") for i in range(2)]
    bt = [S.sb([128, D], BF16, f"bt{i}") for i in range(2)]
    outs = []
    for t in range(Tc // 128):
        j = t % 2
        rs = slice(t * 128, (t + 1) * 128)
        S.dma("sp", lambda e, j=j, rs=rs: e.dma_start(out=xt[j][:], in_=x[rs, :]), writes=[("xt", j)])
        S.add(("dve", "pool")[j], lambda e, j=j: e.tensor_copy(out=bt[j][:], in_=xt[j][:]), reads=[("xt", j)], writes=[("bt", j)])
        outs.append(S.dma("sp", lambda e, j=j, rs=rs: e.dma_start(out=xb[rs, :], in_=bt[j][:]), reads=[("bt", j)]))
    S.emit(final_wait_ops=outs[-4:])
    return nc


import time as _time
import ml_dtypes as _mld
from concourse.bass_utils import run_bass_kernel_spmd

_BF = _mld.bfloat16
_NC_CACHE = {}
DEPTH = 2
LAMBDA_INIT = [0.8 - 0.6 * math.exp(-0.3 * l) for l in range(DEPTH)]
ALPHA = (2 * DEPTH) ** 0.25
_SPL = (768, 768, 1536, 1536, 16, 1280, 1280, 1280, 1280, 256, 256, 2048, 64, 32)
_OFF = [0]
for _s in _SPL:
    _OFF.append(_OFF[-1] + _s)
(O_GQ, O_GK, O_GV, O_GR, O_GLR, O_DQ, O_DK, O_DV, O_SQ, O_SK, O_SV, O_IQ, O_IK, O_IW, O_END) = _OFF
_LOG = []


def _get(key, fn):
    if key not in _NC_CACHE:
        _NC_CACHE[key] = fn()
    return _NC_CACHE[key]


def _run(name, nc, in_maps):
    t0 = _time.time()
    res = run_bass_kernel_spmd(nc, in_maps, core_ids=list(range(8)))
    _LOG.append((name, _time.time() - t0))
    print(f"[mk] launch {name}: {_time.time() - t0:.1f}s", flush=True)
    return res.results


def _c(a):
    return np.ascontiguousarray(a)


def _slot_blocks(r):
    return [4 * k + (r if k % 2 == 0 else 3 - r) for k in range(8)]


def _masks(r, neg):
    m = np.zeros((8, 128, 512), np.float32)
    for k, gq in enumerate(_slot_blocks(r)):
        keys = 4 * k * 128 + np.arange(512)
        qpos = gq * 128 + np.arange(128)
        m[k] = np.where(keys[None, :] <= qpos[:, None], 0.0, neg)
    return m


def _forward(inputs, dbg=None):
    x = np.asarray(inputs["x"], np.float32).reshape(8192, 4096)
    pos = np.asarray(inputs["positions"], np.int32).reshape(8192, 1)
    invf = (1.0 / (np.float32(500000.0) ** (np.arange(16, dtype=np.float32) * np.float32(2.0) / np.float32(32)))).astype(np.float32)[None]
    xb_T = None
    for l in range(DEPTH):
        w_in = np.asarray(inputs["w_in"][l], np.float32)
        Wp = np.zeros((4096, 8 * 1664), np.float32)
        Wp[:, :12400] = w_in
        if l == 0:
            ncc = _get(("cast",), lambda: build_cast(1024))
            res = _run("cast_x", ncc, [{"x": _c(x[c * 1024:(c + 1) * 1024])} for c in range(8)])
            xb_T = _c(np.concatenate([res[c]["xb"] for c in range(8)], axis=0).T)
            del res
        xT = xb_T
        nc = _get(("lin", 1664, False), lambda: build_lin(4096, 8192, 1664, False, BF16))
        res = _run(f"inproj{l}", nc, [{"xT": xT, "W": _c(Wp[:, c * 1664:(c + 1) * 1664])} for c in range(8)])
        hT = np.concatenate([res[c]["YT"] for c in range(8)], axis=0)[:12400]
        del Wp, res
        if dbg is not None:
            dbg[f"hT{l}"] = hT
        h16 = _c(np.concatenate([hT[O_DQ:O_DK], hT[O_DK:O_DV], hT[O_IQ:O_IK], hT[O_IK:O_IW]], axis=0).T)
        h32 = _c(np.concatenate([hT[O_SQ:O_SK], hT[O_SK:O_SV]], axis=0).T)
        nc = _get(("rope",), lambda: build_rope(1024))
        res = _run(f"rope{l}", nc, [{"pos": _c(pos[c * 1024:(c + 1) * 1024]), "invf": invf,
                                     "h16": _c(h16[c * 1024:(c + 1) * 1024]), "h32": _c(h32[c * 1024:(c + 1) * 1024])} for c in range(8)])
        r16 = np.concatenate([res[c]["o16"] for c in range(8)], axis=0)
        r32 = np.concatenate([res[c]["o32"] for c in range(8)], axis=0)
        dq, dk, iq, ik = r16[:, 0:1280], r16[:, 1280:2560], r16[:, 2560:4608], r16[:, 4608:4672]
        sq, sk = r32[:, 0:1280], r32[:, 1280:1536]
        del res, h16, h32
        if dbg is not None:
            dbg[f"dq{l}"] = dq; dbg[f"dk{l}"] = dk; dbg[f"iq{l}"] = iq; dbg[f"ik{l}"] = ik; dbg[f"sq{l}"] = sq; dbg[f"sk{l}"] = sk
        wgu = np.asarray(inputs["w_gate_up"][l], np.float32)
        bgt = np.asarray(inputs["b_gate"][l], np.float32)
        gng = np.asarray(inputs["gla_norm_g"][l], np.float32)[None]

        def chunked(a):
            return _c(a.reshape(64, 64, -1).transpose(1, 0, 2))
        maps = []
        for c in range(8):
            b, hd = c // 4, c % 4
            ts = slice(b * 4096, (b + 1) * 4096)
            maps.append({"qT": _c(hT[O_GQ + hd * 192:O_GQ + (hd + 1) * 192, ts]), "kT": _c(hT[O_GK + hd * 192:O_GK + (hd + 1) * 192, ts]),
                         "lrT": _c(hT[O_GLR:O_GLR + 16, ts]), "Wg": _c(wgu[:, hd * 192:(hd + 1) * 192]), "bg": _c(bgt[hd * 192:(hd + 1) * 192, None]),
                         "v": chunked(hT[O_GV + hd * 384:O_GV + (hd + 1) * 384, ts].T), "gr": chunked(hT[O_GR + hd * 384:O_GR + (hd + 1) * 384, ts].T),
                         "gn": gng})
        nc = _get(("gla",), lambda: build_gla(8))
        res = _run(f"gla{l}", nc, maps)
        cat = np.zeros((8192, 4096), _BF)
        for c in range(8):
            b, hd = c // 4, c % 4
            cat[b * 4096:(b + 1) * 4096, hd * 384:(hd + 1) * 384] = res[c]["o"].transpose(1, 0, 2).reshape(4096, 384)
        del res, maps
        lamv = np.stack([np.asarray(inputs[n][l], np.float32) for n in ("lambda_q1", "lambda_k1", "lambda_q2", "lambda_k2")])
        dng = np.asarray(inputs["diff_norm_g"][l], np.float32)[None]
        dmaps, smaps, qtoks = [], [], []
        for c in range(8):
            b, r = c // 4, c % 4
            ts = slice(b * 4096, (b + 1) * 4096)
            qtok = np.concatenate([np.arange(g * 128, (g + 1) * 128) for g in _slot_blocks(r)])
            qtoks.append(qtok)
            dq_b, dk_b = dq[ts], dk[ts]
            dv_b = hT[O_DV:O_SQ, ts].T
            dmaps.append({"qT": _c(dq_b[qtok].reshape(1024, 10, 128).transpose(1, 2, 0)),
                          "kT": _c(dk_b.reshape(4096, 10, 128).transpose(1, 2, 0)),
                          "v": _c(dv_b.reshape(32, 128, 10, 128).transpose(2, 1, 0, 3)),
                          "msk": _masks(r, -1.0e9), "lamv": lamv, "g": dng})
            sq_b, sk_b, iq_b, ik_b = sq[ts], sk[ts], iq[ts], ik[ts]
            sv_b = hT[O_SV:O_IQ, ts].T
            iw_b = hT[O_IW:O_END, ts].T
            ikT = ik_b.T
            smaps.append({"sqT": _c(sq_b[qtok].reshape(1024, 10, 128).transpose(1, 2, 0)),
                          "skT": _c(sk_b.reshape(4096, 2, 128).transpose(1, 2, 0)),
                          "sv": _c(sv_b.reshape(32, 128, 2, 128).transpose(2, 1, 0, 3)),
                          "iqT": _c(iq_b[qtok].reshape(1024, 16, 2, 64).transpose(1, 2, 3, 0).reshape(16, 128, 1024)),
                          "ikT2": _c(np.concatenate([ikT, ikT], axis=0)),
                          "iw": _c(iw_b[qtok]), "msk": _masks(r, -1.0e30)})
        nc = _get(("diff", l), lambda: build_diff(8, 4, 10, LAMBDA_INIT[l]))
        res = _run(f"diff{l}", nc, dmaps)
        for c in range(8):
            b = c // 4
            cat[b * 4096 + qtoks[c], 1536:2816] = res[c]["o"].reshape(1024, 1280)
        nc = _get(("dsa",), lambda: build_dsa(8, 4))
        res = _run(f"dsa{l}", nc, smaps)
        for c in range(8):
            b = c // 4
            cat[b * 4096 + qtoks[c], 2816:4096] = res[c]["o"].reshape(1024, 1280)
        del res, dmaps, smaps, hT
        if dbg is not None:
            dbg[f"cat{l}"] = cat
        w_out = np.asarray(inputs["w_out"][l], np.float32)
        catT = _c(cat.T)
        nc = _get(("lin", 512, False), lambda: build_lin(4096, 8192, 512, False, BF16))
        res = _run(f"outproj{l}", nc, [{"xT": catT, "W": _c(w_out[:, c * 512:(c + 1) * 512])} for c in range(8)])
        mix = _c(np.concatenate([res[c]["YT"] for c in range(8)], axis=0).T)
        del res, catT, cat
        if dbg is not None:
            dbg[f"mix{l}"] = mix
        nc = _get(("ln", 1), lambda: build_ln(1024, 1, True, ALPHA))
        g1 = np.asarray(inputs["ln1_g"][l], np.float32)[None]
        b1 = np.asarray(inputs["ln1_b"][l], np.float32)[None]
        res = _run(f"ln1_{l}", nc, [{"x": _c(x[c * 1024:(c + 1) * 1024]), "a0": _c(mix[c * 1024:(c + 1) * 1024]), "g": g1, "b": b1} for c in range(8)])
        x1 = np.concatenate([res[c]["y"] for c in range(8)], axis=0)
        x1b_T = _c(np.concatenate([res[c]["yb"] for c in range(8)], axis=0).T)
        del res, mix
        if dbg is not None:
            dbg[f"x1_{l}"] = x1
        if l % 2 == 0:
            j = l // 2
            w1, w3, w2 = (np.asarray(inputs[n][j], np.float32) for n in ("ffn_w1", "ffn_w3", "ffn_w2"))
            maps = []
            for c in range(8):
                fs = slice(c * 1376, (c + 1) * 1376)
                W1 = np.zeros((4096, 1408), np.float32); W1[:, :1376] = w1[:, fs]
                W3 = np.zeros((4096, 1408), np.float32); W3[:, :1376] = w3[:, fs]
                W2 = np.zeros((1408, 4096), np.float32); W2[:1376] = w2[fs]
                maps.append({"xT": x1b_T, "W1": W1, "W3": W3, "W2": W2})
            nc = _get(("ffn", 1408, False), lambda: build_ffn(8192, 1408, False))
            res = _run(f"ffn{l}", nc, maps)
        else:
            j = l // 2
            nc = _get(("router",), lambda: build_router(1024))
            x1T = _c(x1.T)
            rw = np.asarray(inputs["router_w"][j], np.float32)
            rb = np.asarray(inputs["router_b"][j], np.float32)[None]
            res = _run(f"router{l}", nc, [{"xT": _c(x1T[:, c * 1024:(c + 1) * 1024]), "Wr": rw, "rb": rb} for c in range(8)])
            gates = np.concatenate([res[c]["gates"] for c in range(8)], axis=0)
            if dbg is not None:
                dbg[f"gates{l}"] = gates
            del x1T
            maps = [{"xT": x1b_T, "W1": np.asarray(inputs["moe_w1"][j][e], np.float32), "W3": np.asarray(inputs["moe_w3"][j][e], np.float32),
                     "W2": np.asarray(inputs["moe_w2"][j][e], np.float32), "gate": _c(gates[:, e][None])} for e in range(8)]
            nc = _get(("ffn", 4096, True), lambda: build_ffn(8192, 4096, True))
            res = _run(f"moe{l}", nc, maps)
        parts = [_c(res[c]["yT"].T) for c in range(8)]
        del res, maps
        if dbg is not None:
            dbg[f"parts{l}"] = parts
        nc = _get(("ln", 8), lambda: build_ln(1024, 8, True, ALPHA))
        g2 = np.asarray(inputs["ln2_g"][l], np.float32)[None]
        b2 = np.asarray(inputs["ln2_b"][l], np.float32)[None]
        maps = []
        for c in range(8):
            m = {"x": _c(x1[c * 1024:(c + 1) * 1024]), "g": g2, "b": b2}
            for i in range(8):
                m[f"a{i}"] = _c(parts[i][c * 1024:(c + 1) * 1024])
            maps.append(m)
        res = _run(f"ln2_{l}", nc, maps)
        x = np.concatenate([res[c]["y"] for c in range(8)], axis=0)
        xb_T = _c(np.concatenate([res[c]["yb"] for c in range(8)], axis=0).T)
        del res, maps, parts
        if dbg is not None:
            dbg[f"x2_{l}"] = x
    return x.reshape(2, 4096, 4096).astype(np.float32)


def kernel(**inputs):
    return _forward(inputs)
```

```python
import contextlib
import numpy as np
import concourse.bass as bass
import concourse.mybir as mybir

F32 = mybir.dt.float32
BF16 = mybir.dt.bfloat16
I32 = mybir.dt.int32
U32 = mybir.dt.uint32
AF = mybir.ActivationFunctionType
ALU = mybir.AluOpType
AX = mybir.AxisListType

ENGS = ("pe", "act", "dve", "pool", "sp")
NDMASEM = 6


class Sched:
    def __init__(self, nc):
        self.nc = nc
        self.ops = []
        self.lastw = {}
        self.readers = {}
        self.es = contextlib.ExitStack()
        self._nt = 0

    def sb(self, shape, dt, name=None):
        self._nt += 1
        return self.es.enter_context(self.nc.sbuf_tensor(name or f"t{self._nt}", list(shape), dt))

    def ps(self, shape, dt=F32, name=None):
        self._nt += 1
        return self.es.enter_context(self.nc.psum_tensor(name or f"p{self._nt}", list(shape), dt))

    def add(self, eng, fn, reads=(), writes=(), dma=False):
        idx = len(self.ops)
        deps = set()
        for k in reads:
            w = self.lastw.get(k)
            if w is not None:
                deps.add(w)
        for k in writes:
            w = self.lastw.get(k)
            if w is not None:
                deps.add(w)
            for r in self.readers.get(k, {}).values():
                deps.add(r)
        deps.discard(idx)
        self.ops.append((eng, fn, sorted(deps), dma))
        for k in writes:
            self.lastw[k] = idx
            self.readers[k] = {}
        rk = ("dma", idx) if dma else eng
        for k in reads:
            self.readers.setdefault(k, {})[rk] = idx
        return idx

    def pe(self, fn, reads=(), writes=()):
        return self.add("pe", fn, reads, writes)

    def act(self, fn, reads=(), writes=()):
        return self.add("act", fn, reads, writes)

    def dve(self, fn, reads=(), writes=()):
        return self.add("dve", fn, reads, writes)

    def pool(self, fn, reads=(), writes=()):
        return self.add("pool", fn, reads, writes)

    def dma(self, q, fn, reads=(), writes=()):
        return self.add(q, fn, reads, writes, dma=True)

    def emit(self, final_wait_ops=()):
        nc = self.nc
        ops = self.ops
        n = len(ops)
        needed = [False] * n
        for (eng, fn, deps, dma) in ops:
            for d in deps:
                de, _, _, ddma = ops[d]
                if (not ddma) and de == "pe" and eng == "pe" and not dma:
                    continue
                needed[d] = True
        for d in final_wait_ops:
            needed[d] = True
        sem = {e: self.es.enter_context(nc.semaphore(f"s_{e}")) for e in ENGS if e != "sp"}
        dsem = {e: [self.es.enter_context(nc.semaphore(f"d_{e}{i}")) for i in range(NDMASEM)]
                for e in ENGS if e != "pe"}
        sig = [None] * n
        cnt = {e: 0 for e in ENGS}
        dcnt = {e: 0 for e in ENGS}
        dma_prev = {}
        dq = {e: [] for e in ENGS}
        for i, (eng, fn, deps, dma) in enumerate(ops):
            if dma:
                k = dcnt[eng]
                dcnt[eng] += 1
                sig[i] = (dsem[eng][k % NDMASEM], 16 * (k // NDMASEM + 1))
                if k >= NDMASEM:
                    dma_prev[i] = dq[eng][k - NDMASEM]
                dq[eng].append(i)
            elif needed[i]:
                cnt[eng] += 1
                sig[i] = (sem[eng], cnt[eng])
        per = {e: [] for e in ENGS}
        for i, o in enumerate(ops):
            per[o[0]].append(i)

        def run(engname, e):
            waited = {}
            for i in per[engname]:
                _, fn, deps, dma = ops[i]
                dl = list(deps)
                if dma and i in dma_prev:
                    dl.append(dma_prev[i])
                for d in dl:
                    de, _, _, ddma = ops[d]
                    if (not ddma) and de == "pe" and engname == "pe" and not dma:
                        continue
                    s, v = sig[d]
                    key = id(s)
                    if waited.get(key, 0) >= v:
                        continue
                    e.wait_ge(s, v)
                    waited[key] = v
                ins = fn(e)
                if dma:
                    s, v = sig[i]
                    ins.then_inc(s, 16)
                elif needed[i]:
                    s, v = sig[i]
                    ins.then_inc(s, 1)
            if engname == self.final_eng:
                for d in final_wait_ops:
                    s, v = sig[d]
                    e.wait_ge(s, v)

        self.final_eng = "sp"
        with nc.Block() as block:
            if per["sp"] or final_wait_ops:
                @block.sync
                def _(e):
                    run("sp", e)
            if per["pe"]:
                @block.tensor
                def _(e):
                    run("pe", e)
            if per["act"]:
                @block.scalar
                def _(e):
                    run("act", e)
            if per["dve"]:
                @block.vector
                def _(e):
                    run("dve", e)
            if per["pool"]:
                @block.gpsimd
                def _(e):
                    run("pool", e)
        self.es.close()


def _cast_eng(S, i):
    return ("dve", "pool")[i % 2]


def build_lin(K, T, Nc, x_f32, out_dt):
    nc = bass.Bass("TRN2", target_bir_lowering=False)
    KC, NT, TB = K // 128, Nc // 128, 512
    NB = T // TB
    xT = nc.dram_tensor("xT", [K, T], F32 if x_f32 else BF16, kind="ExternalInput").ap()
    W = nc.dram_tensor("W", [K, Nc], F32, kind="ExternalInput").ap()
    YT = nc.dram_tensor("YT", [Nc, T], out_dt, kind="ExternalOutput").ap()
    S = Sched(nc)
    Wb = S.sb([128, KC, Nc], BF16, "Wb")
    stg = [S.sb([128, Nc], F32, f"stg{i}") for i in range(2)]
    xb = [S.sb([128, KC, TB], BF16, f"xb{i}") for i in range(2)]
    ob = [S.sb([128, TB], out_dt, f"ob{i}") for i in range(4)]
    pb = [S.ps([128, TB], F32, f"pb{i}") for i in range(8)]
    Wv = W.rearrange("(c p) n -> p c n", p=128)
    xv = xT.rearrange("(c p) t -> p c t", p=128)
    for kc in range(KC):
        s = stg[kc % 2]
        S.dma("sp", lambda e, s=s, kc=kc: e.dma_start(out=s[:], in_=Wv[:, kc, :]), writes=[("stg", kc % 2)])
        S.add(_cast_eng(S, kc), lambda e, s=s, kc=kc: e.tensor_copy(out=Wb[:, kc, :], in_=s[:]),
              reads=[("stg", kc % 2)], writes=[("Wb", kc)])
    outs = []
    it = 0
    for tb in range(NB):
        xq = "pool" if x_f32 else "sp"
        b = xb[tb % 2]
        S.dma(xq, lambda e, b=b, tb=tb: e.dma_start(out=b[:], in_=xv[:, :, tb * TB:(tb + 1) * TB]),
              writes=[("xb", tb % 2)])
        for nt in range(NT):
            p = pb[it % 8]
            o = ob[it % 4]
            for kc in range(KC):
                S.pe(lambda e, p=p, b=b, kc=kc, nt=nt: e.matmul(p[:], lhsT=Wb[:, kc, nt * 128:(nt + 1) * 128],
                                                               rhs=b[:, kc, :], start=(kc == 0), stop=(kc == KC - 1)),
                     reads=[("xb", tb % 2), ("Wb", kc)], writes=[("pb", it % 8)])
            if it % 2 == 0:
                S.dve(lambda e, p=p, o=o: e.tensor_copy(out=o[:], in_=p[:]), reads=[("pb", it % 8)], writes=[("ob", it % 4)])
            else:
                S.act(lambda e, p=p, o=o: e.copy(out=o[:], in_=p[:]), reads=[("pb", it % 8)], writes=[("ob", it % 4)])
            outs.append(S.dma("sp", lambda e, o=o, nt=nt, tb=tb: e.dma_start(
                out=YT[nt * 128:(nt + 1) * 128, tb * TB:(tb + 1) * TB], in_=o[:]), reads=[("ob", it % 4)]))
            it += 1
    S.emit(final_wait_ops=outs[-8:])
    return nc


def build_ffn(T, F, gated):
    nc = bass.Bass("TRN2", target_bir_lowering=False)
    D = 4096
    KC, FT, SUBW, NSUB, NO = D // 128, F // 128, 512, 2, D // 128
    TB = SUBW * NSUB
    NB = T // TB
    xT = nc.dram_tensor("xT", [D, T], BF16, kind="ExternalInput").ap()
    W1 = nc.dram_tensor("W1", [D, F], F32, kind="ExternalInput").ap()
    W3 = nc.dram_tensor("W3", [D, F], F32, kind="ExternalInput").ap()
    W2 = nc.dram_tensor("W2", [F, D], F32, kind="ExternalInput").ap()
    if gated:
        gate = nc.dram_tensor("gate", [1, T], F32, kind="ExternalInput").ap()
    yT = nc.dram_tensor("yT", [D, T], BF16, kind="ExternalOutput").ap()
    W1s = nc.dram_tensor("W1s", [FT, 128, KC * 128], BF16).ap()
    W3s = nc.dram_tensor("W3s", [FT, 128, KC * 128], BF16).ap()
    W2s = nc.dram_tensor("W2s", [NO, 128, FT * 128], BF16).ap()
    S = Sched(nc)
    HW = 2048 if max(F, D) > 2048 else max(F, D)
    stg = S.sb([128, HW], F32, "stg")
    stb = [S.sb([128, HW], BF16, f"stb{i}") for i in range(2)]
    xb = S.sb([128, KC, TB], BF16, "xb")
    h1 = S.sb([128, FT, TB], BF16, "h1")
    w1t = [S.sb([128, KC * 128], BF16, f"w1t{i}") for i in range(2)]
    w3t = [S.sb([128, KC * 128], BF16, f"w3t{i}") for i in range(2)]
    w2t = [S.sb([128, FT * 128], BF16, f"w2t{i}") for i in range(2)]
    sg = [S.sb([128, SUBW], F32, f"sg{i}") for i in range(2)]
    ob = [S.sb([128, SUBW], BF16, f"ob{i}") for i in range(4)]
    gb = S.sb([128, TB], F32, "gb") if gated else None
    pb = [S.ps([128, SUBW], F32, f"pb{i}") for i in range(8)]
    n = 0
    for (Wsrc, Wdst, rows, cols, cch) in ((W1, W1s, D, F, KC), (W3, W3s, D, F, KC), (W2, W2s, F, D, FT)):
        Wv = Wsrc.rearrange("(c p) n -> p c n", p=128)
        Dv = Wdst.rearrange("n p (c j) -> p c n j", c=cch)
        for c in range(rows // 128):
            for c0 in range(0, cols, HW):
                cw = min(HW, cols - c0)
                sb_ = stb[n % 2]
                S.dma("sp", lambda e, Wv=Wv, c=c, c0=c0, cw=cw: e.dma_start(out=stg[:, 0:cw], in_=Wv[:, c, c0:c0 + cw]), writes=["stg"])
                S.add(_cast_eng(S, n), lambda e, sb_=sb_, cw=cw: e.tensor_copy(out=sb_[:, 0:cw], in_=stg[:, 0:cw]),
                      reads=["stg"], writes=[("stb", n % 2)])
                S.dma("act", lambda e, sb_=sb_, Dv=Dv, c=c, c0=c0, cw=cw: e.dma_start(
                    out=Dv[:, c, c0 // 128:(c0 + cw) // 128, :], in_=sb_[:, 0:cw].rearrange("p (n j) -> p n j", j=128)),
                    reads=[("stb", n % 2)], writes=[(id(Wdst.tensor), "scr")])
                n += 1
    keyW1, keyW3, keyW2 = (id(W1s.tensor), "scr"), (id(W3s.tensor), "scr"), (id(W2s.tensor), "scr")
    xv = xT.rearrange("(c p) t -> p c t", p=128)
    outs = []
    ip = iw = io = i2 = isg = 0
    for tb in range(NB):
        t0 = tb * TB
        for kh in range(2):
            ks = slice(kh * KC // 2, (kh + 1) * KC // 2)
            S.dma("sp", lambda e, ks=ks, t0=t0: e.dma_start(out=xb[:, ks, :], in_=xv[:, ks, t0:t0 + TB]), writes=[("xb", kh)])
        if gated:
            S.dma("sp", lambda e, t0=t0: e.dma_start(out=gb[:], in_=gate[0, t0:t0 + TB].partition_broadcast(128)), writes=["gb"])
        for ft in range(FT):
            a1, a3 = w1t[iw % 2], w3t[iw % 2]
            S.dma("sp", lambda e, a1=a1, ft=ft: e.dma_start(out=a1[:], in_=W1s[ft]), reads=[keyW1], writes=[("w1t", iw % 2)])
            S.dma("sp", lambda e, a3=a3, ft=ft: e.dma_start(out=a3[:], in_=W3s[ft]), reads=[keyW3], writes=[("w3t", iw % 2)])
            for su in range(NSUB):
                cs = slice(su * SUBW, (su + 1) * SUBW)
                pa, pg = pb[ip % 8], pb[(ip + 1) % 8]
                for kc in range(KC):
                    S.pe(lambda e, pa=pa, a1=a1, kc=kc, cs=cs: e.matmul(pa[:], lhsT=a1[:, kc * 128:(kc + 1) * 128], rhs=xb[:, kc, cs],
                                                                       start=(kc == 0), stop=(kc == KC - 1)),
                         reads=[("w1t", iw % 2), ("xb", kc * 2 // KC)], writes=[("pb", ip % 8)])
                for kc in range(KC):
                    S.pe(lambda e, pg=pg, a3=a3, kc=kc, cs=cs: e.matmul(pg[:], lhsT=a3[:, kc * 128:(kc + 1) * 128], rhs=xb[:, kc, cs],
                                                                       start=(kc == 0), stop=(kc == KC - 1)),
                         reads=[("w3t", iw % 2), ("xb", kc * 2 // KC)], writes=[("pb", (ip + 1) % 8)])
                s_ = sg[isg % 2]
                S.act(lambda e, s_=s_, pa=pa: e.activation(out=s_[:], in_=pa[:], func=AF.Silu),
                      reads=[("pb", ip % 8)], writes=[("sg", isg % 2)])
                S.dve(lambda e, s_=s_, pg=pg, ft=ft, cs=cs: e.tensor_tensor(out=h1[:, ft, cs], in0=s_[:], in1=pg[:], op=ALU.mult),
                      reads=[("sg", isg % 2), ("pb", (ip + 1) % 8)], writes=[("h1", ft, su)])
                ip += 2
                isg += 1
            iw += 1
        for no in range(NO):
            a2 = w2t[i2 % 2]
            S.dma("sp", lambda e, a2=a2, no=no: e.dma_start(out=a2[:], in_=W2s[no]), reads=[keyW2], writes=[("w2t", i2 % 2)])
            for su in range(NSUB):
                cs = slice(su * SUBW, (su + 1) * SUBW)
                po = pb[ip % 8]
                for fc in range(FT):
                    S.pe(lambda e, po=po, a2=a2, fc=fc, cs=cs: e.matmul(po[:], lhsT=a2[:, fc * 128:(fc + 1) * 128], rhs=h1[:, fc, cs],
                                                                       start=(fc == 0), stop=(fc == FT - 1)),
                         reads=[("w2t", i2 % 2), ("h1", fc, su)], writes=[("pb", ip % 8)])
                o = ob[io % 4]
                if gated:
                    S.dve(lambda e, o=o, po=po, cs=cs: e.tensor_tensor(out=o[:], in0=po[:], in1=gb[:, cs], op=ALU.mult),
                          reads=[("pb", ip % 8), "gb"], writes=[("ob", io % 4)])
                elif io % 2 == 0:
                    S.dve(lambda e, o=o, po=po: e.tensor_copy(out=o[:], in_=po[:]), reads=[("pb", ip % 8)], writes=[("ob", io % 4)])
                else:
                    S.act(lambda e, o=o, po=po: e.copy(out=o[:], in_=po[:]), reads=[("pb", ip % 8)], writes=[("ob", io % 4)])
                outs.append(S.dma("sp", lambda e, o=o, no=no, t0=t0, su=su: e.dma_start(
                    out=yT[no * 128:(no + 1) * 128, t0 + su * SUBW:t0 + (su + 1) * SUBW], in_=o[:]), reads=[("ob", io % 4)]))
                ip += 1
                io += 1
            i2 += 1
    S.emit(final_wait_ops=outs[-8:])
    return nc


def build_ln(Tc, n_add, add_bf16, alpha, eps=1e-5):
    nc = bass.Bass("TRN2", target_bir_lowering=False)
    D = 4096
    NTL = Tc // 128
    adt = BF16 if add_bf16 else F32
    x = nc.dram_tensor("x", [Tc, D], F32, kind="ExternalInput").ap()
    adds = [nc.dram_tensor(f"a{i}", [Tc, D], adt, kind="ExternalInput").ap() for i in range(n_add)]
    g = nc.dram_tensor("g", [1, D], F32, kind="ExternalInput").ap()
    bb = nc.dram_tensor("b", [1, D], F32, kind="ExternalInput").ap()
    y = nc.dram_tensor("y", [Tc, D], F32, kind="ExternalOutput").ap()
    yb = nc.dram_tensor("yb", [Tc, D], BF16, kind="ExternalOutput").ap()
    S = Sched(nc)
    gt = S.sb([128, D], F32, "gt")
    bt = S.sb([128, D], F32, "bt")
    xt = [S.sb([128, D], F32, f"xt{i}") for i in range(2)]
    at = [S.sb([128, D], adt, f"at{i}") for i in range(2)]
    yt = [S.sb([128, D], F32, f"yt{i}") for i in range(2)]
    ybt = [S.sb([128, D], BF16, f"ybt{i}") for i in range(2)]
    st = [S.sb([128, 8, 6], F32, f"st{i}") for i in range(2)]
    mv = [S.sb([128, 8], F32, f"mv{i}") for i in range(2)]
    S.dma("sp", lambda e: e.dma_start(out=gt[:], in_=g[0, :].partition_broadcast(128)), writes=["gt"])
    S.dma("sp", lambda e: e.dma_start(out=bt[:], in_=bb[0, :].partition_broadcast(128)), writes=["bt"])
    outs = []
    ia = 0
    for t in range(NTL):
        j = t % 2
        X, Y, YB, ST, MV = xt[j], yt[j], ybt[j], st[j], mv[j]
        rs = slice(t * 128, (t + 1) * 128)
        S.dma("sp", lambda e, X=X, rs=rs: e.dma_start(out=X[:], in_=x[rs, :]), writes=[("xt", j)])
        for i in range(n_add):
            A = at[ia % 2]
            S.dma("act" if i % 2 else "sp", lambda e, A=A, rs=rs, i=i: e.dma_start(out=A[:], in_=adds[i][rs, :]), writes=[("at", ia % 2)])
            if i == 0:
                S.dve(lambda e, X=X, A=A: e.scalar_tensor_tensor(out=X[:], in0=X[:], scalar=float(alpha), in1=A[:],
                                                                 op0=ALU.mult, op1=ALU.add),
                      reads=[("at", ia % 2), ("xt", j)], writes=[("xt", j)])
            else:
                S.dve(lambda e, X=X, A=A: e.tensor_tensor(out=X[:], in0=X[:], in1=A[:], op=ALU.add),
                      reads=[("at", ia % 2), ("xt", j)], writes=[("xt", j)])
            ia += 1
        for c in range(8):
            S.dve(lambda e, X=X, ST=ST, c=c: e.bn_stats(out=ST[:, c, :], in_=X[:, c * 512:(c + 1) * 512]),
                  reads=[("xt", j)], writes=[("st", j, c)])
        S.dve(lambda e, ST=ST, MV=MV: e.bn_aggr(out=MV[:, 0:2], in_=ST[:].rearrange("p c s -> p (c s)")),
              reads=[("st", j, c) for c in range(8)], writes=[("mv", j)])
        S.dve(lambda e, MV=MV: e.tensor_scalar(out=MV[:, 2:3], in0=MV[:, 1:2], scalar1=float(eps), scalar2=None, op0=ALU.add),
              reads=[("mv", j)], writes=[("mv", j)])
        S.act(lambda e, MV=MV: e.activation(out=MV[:, 3:4], in_=MV[:, 2:3], func=AF.Sqrt), reads=[("mv", j)], writes=[("mv", j)])
        S.dve(lambda e, MV=MV: e.reciprocal(out=MV[:, 4:5], in_=MV[:, 3:4]), reads=[("mv", j)], writes=[("mv", j)])
        S.dve(lambda e, MV=MV: e.scalar_tensor_tensor(out=MV[:, 5:6], in0=MV[:, 0:1], scalar=-1.0, in1=MV[:, 4:5],
                                                      op0=ALU.mult, op1=ALU.mult), reads=[("mv", j)], writes=[("mv", j)])
        S.act(lambda e, X=X, Y=Y, MV=MV: e.activation(out=Y[:], in_=X[:], func=AF.Identity, bias=MV[:, 5:6], scale=MV[:, 4:5]),
              reads=[("xt", j), ("mv", j)], writes=[("yt", j)])
        S.dve(lambda e, Y=Y: e.tensor_tensor(out=Y[:], in0=Y[:], in1=gt[:], op=ALU.mult), reads=[("yt", j), "gt"], writes=[("yt", j)])
        S.dve(lambda e, Y=Y: e.tensor_tensor(out=Y[:], in0=Y[:], in1=bt[:], op=ALU.add), reads=[("yt", j), "bt"], writes=[("yt", j)])
        S.pool(lambda e, Y=Y, YB=YB: e.tensor_copy(out=YB[:], in_=Y[:]), reads=[("yt", j)], writes=[("ybt", j)])
        outs.append(S.dma("sp", lambda e, Y=Y, rs=rs: e.dma_start(out=y[rs, :], in_=Y[:]), reads=[("yt", j)]))
        outs.append(S.dma("sp", lambda e, YB=YB, rs=rs: e.dma_start(out=yb[rs, :], in_=YB[:]), reads=[("ybt", j)]))
    S.emit(final_wait_ops=outs[-6:])
    return nc


def build_router(Tc):
    nc = bass.Bass("TRN2", target_bir_lowering=False)
    D, E = 4096, 8
    KC = D // 128
    NTL = Tc // 128
    xT = nc.dram_tensor("xT", [D, Tc], F32, kind="ExternalInput").ap()
    Wr = nc.dram_tensor("Wr", [D, E], F32, kind="ExternalInput").ap()
    rb = nc.dram_tensor("rb", [1, E], F32, kind="ExternalInput").ap()
    gates = nc.dram_tensor("gates", [Tc, E], F32, kind="ExternalOutput").ap()
    S = Sched(nc)
    wt = S.sb([128, KC, E], F32, "wt")
    rbt = S.sb([128, E], F32, "rbt")
    xt = [S.sb([128, KC, 128], F32, f"xt{i}") for i in range(2)]
    wk = [S.sb([128, 48], F32, f"wk{i}") for i in range(2)]
    pp = [S.ps([128, E], F32, f"pp{i}") for i in range(2)]
    S.dma("sp", lambda e: e.dma_start(out=wt[:], in_=Wr.rearrange("(c p) n -> p c n", p=128)), writes=["wt"])
    S.dma("sp", lambda e: e.dma_start(out=rbt[:], in_=rb[0, :].partition_broadcast(128)), writes=["rbt"])
    xv = xT.rearrange("(c p) t -> p c t", p=128)
    outs = []
    for t in range(NTL):
        j = t % 2
        X, P, Wk = xt[j], pp[j], wk[j]
        S.dma("sp", lambda e, X=X, t=t: e.dma_start(out=X[:], in_=xv[:, :, t * 128:(t + 1) * 128]), writes=[("xt", j)])
        for kc in range(KC):
            S.pe(lambda e, X=X, P=P, kc=kc: e.matmul(P[:], lhsT=X[:, kc, :], rhs=wt[:, kc, :], start=(kc == 0), stop=(kc == KC - 1)),
                 reads=[("xt", j), "wt"], writes=[("pp", j)])
        lg, m8, nm1, msk, ex, ssum = Wk[:, 0:8], Wk[:, 8:16], Wk[:, 16:17], Wk[:, 24:32], Wk[:, 32:40], Wk[:, 17:18]
        K = ("wk", j)
        S.dve(lambda e, P=P, lg=lg: e.tensor_tensor(out=lg, in0=P[:], in1=rbt[:], op=ALU.add), reads=[("pp", j), "rbt"], writes=[K])
        S.dve(lambda e, lg=lg, m8=m8: e.max(out=m8, in_=lg), reads=[K], writes=[K])
        S.dve(lambda e, m8=m8, nm1=nm1: e.tensor_scalar(out=nm1, in0=m8[:, 0:1], scalar1=-1.0, scalar2=None, op0=ALU.mult), reads=[K], writes=[K])
        S.dve(lambda e, lg=lg, m8=m8, msk=msk: e.tensor_scalar(out=msk, in0=lg, scalar1=m8[:, 1:2], scalar2=None, op0=ALU.is_ge),
              reads=[K], writes=[K])
        S.act(lambda e, lg=lg, ex=ex, nm1=nm1: e.activation(out=ex, in_=lg, func=AF.Exp, bias=nm1, scale=1.0), reads=[K], writes=[K])
        S.dve(lambda e, ex=ex, msk=msk: e.tensor_tensor(out=ex, in0=ex, in1=msk, op=ALU.mult), reads=[K], writes=[K])
        S.dve(lambda e, ex=ex, ssum=ssum: e.reduce_sum(out=ssum, in_=ex, axis=AX.X), reads=[K], writes=[K])
        S.dve(lambda e, ssum=ssum: e.reciprocal(out=ssum, in_=ssum), reads=[K], writes=[K])
        S.dve(lambda e, ex=ex, ssum=ssum: e.tensor_scalar(out=ex, in0=ex, scalar1=ssum, scalar2=None, op0=ALU.mult), reads=[K], writes=[K])
        outs.append(S.dma("sp", lambda e, ex=ex, t=t: e.dma_start(out=gates[t * 128:(t + 1) * 128, :], in_=ex), reads=[K]))
    S.emit(final_wait_ops=outs[-4:])
    return nc

import math
C1_2PI = 6.28125
C2_2PI = 2.0 * math.pi - 6.28125
MAGIC = 12582912.0


def build_rope(Tc):
    nc = bass.Bass("TRN2", target_bir_lowering=False)
    N16, N32 = 73, 12
    NTL = Tc // 128
    pos = nc.dram_tensor("pos", [Tc, 1], I32, kind="ExternalInput").ap()
    invf = nc.dram_tensor("invf", [1, 16], F32, kind="ExternalInput").ap()
    h16 = nc.dram_tensor("h16", [Tc, N16 * 64], BF16, kind="ExternalInput").ap()
    h32 = nc.dram_tensor("h32", [Tc, N32 * 128], BF16, kind="ExternalInput").ap()
    o16 = nc.dram_tensor("o16", [Tc, N16 * 64], BF16, kind="ExternalOutput").ap()
    o32 = nc.dram_tensor("o32", [Tc, N32 * 128], BF16, kind="ExternalOutput").ap()
    S = Sched(nc)
    ivt = S.sb([128, 16], F32, "ivt")
    S.dma("sp", lambda e: e.dma_start(out=ivt[:], in_=invf[0, :].partition_broadcast(128)), writes=["ivt"])
    A = [S.sb([128, N16 * 64], BF16, f"A{i}") for i in range(2)]
    Bt = [S.sb([128, N32 * 128], BF16, f"B{i}") for i in range(2)]
    pt = [S.sb([128, 1], I32, f"pt{i}") for i in range(2)]
    wk = [S.sb([128, 8, 16], F32, f"wk{i}") for i in range(2)]
    tmp = [[S.sb([128, N16 * 8], F32, f"tmp{i}_{k}") for k in range(4)] for i in range(2)]
    outs = []
    for t in range(NTL):
        j = t % 2
        rs = slice(t * 128, (t + 1) * 128)
        a, b, p, w, tm = A[j], Bt[j], pt[j], wk[j], tmp[j]
        S.dma("sp", lambda e, p=p, rs=rs: e.dma_start(out=p[:], in_=pos[rs, :]), writes=[("pt", j)])
        S.dma("sp", lambda e, a=a, rs=rs: e.dma_start(out=a[:], in_=h16[rs, :]), writes=[("A", j)])
        S.dma("act", lambda e, b=b, rs=rs: e.dma_start(out=b[:], in_=h32[rs, :]), writes=[("B", j)])
        K = ("wk", j)
        pf, ang, u, n_, r, ab, sn, cs = w[:, 0, 0:1], w[:, 1, :], w[:, 2, :], w[:, 3, :], w[:, 4, :], w[:, 5, :], w[:, 6, :], w[:, 7, :]
        S.dve(lambda e, pf=pf, p=p: e.tensor_copy(out=pf, in_=p[:]), reads=[("pt", j)], writes=[K])
        S.dve(lambda e, ang=ang, pf=pf: e.tensor_scalar(out=ang, in0=ivt[:], scalar1=pf, scalar2=None, op0=ALU.mult), reads=[K, "ivt"], writes=[K])
        S.dve(lambda e, u=u, ang=ang: e.tensor_scalar(out=u, in0=ang, scalar1=float(1.0 / (2 * math.pi)), scalar2=MAGIC, op0=ALU.mult, op1=ALU.add), reads=[K], writes=[K])
        S.dve(lambda e, u=u, n_=n_: e.tensor_scalar(out=n_, in0=u, scalar1=-MAGIC, scalar2=None, op0=ALU.add), reads=[K], writes=[K])
        S.dve(lambda e, r=r, n_=n_, ang=ang: e.scalar_tensor_tensor(out=r, in0=n_, scalar=-C1_2PI, in1=ang, op0=ALU.mult, op1=ALU.add), reads=[K], writes=[K])
        S.dve(lambda e, r=r, n_=n_: e.scalar_tensor_tensor(out=r, in0=n_, scalar=-C2_2PI, in1=r, op0=ALU.mult, op1=ALU.add), reads=[K], writes=[K])
        S.dve(lambda e, r=r: e.tensor_scalar(out=r, in0=r, scalar1=float(math.pi), scalar2=float(-math.pi), op0=ALU.min, op1=ALU.max), reads=[K], writes=[K])
        S.dve(lambda e, r=r, ab=ab: e.scalar_tensor_tensor(out=ab, in0=r, scalar=-1.0, in1=r, op0=ALU.mult, op1=ALU.max), reads=[K], writes=[K])
        S.dve(lambda e, ab=ab: e.tensor_scalar(out=ab, in0=ab, scalar1=-1.0, scalar2=float(math.pi / 2), op0=ALU.mult, op1=ALU.add), reads=[K], writes=[K])
        S.act(lambda e, sn=sn, r=r: e.activation(out=sn, in_=r, func=AF.Sin), reads=[K], writes=[K])
        S.act(lambda e, cs=cs, ab=ab: e.activation(out=cs, in_=ab, func=AF.Sin), reads=[K], writes=[K])
        for (buf, key, nh, hd, half, step) in ((a, ("A", j), N16, 64, 8, 2), (b, ("B", j), N32, 128, 16, 1)):
            hv = buf[:].rearrange("p (n d) -> p n d", d=hd)
            x1, x2 = hv[:, :, 0:half], hv[:, :, half:2 * half]
            cb = w[:, 7, 0:16:step].unsqueeze(1).broadcast_to([128, nh, half])
            sb_ = w[:, 6, 0:16:step].unsqueeze(1).broadcast_to([128, nh, half])
            T = [tm[k][:, 0:nh * half].rearrange("p (n d) -> p n d", d=half) for k in range(4)]
            TK = [("tmp", j, k) for k in range(4)]
            S.dve(lambda e, T=T, x1=x1, cb=cb: e.tensor_tensor(out=T[0], in0=x1, in1=cb, op=ALU.mult), reads=[key, K], writes=[TK[0]])
            S.dve(lambda e, T=T, x2=x2, sb_=sb_: e.tensor_tensor(out=T[1], in0=x2, in1=sb_, op=ALU.mult), reads=[key, K], writes=[TK[1]])
            S.dve(lambda e, T=T, x2=x2, cb=cb: e.tensor_tensor(out=T[2], in0=x2, in1=cb, op=ALU.mult), reads=[key, K], writes=[TK[2]])
            S.dve(lambda e, T=T, x1=x1, sb_=sb_: e.tensor_tensor(out=T[3], in0=x1, in1=sb_, op=ALU.mult), reads=[key, K], writes=[TK[3]])
            S.dve(lambda e, T=T, x1=x1: e.tensor_tensor(out=x1, in0=T[0], in1=T[1], op=ALU.subtract), reads=[TK[0], TK[1]], writes=[key])
            S.dve(lambda e, T=T, x2=x2: e.tensor_tensor(out=x2, in0=T[2], in1=T[3], op=ALU.add), reads=[TK[2], TK[3]], writes=[key])
        outs.append(S.dma("sp", lambda e, a=a, rs=rs: e.dma_start(out=o16[rs, :], in_=a[:]), reads=[("A", j)]))
        outs.append(S.dma("sp", lambda e, b=b, rs=rs: e.dma_start(out=o32[rs, :], in_=b[:]), reads=[("B", j)]))
    S.emit(final_wait_ops=outs[-6:])
    return nc


def build_gla(NSEG=8, eps=1e-5):
    nc = bass.Bass("TRN2", target_bir_lowering=False)
    DK, DV, C, SEG = 192, 384, 64, 512
    CPS = SEG // C
    Ttot = NSEG * SEG
    NCH = Ttot // C
    qT_d = nc.dram_tensor("qT", [DK, Ttot], BF16, kind="ExternalInput").ap()
    kT_d = nc.dram_tensor("kT", [DK, Ttot], BF16, kind="ExternalInput").ap()
    lrT_d = nc.dram_tensor("lrT", [16, Ttot], BF16, kind="ExternalInput").ap()
    Wg_d = nc.dram_tensor("Wg", [16, DK], F32, kind="ExternalInput").ap()
    bg_d = nc.dram_tensor("bg", [DK, 1], F32, kind="ExternalInput").ap()
    v_d = nc.dram_tensor("v", [C, NCH, DV], BF16, kind="ExternalInput").ap()
    gr_d = nc.dram_tensor("gr", [C, NCH, DV], BF16, kind="ExternalInput").ap()
    gn_d = nc.dram_tensor("gn", [1, DV], F32, kind="ExternalInput").ap()
    o_d = nc.dram_tensor("o", [C, NCH, DV], BF16, kind="ExternalOutput").ap()
    S = Sched(nc)
    DCS = ((0, 128), (128, 64))
    ones = S.sb([128, 128], F32, "ones")
    ident_f = S.sb([128, 128], F32, "ident_f")
    ident = S.sb([128, 128], BF16, "ident")
    umask = S.sb([64, 64], F32, "umask")
    rmask = S.sb([128, SEG], F32, "rmask")
    Wg32 = S.sb([16, DK], F32, "Wg32")
    Wg = S.sb([16, DK], BF16, "Wgb")
    nbg = [S.sb([sz, 1], F32, f"nbg{i}") for i, (o_, sz) in enumerate(DCS)]
    gn = S.sb([64, DV], F32, "gnb")
    S.pool(lambda e: e.memset(ones[:], 1.0), writes=["ones"])
    S.pool(lambda e: e.affine_select(out=ident_f[:], in_=ones[:], pattern=[[1, 128]], compare_op=ALU.is_equal, fill=0.0,
                                     base=0, channel_multiplier=-1), reads=["ones"], writes=["ident_f"])
    S.dve(lambda e: e.tensor_copy(out=ident[:], in_=ident_f[:]), reads=["ident_f"], writes=["ident"])
    S.pool(lambda e: e.affine_select(out=umask[:], in_=ones[0:64, 0:64], pattern=[[1, 64]], compare_op=ALU.is_ge, fill=0.0,
                                     base=0, channel_multiplier=-1), reads=["ones"], writes=["umask"])
    S.pool(lambda e: e.memset(rmask[:], 1.0), writes=["rmask"])
    S.pool(lambda e: e.memset(rmask[:, 0:SEG:C], 0.0), writes=["rmask"])
    S.dma("sp", lambda e: e.dma_start(out=Wg32[:], in_=Wg_d), writes=["Wg32"])
    S.dve(lambda e: e.tensor_copy(out=Wg[:], in_=Wg32[:]), reads=["Wg32"], writes=["Wg"])
    for i, (o_, sz) in enumerate(DCS):
        S.dma("sp", lambda e, i=i, o_=o_, sz=sz: e.dma_start(out=nbg[i][:], in_=bg_d[o_:o_ + sz, :]), writes=[("nbg", i)])
        S.dve(lambda e, i=i: e.tensor_scalar(out=nbg[i][:], in0=nbg[i][:], scalar1=-1.0, scalar2=None, op0=ALU.mult),
              reads=[("nbg", i)], writes=[("nbg", i)])
    S.dma("sp", lambda e: e.dma_start(out=gn[:], in_=gn_d[0, :].partition_broadcast(64)), writes=["gn"])
    st32 = [S.sb([sz, DV], F32, f"st32_{i}") for i, (o_, sz) in enumerate(DCS)]
    stbf = [S.sb([sz, DV], BF16, f"stbf_{i}") for i, (o_, sz) in enumerate(DCS)]
    def mk(shape, dt, nm):
        return [S.sb(shape, dt, f"{nm}{j}") for j in range(2)]
    qr = [mk([sz, SEG], BF16, f"qr{i}_") for i, (o_, sz) in enumerate(DCS)]
    kr = [mk([sz, SEG], BF16, f"kr{i}_") for i, (o_, sz) in enumerate(DCS)]
    qt = [mk([sz, SEG], BF16, f"qt{i}_") for i, (o_, sz) in enumerate(DCS)]
    kt = [mk([sz, SEG], BF16, f"kt{i}_") for i, (o_, sz) in enumerate(DCS)]
    kh = [mk([sz, SEG], BF16, f"kh{i}_") for i, (o_, sz) in enumerate(DCS)]
    gdec = [mk([sz, CPS], F32, f"gdec{i}_") for i, (o_, sz) in enumerate(DCS)]
    nbl = [mk([sz, CPS], F32, f"nbl{i}_") for i, (o_, sz) in enumerate(DCS)]
    lr = mk([16, SEG], BF16, "lr")
    vv = mk([C, CPS, DV], BF16, "vv")
    grr = mk([C, CPS, DV], BF16, "grr")
    sgr = mk([C, CPS, DV], BF16, "sgr")
    khat = mk([C, CPS, DK], BF16, "khat")
    sTb = mk([C, CPS, C], BF16, "sTb")
    ob = mk([C, CPS, DV], BF16, "ob")
    t_e = [S.sb([sz, SEG], F32, f"t_e{i}") for i, (o_, sz) in enumerate(DCS)]
    t_cs = [S.sb([sz, SEG], F32, f"t_cs{i}") for i, (o_, sz) in enumerate(DCS)]
    t_x = [S.sb([sz, SEG], F32, f"t_x{i}") for i, (o_, sz) in enumerate(DCS)]
    rs_t = [S.sb([C, 8], F32, f"rs{i}") for i in range(2)]
    junk = S.sb([C, DV], F32, "junk")
    pz = S.ps([128, SEG], F32, "pz")
    pS = S.ps([C, 512], F32, "pS")
    pT = S.ps([128, 512], BF16, "pT")
    pO = [S.ps([C, 512], F32, f"pO{i}") for i in range(2)]
    pD = [S.ps([128, 512], F32, f"pD{i}") for i in range(2)]
    outs = []
    INV16 = 1.0 / 16.0
    for sg in range(NSEG):
        j = sg % 2
        ts = slice(sg * SEG, (sg + 1) * SEG)
        cs0 = sg * CPS
        S.dma("sp", lambda e, j=j, ts=ts: e.dma_start(out=lr[j][:], in_=lrT_d[:, ts]), writes=[("lr", j)])
        for i, (o_, sz) in enumerate(DCS):
            S.dma("sp", lambda e, i=i, j=j, o_=o_, sz=sz, ts=ts: e.dma_start(out=qr[i][j][:], in_=qT_d[o_:o_ + sz, ts]), writes=[("qr", i, j)])
            S.dma("sp", lambda e, i=i, j=j, o_=o_, sz=sz, ts=ts: e.dma_start(out=kr[i][j][:], in_=kT_d[o_:o_ + sz, ts]), writes=[("kr", i, j)])
        S.dma("act", lambda e, j=j, cs0=cs0: e.dma_start(out=vv[j][:], in_=v_d[:, cs0:cs0 + CPS, :]), writes=[("vv", j)])
        S.dma("act", lambda e, j=j, cs0=cs0: e.dma_start(out=grr[j][:], in_=gr_d[:, cs0:cs0 + CPS, :]), writes=[("grr", j)])
        S.act(lambda e, j=j: e.activation(out=sgr[j][:], in_=grr[j][:], func=AF.Silu), reads=[("grr", j)], writes=[("sgr", j)])
        S.pool(lambda e, j=j: e.tensor_tensor(out=sgr[j][:], in0=sgr[j][:], in1=gn[:].unsqueeze(1).broadcast_to([C, CPS, DV]), op=ALU.mult),
               reads=[("sgr", j), "gn"], writes=[("sgr", j)])
        for i, (o_, sz) in enumerate(DCS):
            E, CSM, X = t_e[i], t_cs[i], t_x[i]
            kE, kC, kX = ("t_e", i), ("t_cs", i), ("t_x", i)
            S.pe(lambda e, i=i, j=j, o_=o_, sz=sz: e.matmul(pz[0:sz, :], lhsT=Wg[:, o_:o_ + sz], rhs=lr[j][:], start=True, stop=True),
                 reads=["Wg", ("lr", j)], writes=["pz"])
            S.act(lambda e, i=i, sz=sz, E=E: e.activation(out=E[:], in_=pz[0:sz, :], func=AF.Exp, bias=nbg[i][:, 0:1], scale=-1.0),
                  reads=["pz", ("nbg", i)], writes=[kE])
            S.act(lambda e, E=E: e.activation(out=E[:], in_=E[:], func=AF.Ln, bias=1.0, scale=1.0), reads=[kE], writes=[kE])
            S.dve(lambda e, E=E, CSM=CSM, sz=sz: e.tensor_tensor_scan(out=CSM[:], data0=rmask[0:sz, :], data1=E[:], initial=0.0,
                                                                       op0=ALU.mult, op1=ALU.add), reads=[kE, "rmask"], writes=[kC])
            S.dve(lambda e, i=i, j=j, CSM=CSM: e.tensor_scalar(out=nbl[i][j][:], in0=CSM[:, C - 1:SEG:C], scalar1=-INV16, scalar2=None, op0=ALU.mult),
                  reads=[kC], writes=[("nbl", i, j)])
            S.act(lambda e, i=i, j=j: e.activation(out=gdec[i][j][:], in_=nbl[i][j][:], func=AF.Exp), reads=[("nbl", i, j)], writes=[("gdec", i, j)])
            S.act(lambda e, CSM=CSM, X=X: e.activation(out=X[:], in_=CSM[:], func=AF.Exp, scale=-INV16), reads=[kC], writes=[kX])
            S.dve(lambda e, i=i, j=j, X=X: e.scalar_tensor_tensor(out=qt[i][j][:], in0=X[:], scalar=float(DK ** -0.5), in1=qr[i][j][:],
                                                                   op0=ALU.mult, op1=ALU.mult), reads=[kX, ("qr", i, j)], writes=[("qt", i, j)])
            S.act(lambda e, CSM=CSM, X=X: e.activation(out=X[:], in_=CSM[:], func=AF.Exp, scale=INV16), reads=[kC], writes=[kX])
            S.dve(lambda e, i=i, j=j, X=X: e.tensor_tensor(out=kt[i][j][:], in0=X[:], in1=kr[i][j][:], op=ALU.mult),
                  reads=[kX, ("kr", i, j)], writes=[("kt", i, j)])
            for c in range(CPS):
                S.act(lambda e, i=i, j=j, c=c, CSM=CSM, X=X: e.activation(out=X[:, c * C:(c + 1) * C], in_=CSM[:, c * C:(c + 1) * C], func=AF.Exp,
                                                                           bias=nbl[i][j][:, c:c + 1], scale=INV16),
                      reads=[kC, ("nbl", i, j)], writes=[kX])
            S.dve(lambda e, i=i, j=j, X=X: e.tensor_tensor(out=kh[i][j][:], in0=X[:], in1=kr[i][j][:], op=ALU.mult),
                  reads=[kX, ("kr", i, j)], writes=[("kh", i, j)])
        for c in range(CPS):
            cs = slice(c * C, (c + 1) * C)
            S.pe(lambda e, j=j, cs=cs: e.transpose(out=pT[0:C, 0:128], in_=kh[0][j][:, cs], identity=ident[:]),
                 reads=[("kh", 0, j), "ident"], writes=["pT"])
            S.pe(lambda e, j=j, cs=cs: e.transpose(out=pT[0:C, 128:192], in_=kh[1][j][:, cs], identity=ident[0:64, 0:64]),
                 reads=[("kh", 1, j), "ident"], writes=["pT"])
            S.act(lambda e, j=j, c=c: e.copy(out=khat[j][:, c, :], in_=pT[0:C, 0:DK]), reads=["pT"], writes=[("khat", j, c)])
            S.pe(lambda e, j=j, cs=cs: e.matmul(pS[:, 0:C], lhsT=kt[0][j][:, cs], rhs=qt[0][j][:, cs], start=True, stop=False),
                 reads=[("kt", 0, j), ("qt", 0, j)], writes=["pS"])
            S.pe(lambda e, j=j, cs=cs: e.matmul(pS[:, 0:C], lhsT=kt[1][j][:, cs], rhs=qt[1][j][:, cs], start=False, stop=True),
                 reads=[("kt", 1, j), ("qt", 1, j)], writes=["pS"])
            S.dve(lambda e, j=j, c=c: e.tensor_tensor(out=sTb[j][:, c, :], in0=pS[:, 0:C], in1=umask[:], op=ALU.mult),
                  reads=["pS", "umask"], writes=[("sTb", j, c)])
        for c in range(CPS):
            gc = cs0 + c
            cs = slice(c * C, (c + 1) * C)
            po = pO[gc % 2]
            kpo = ("pO", gc % 2)
            first = (gc == 0)
            S.pe(lambda e, j=j, c=c, po=po, first=first: e.matmul(po[:, 0:DV], lhsT=sTb[j][:, c, :], rhs=vv[j][:, c, :], start=True, stop=first),
                 reads=[("sTb", j, c), ("vv", j)], writes=[kpo])
            if not first:
                S.pe(lambda e, j=j, cs=cs, po=po: e.matmul(po[:, 0:DV], lhsT=qt[0][j][:, cs], rhs=stbf[0][:], start=False, stop=False),
                     reads=[("qt", 0, j), ("stbf", 0)], writes=[kpo])
                S.pe(lambda e, j=j, cs=cs, po=po: e.matmul(po[:, 0:DV], lhsT=qt[1][j][:, cs], rhs=stbf[1][:], start=False, stop=True),
                     reads=[("qt", 1, j), ("stbf", 1)], writes=[kpo])
            for i, (o_, sz) in enumerate(DCS):
                pd = pD[i]
                S.pe(lambda e, i=i, j=j, c=c, o_=o_, sz=sz, pd=pd: e.matmul(pd[0:sz, 0:DV], lhsT=khat[j][:, c, o_:o_ + sz], rhs=vv[j][:, c, :],
                                                                             start=True, stop=True),
                     reads=[("khat", j, c), ("vv", j)], writes=[("pD", i)])
                if first:
                    S.dve(lambda e, i=i, sz=sz, pd=pd: e.tensor_copy(out=st32[i][:], in_=pd[0:sz, 0:DV]), reads=[("pD", i)], writes=[("st32", i)])
                else:
                    S.dve(lambda e, i=i, j=j, c=c, sz=sz, pd=pd: e.scalar_tensor_tensor(out=st32[i][:], in0=st32[i][:], scalar=gdec[i][j][:, c:c + 1],
                                                                                         in1=pd[0:sz, 0:DV], op0=ALU.mult, op1=ALU.add),
                          reads=[("pD", i), ("st32", i), ("gdec", i, j)], writes=[("st32", i)])
                S.act(lambda e, i=i: e.copy(out=stbf[i][:], in_=st32[i][:]), reads=[("st32", i)], writes=[("stbf", i)])
            R = rs_t[gc % 2]
            kR = ("rs", gc % 2)
            S.act(lambda e, po=po, R=R: e.activation(out=junk[:], in_=po[:, 0:DV], func=AF.Square, accum_out=R[:, 0:1]),
                  reads=[kpo], writes=[kR, "junk"])
            S.dve(lambda e, R=R: e.tensor_scalar(out=R[:, 1:2], in0=R[:, 0:1], scalar1=float(1.0 / DV), scalar2=float(eps), op0=ALU.mult, op1=ALU.add),
                  reads=[kR], writes=[kR])
            S.act(lambda e, R=R: e.activation(out=R[:, 2:3], in_=R[:, 1:2], func=AF.Sqrt), reads=[kR], writes=[kR])
            S.dve(lambda e, R=R: e.reciprocal(out=R[:, 3:4], in_=R[:, 2:3]), reads=[kR], writes=[kR])
            S.dve(lambda e, j=j, c=c, po=po, R=R: e.scalar_tensor_tensor(out=ob[j][:, c, :], in0=po[:, 0:DV], scalar=R[:, 3:4], in1=sgr[j][:, c, :],
                                                                          op0=ALU.mult, op1=ALU.mult), reads=[kpo, kR, ("sgr", j)], writes=[("ob", j)])
        outs.append(S.dma("sp", lambda e, j=j, cs0=cs0: e.dma_start(out=o_d[:, cs0:cs0 + CPS, :], in_=ob[j][:]), reads=[("ob", j)]))
    S.emit(final_wait_ops=outs[-3:])
    return nc

NEG = -1.0e9


def _softmax_pv_consts(S):
    ones = S.sb([128, 128], F32, "c_ones")
    ident_f = S.sb([128, 128], F32, "c_identf")
    ident = S.sb([128, 128], BF16, "c_ident")
    S.pool(lambda e: e.memset(ones[:], 1.0), writes=["c_ones"])
    S.pool(lambda e: e.affine_select(out=ident_f[:], in_=ones[:], pattern=[[1, 128]], compare_op=ALU.is_equal, fill=0.0,
                                     base=0, channel_multiplier=-1), reads=["c_ones"], writes=["c_identf"])
    S.dve(lambda e: e.tensor_copy(out=ident[:], in_=ident_f[:]), reads=["c_identf"], writes=["c_ident"])
    return ident


def build_diff(NSLOT, SB, NH, lam_init, eps=1e-5):
    nc = bass.Bass("TRN2", target_bir_lowering=False)
    D, DV = 64, 128
    TK = NSLOT * SB * 128
    TQ = NSLOT * 128
    NKBT = TK // 128
    qT_d = nc.dram_tensor("qT", [NH, 128, TQ], BF16, kind="ExternalInput").ap()
    kT_d = nc.dram_tensor("kT", [NH, 128, TK], BF16, kind="ExternalInput").ap()
    v_d = nc.dram_tensor("v", [NH, 128, NKBT, DV], BF16, kind="ExternalInput").ap()
    msk_d = nc.dram_tensor("msk", [NSLOT, 128, SB * 128], F32, kind="ExternalInput").ap()
    lam_d = nc.dram_tensor("lamv", [4, D], F32, kind="ExternalInput").ap()
    g_d = nc.dram_tensor("g", [1, DV], F32, kind="ExternalInput").ap()
    o_d = nc.dram_tensor("o", [TQ, NH, DV], BF16, kind="ExternalOutput").ap()
    S = Sched(nc)
    ident = _softmax_pv_consts(S)
    lv = S.sb([128, 4, D], F32, "lv")
    lw = S.sb([128, 8], F32, "lw")
    junk = S.sb([128, DV], F32, "junk")
    gt = S.sb([128, DV], F32, "gt")
    for i in range(4):
        S.dma("sp", lambda e, i=i: e.dma_start(out=lv[:, i, :], in_=lam_d[i, :].partition_broadcast(128)), writes=[("lv", i)])
    S.dma("sp", lambda e: e.dma_start(out=gt[:], in_=g_d[0, :].partition_broadcast(128)), writes=["gt"])
    S.dve(lambda e: e.tensor_scalar(out=gt[:], in0=gt[:], scalar1=float(1.0 - lam_init), scalar2=None, op0=ALU.mult), reads=["gt"], writes=["gt"])
    for i in range(2):
        S.dve(lambda e, i=i: e.tensor_tensor(out=junk[:, 0:D], in0=lv[:, 2 * i, :], in1=lv[:, 2 * i + 1, :], op=ALU.mult),
              reads=[("lv", 2 * i), ("lv", 2 * i + 1)], writes=["junk"])
        S.dve(lambda e, i=i: e.reduce_sum(out=lw[:, i:i + 1], in_=junk[:, 0:D], axis=AX.X), reads=["junk"], writes=["lw"])
    S.act(lambda e: e.activation(out=lw[:, 2:4], in_=lw[:, 0:2], func=AF.Exp), reads=["lw"], writes=["lw"])
    S.dve(lambda e: e.tensor_tensor(out=lw[:, 4:5], in0=lw[:, 3:4], in1=lw[:, 2:3], op=ALU.subtract), reads=["lw"], writes=["lw"])
    S.dve(lambda e: e.tensor_scalar(out=lw[:, 5:6], in0=lw[:, 4:5], scalar1=float(-lam_init), scalar2=None, op0=ALU.add), reads=["lw"], writes=["lw"])
    nlam = lw[:, 5:6]
    mk_ = S.sb([128, NSLOT, SB * 128], F32, "mk_sb")
    S.dma("sp", lambda e: e.dma_start(out=mk_[:], in_=msk_d.rearrange("s p k -> p s k")), writes=["mk"])
    qt = [S.sb([128, TQ], BF16, f"qt{i}") for i in range(2)]
    kt = [S.sb([128, TK], BF16, f"kt{i}") for i in range(2)]
    vt = [S.sb([128, NKBT, DV], BF16, f"vt{i}") for i in range(2)]
    Sb = [[S.sb([128, TK], F32, f"S{c}_{i}") for i in range(2)] for c in range(2)]
    Ab = [S.sb([128, TK], BF16, f"A{i}") for i in range(2)]
    AT = [S.sb([128, NKBT, 128], BF16, f"AT{i}") for i in range(2)]
    st = [S.sb([128, 64], F32, f"stt{i}") for i in range(2)]
    ob = [S.sb([128, DV], BF16, f"ob{i}") for i in range(4)]
    pS = [S.ps([128, 512], F32, f"pS{i}") for i in range(4)]
    pT = [S.ps([128, 8, 128], BF16, f"pT{i}") for i in range(2)]
    pO = [S.ps([128, 512], F32, f"pO{i}") for i in range(2)]
    outs = []
    it = 0
    ips = 0
    ipt = 0
    for h in range(NH):
        hj = h % 2
        S.dma("sp", lambda e, h=h, hj=hj: e.dma_start(out=qt[hj][:], in_=qT_d[h]), writes=[("qt", hj)])
        S.dma("sp", lambda e, h=h, hj=hj: e.dma_start(out=kt[hj][:], in_=kT_d[h]), writes=[("kt", hj)])
        S.dma("act", lambda e, h=h, hj=hj: e.dma_start(out=vt[hj][:], in_=v_d[h]), writes=[("vt", hj)])
        for k in range(NSLOT):
            j = it % 2
            L = SB * (k + 1) * 128
            NKB = L // 128
            L0 = L - SB * 128
            ST = st[j]
            kST = ("st", j)
            qs = slice(k * 128, (k + 1) * 128)
            nmx = 0
            for c in range(2):
                Sc = Sb[c][j]
                kS = ("S", c, j)
                ps_ = slice(64 * c, 64 * c + 64)
                col = 0
                while col < L:
                    w = min(512, L - col) if col >= L0 else min(512, L0 - col)
                    p = pS[ips % 4]
                    kp = ("pS", ips % 4)
                    S.pe(lambda e, p=p, hj=hj, ps_=ps_, qs=qs, col=col, w=w: e.matmul(p[:, 0:w], lhsT=qt[hj][ps_, qs], rhs=kt[hj][ps_, col:col + w],
                                                                                  start=True, stop=True),
                         reads=[("qt", hj), ("kt", hj)], writes=[kp])
                    mcol = ST[:, 16 * c + nmx % 16: 16 * c + nmx % 16 + 1]
                    if col >= L0:
                        S.dve(lambda e, p=p, Sc=Sc, col=col, w=w, k=k, L0=L0: e.tensor_tensor(
                            out=Sc[:, col:col + w], in0=p[:, 0:w], in1=mk_[:, k, col - L0:col - L0 + w], op=ALU.add),
                            reads=[kp, "mk"], writes=[kS])
                        S.dve(lambda e, Sc=Sc, col=col, w=w, mcol=mcol: e.reduce_max(out=mcol, in_=Sc[:, col:col + w], axis=AX.X),
                              reads=[kS], writes=[(kST, c, nmx)])
                    else:
                        S.dve(lambda e, p=p, Sc=Sc, col=col, w=w, mcol=mcol: e.tensor_scalar(
                            out=Sc[:, col:col + w], in0=p[:, 0:w], scalar1=1.0, scalar2=-3.0e38, op0=ALU.mult, op1=ALU.max, accum_out=mcol),
                            reads=[kp], writes=[kS, (kST, c, nmx)])
                    ips += 1
                    nmx += 1
                    col += w
                nm = nmx
                nmx = 0
                S.dve(lambda e, ST=ST, c=c, nm=nm: e.reduce_max(out=ST[:, 32 + c:33 + c], in_=ST[:, 16 * c:16 * c + nm], axis=AX.X),
                      reads=[(kST, c, i_) for i_ in range(nm)], writes=[(kST, "m", c)])
                S.dve(lambda e, ST=ST, c=c: e.tensor_scalar(out=ST[:, 34 + c:35 + c], in0=ST[:, 32 + c:33 + c], scalar1=-0.125, scalar2=None, op0=ALU.mult),
                      reads=[(kST, "m", c)], writes=[(kST, "nmb", c)])
                S.act(lambda e, Sc=Sc, ST=ST, c=c, L=L: e.activation(out=Sc[:, 0:L], in_=Sc[:, 0:L], func=AF.Exp, bias=ST[:, 34 + c:35 + c], scale=0.125,
                                                                     accum_out=ST[:, 36 + c:37 + c]),
                      reads=[kS, (kST, "nmb", c)], writes=[kS, (kST, "l", c)])
            S.dve(lambda e, ST=ST: e.reciprocal(out=ST[:, 38:40], in_=ST[:, 36:38]), reads=[(kST, "l", 0), (kST, "l", 1)], writes=[(kST, "r")])
            S.dve(lambda e, ST=ST: e.tensor_scalar(out=ST[:, 40:41], in0=ST[:, 39:40], scalar1=nlam, scalar2=None, op0=ALU.mult),
                  reads=[(kST, "r"), "lw"], writes=[(kST, "r2")])
            A = Ab[j]
            kA = ("A", j)
            S.act(lambda e, A=A, j=j, ST=ST, L=L: e.activation(out=A[:, 0:L], in_=Sb[0][j][:, 0:L], func=AF.Copy, scale=ST[:, 38:39]),
                  reads=[("S", 0, j), (kST, "r")], writes=[kA])
            S.dve(lambda e, A=A, j=j, ST=ST, L=L: e.scalar_tensor_tensor(out=A[:, 0:L], in0=Sb[1][j][:, 0:L], scalar=ST[:, 40:41], in1=A[:, 0:L],
                                                                          op0=ALU.mult, op1=ALU.add),
                  reads=[("S", 1, j), (kST, "r2"), kA], writes=[kA])
            ATj = AT[j]
            kb = 0
            while kb < NKB:
                g = min(4, NKB - kb)
                pt = pT[ipt % 2]
                kpt = ("pT", ipt % 2)
                for u in range(g):
                    S.pe(lambda e, pt=pt, A=A, kb=kb, u=u: e.transpose(out=pt[:, u, :], in_=A[:, (kb + u) * 128:(kb + u + 1) * 128], identity=ident[:]),
                         reads=[kA, "c_ident"], writes=[kpt])
                if ipt % 2 == 0:
                    S.act(lambda e, pt=pt, ATj=ATj, kb=kb, g=g: e.copy(out=ATj[:, kb:kb + g, :], in_=pt[:, 0:g, :]), reads=[kpt], writes=[("AT", j, kb)])
                else:
                    S.dve(lambda e, pt=pt, ATj=ATj, kb=kb, g=g: e.tensor_copy(out=ATj[:, kb:kb + g, :], in_=pt[:, 0:g, :]), reads=[kpt], writes=[("AT", j, kb)])
                ipt += 1
                kb += g
            po = pO[it % 2]
            kpo = ("pO", it % 2)
            for kb in range(NKB):
                S.pe(lambda e, po=po, ATj=ATj, hj=hj, kb=kb, NKB=NKB: e.matmul(po[:, 0:DV], lhsT=ATj[:, kb, :], rhs=vt[hj][:, kb, :],
                                                                               start=(kb == 0), stop=(kb == NKB - 1)),
                     reads=[("AT", j, (kb // 4) * 4), ("vt", hj)], writes=[kpo])
            S.act(lambda e, po=po, ST=ST: e.activation(out=junk[:], in_=po[:, 0:DV], func=AF.Square, accum_out=ST[:, 42:43]),
                  reads=[kpo], writes=[(kST, "ss"), "junk"])
            S.dve(lambda e, ST=ST: e.tensor_scalar(out=ST[:, 43:44], in0=ST[:, 42:43], scalar1=float(1.0 / DV), scalar2=float(eps), op0=ALU.mult, op1=ALU.add),
                  reads=[(kST, "ss")], writes=[(kST, "ms")])
            S.act(lambda e, ST=ST: e.activation(out=ST[:, 44:45], in_=ST[:, 43:44], func=AF.Sqrt), reads=[(kST, "ms")], writes=[(kST, "sq")])
            S.dve(lambda e, ST=ST: e.reciprocal(out=ST[:, 45:46], in_=ST[:, 44:45]), reads=[(kST, "sq")], writes=[(kST, "rs")])
            o = ob[it % 4]
            S.dve(lambda e, o=o, po=po, ST=ST: e.scalar_tensor_tensor(out=o[:], in0=po[:, 0:DV], scalar=ST[:, 45:46], in1=gt[:], op0=ALU.mult, op1=ALU.mult),
                  reads=[kpo, (kST, "rs"), "gt"], writes=[("ob", it % 4)])
            outs.append(S.dma("sp", lambda e, o=o, qs=qs, h=h: e.dma_start(out=o_d[qs, h, :], in_=o[:]), reads=[("ob", it % 4)]))
            it += 1
    S.emit(final_wait_ops=outs[-6:])
    return nc

NEGBIG = -1.0e30


def build_dsa(NSLOT, SB, NH=10, NG=2, NIH=32, KSEL=256):
    nc = bass.Bass("TRN2", target_bir_lowering=False)
    DH, DI = 128, 64
    TK = NSLOT * SB * 128
    TQ = NSLOT * 128
    NKBT = TK // 128
    R = NH // NG
    NPR = NIH // 2
    NRND = KSEL // 8
    scale = float(DH ** -0.5)
    sq_d = nc.dram_tensor("sqT", [NH, DH, TQ], BF16, kind="ExternalInput").ap()
    sk_d = nc.dram_tensor("skT", [NG, DH, TK], BF16, kind="ExternalInput").ap()
    sv_d = nc.dram_tensor("sv", [NG, 128, NKBT, DH], BF16, kind="ExternalInput").ap()
    iq_d = nc.dram_tensor("iqT", [NPR, 128, TQ], BF16, kind="ExternalInput").ap()
    ik_d = nc.dram_tensor("ikT2", [128, TK], BF16, kind="ExternalInput").ap()
    iw_d = nc.dram_tensor("iw", [TQ, NIH], BF16, kind="ExternalInput").ap()
    msk_d = nc.dram_tensor("msk", [NSLOT, 128, SB * 128], F32, kind="ExternalInput").ap()
    o_d = nc.dram_tensor("o", [TQ, NH, DH], BF16, kind="ExternalOutput").ap()
    S = Sched(nc)
    ident = _softmax_pv_consts(S)
    sk = S.sb([128, NG, TK], BF16, "sk_sb")
    sv = S.sb([128, NG, NKBT, DH], BF16, "sv_sb")
    ik = S.sb([128, TK], BF16, "ik_sb")
    iwb = S.sb([128, NSLOT, NIH], BF16, "iwb")
    iw = S.sb([128, NSLOT, NIH], F32, "iw32")
    mk_ = S.sb([128, NSLOT, SB * 128], F32, "mk_sb")
    for g in range(NG):
        S.dma("sp", lambda e, g=g: e.dma_start(out=sk[:, g, :], in_=sk_d[g]), writes=[("sk", g)])
        S.dma("act", lambda e, g=g: e.dma_start(out=sv[:, g, :, :], in_=sv_d[g]), writes=[("sv", g)])
    S.dma("sp", lambda e: e.dma_start(out=ik[:], in_=ik_d), writes=["ik"])
    S.dma("sp", lambda e: e.dma_start(out=iwb[:], in_=iw_d.rearrange("(s p) h -> p s h", p=128)), writes=["iwb"])
    S.dve(lambda e: e.tensor_copy(out=iw[:], in_=iwb[:]), reads=["iwb"], writes=["iw"])
    S.dma("sp", lambda e: e.dma_start(out=mk_[:], in_=msk_d.rearrange("s p k -> p s k")), writes=["mk"])
    iq = [S.sb([128, NPR, 128], BF16, f"iq{i}") for i in range(2)]
    sq = [S.sb([128, NH, 128], BF16, f"sq{i}") for i in range(2)]
    score = S.sb([128, TK], F32, "score")
    Wk = S.sb([128, TK], F32, "Wk")
    sel = S.sb([128, TK], BF16, "sel")
    Rb = [S.sb([128, 512], F32, f"Rb{i}") for i in range(4)]
    Sb = [S.sb([128, TK], F32, f"S{i}") for i in range(2)]
    Pb = [S.sb([128, TK], BF16, f"P{i}") for i in range(2)]
    AT = [S.sb([128, NKBT, 128], BF16, f"AT{i}") for i in range(2)]
    st = [S.sb([128, 32], F32, f"stt{i}") for i in range(2)]
    m8 = S.sb([128, 8], F32, "m8")
    thr = S.sb([128, 1], F32, "thr")
    ob = [S.sb([128, DH], BF16, f"ob{i}") for i in range(4)]
    pS = [S.ps([128, 512], F32, f"pS{i}") for i in range(4)]
    pT = [S.ps([128, 8, 128], BF16, f"pT{i}") for i in range(2)]
    pO = [S.ps([128, 512], F32, f"pO{i}") for i in range(2)]
    outs = []
    ips = ir = ipt = it = 0
    for k in range(NSLOT):
        kj = k % 2
        L = SB * (k + 1) * 128
        NKB = L // 128
        L0 = L - SB * 128
        qs = slice(k * 128, (k + 1) * 128)
        S.dma("sp", lambda e, kj=kj, qs=qs: e.dma_start(out=iq[kj][:], in_=iq_d[:, :, qs].rearrange("n p q -> p n q")), writes=[("iq", kj)])
        S.dma("sp", lambda e, kj=kj, qs=qs: e.dma_start(out=sq[kj][:], in_=sq_d[:, :, qs].rearrange("n p q -> p n q")), writes=[("sq", kj)])
        chunks = []
        col = 0
        while col < L:
            w = min(512, L - col)
            chunks.append((col, w))
            col += w
        for hh in range(NIH):
            pr, mem = hh // 2, hh % 2
            ps_ = slice(64 * mem, 64 * mem + 64)
            for (col, w) in chunks:
                p = pS[ips % 4]
                kp = ("pS", ips % 4)
                rb = Rb[ir % 4]
                krb = ("Rb", ir % 4)
                S.pe(lambda e, p=p, kj=kj, pr=pr, ps_=ps_, col=col, w=w: e.matmul(p[:, 0:w], lhsT=iq[kj][ps_, pr, :], rhs=ik[ps_, col:col + w],
                                                                                  start=True, stop=True),
                     reads=[("iq", kj), "ik"], writes=[kp])
                S.act(lambda e, p=p, rb=rb, w=w: e.activation(out=rb[:, 0:w], in_=p[:, 0:w], func=AF.Relu), reads=[kp], writes=[krb])
                wcol = iw[:, k, hh:hh + 1]
                if hh == 0:
                    S.dve(lambda e, rb=rb, col=col, w=w, wcol=wcol: e.tensor_scalar(out=score[:, col:col + w], in0=rb[:, 0:w], scalar1=wcol, scalar2=None,
                                                                                    op0=ALU.mult), reads=[krb, "iw"], writes=[("score", col)])
                else:
                    S.dve(lambda e, rb=rb, col=col, w=w, wcol=wcol: e.scalar_tensor_tensor(out=score[:, col:col + w], in0=rb[:, 0:w], scalar=wcol,
                                                                                           in1=score[:, col:col + w], op0=ALU.mult, op1=ALU.add),
                          reads=[krb, "iw", ("score", col)], writes=[("score", col)])
                ips += 1
                ir += 1
        allsc = [("score", c_) for (c_, w_) in chunks]
        S.dve(lambda e, k=k, L=L, L0=L0: e.tensor_tensor(out=score[:, L0:L], in0=score[:, L0:L], in1=mk_[:, k, :], op=ALU.add),
              reads=allsc + ["mk"], writes=allsc)
        S.act(lambda e, L=L: e.copy(out=Wk[:, 0:L], in_=score[:, 0:L]), reads=allsc, writes=["Wk"])
        for r in range(NRND):
            S.dve(lambda e, L=L: e.max(out=m8[:], in_=Wk[:, 0:L]), reads=["Wk"], writes=["m8"])
            if r < NRND - 1:
                S.dve(lambda e, L=L: e.match_replace(out=Wk[:, 0:L], in_to_replace=m8[:], in_values=Wk[:, 0:L], imm_value=NEGBIG),
                      reads=["Wk", "m8"], writes=["Wk"])
        S.dve(lambda e: e.tensor_scalar(out=thr[:], in0=m8[:, 7:8], scalar1=-1.0e29, scalar2=None, op0=ALU.max), reads=["m8"], writes=["thr"])
        S.dve(lambda e, L=L: e.tensor_scalar(out=sel[:, 0:L], in0=score[:, 0:L], scalar1=thr[:, 0:1], scalar2=None, op0=ALU.is_ge),
              reads=allsc + ["thr"], writes=["sel"])
        for h in range(NH):
            g = h // R
            j = it % 2
            ST = st[j]
            kST = ("st", j)
            Sc, P = Sb[j], Pb[j]
            kS, kP = ("S", j), ("P", j)
            for ci, (col, w) in enumerate(chunks):
                p = pS[ips % 4]
                kp = ("pS", ips % 4)
                S.pe(lambda e, p=p, kj=kj, h=h, g=g, col=col, w=w: e.matmul(p[:, 0:w], lhsT=sq[kj][:, h, :], rhs=sk[:, g, col:col + w], start=True, stop=True),
                     reads=[("sq", kj), ("sk", g)], writes=[kp])
                S.dve(lambda e, p=p, Sc=Sc, col=col, w=w, ST=ST, ci=ci: e.tensor_scalar(out=Sc[:, col:col + w], in0=p[:, 0:w], scalar1=1.0, scalar2=-3.0e38,
                                                                                        op0=ALU.mult, op1=ALU.max, accum_out=ST[:, ci:ci + 1]),
                      reads=[kp], writes=[kS, (kST, ci)])
                ips += 1
            nch = len(chunks)
            S.dve(lambda e, ST=ST, nch=nch: e.reduce_max(out=ST[:, 16:17], in_=ST[:, 0:nch], axis=AX.X), reads=[(kST, i_) for i_ in range(nch)], writes=[(kST, "m")])
            S.dve(lambda e, ST=ST: e.tensor_scalar(out=ST[:, 17:18], in0=ST[:, 16:17], scalar1=-scale, scalar2=None, op0=ALU.mult), reads=[(kST, "m")], writes=[(kST, "nmb")])
            S.act(lambda e, Sc=Sc, P=P, ST=ST, L=L: e.activation(out=P[:, 0:L], in_=Sc[:, 0:L], func=AF.Exp, bias=ST[:, 17:18], scale=scale),
                  reads=[kS, (kST, "nmb")], writes=[kP])
            S.dve(lambda e, P=P, ST=ST, L=L: e.scalar_tensor_tensor(out=P[:, 0:L], in0=P[:, 0:L], scalar=1.0, in1=sel[:, 0:L], op0=ALU.mult, op1=ALU.mult,
                                                                    accum_out=ST[:, 18:19]), reads=[kP, "sel"], writes=[kP, (kST, "l")])
            S.dve(lambda e, ST=ST: e.reciprocal(out=ST[:, 19:20], in_=ST[:, 18:19]), reads=[(kST, "l")], writes=[(kST, "r")])
            ATj = AT[j]
            kb = 0
            while kb < NKB:
                gsz = min(4, NKB - kb)
                pt = pT[ipt % 2]
                kpt = ("pT", ipt % 2)
                for u in range(gsz):
                    S.pe(lambda e, pt=pt, P=P, kb=kb, u=u: e.transpose(out=pt[:, u, :], in_=P[:, (kb + u) * 128:(kb + u + 1) * 128], identity=ident[:]),
                         reads=[kP, "c_ident"], writes=[kpt])
                S.act(lambda e, pt=pt, ATj=ATj, kb=kb, gsz=gsz: e.copy(out=ATj[:, kb:kb + gsz, :], in_=pt[:, 0:gsz, :]), reads=[kpt], writes=[("AT", j, kb)])
                ipt += 1
                kb += gsz
            po = pO[it % 2]
            kpo = ("pO", it % 2)
            for kb in range(NKB):
                S.pe(lambda e, po=po, ATj=ATj, g=g, kb=kb, NKB=NKB: e.matmul(po[:, 0:DH], lhsT=ATj[:, kb, :], rhs=sv[:, g, kb, :],
                                                                             start=(kb == 0), stop=(kb == NKB - 1)),
                     reads=[("AT", j, (kb // 4) * 4), ("sv", g)], writes=[kpo])
            o = ob[it % 4]
            S.dve(lambda e, o=o, po=po, ST=ST: e.tensor_scalar(out=o[:], in0=po[:, 0:DH], scalar1=ST[:, 19:20], scalar2=None, op0=ALU.mult),
                  reads=[kpo, (kST, "r")], writes=[("ob", it % 4)])
            outs.append(S.dma("sp", lambda e, o=o, qs=qs, h=h: e.dma_start(out=o_d[qs, h, :], in_=o[:]), reads=[("ob", it % 4)]))
            it += 1
    S.emit(final_wait_ops=outs[-6:])
    return nc


def build_cast(Tc):
    nc = bass.Bass("TRN2", target_bir_lowering=False)
    D = 4096
    x = nc.dram_tensor("x", [Tc, D], F32, kind="ExternalInput").ap()
    xb = nc.dram_tensor("xb", [Tc, D], BF16, kind="ExternalOutput").ap()
    S = Sched(nc)
    xt = [S.sb([128, D], F32, f"xt{i}") for i in range(2)]
    bt = [S.sb([128, D], BF16, f"bt{i}") for i in range(2)]
    outs = []
    for t in range(Tc // 128):
        j = t % 2
        rs = slice(t * 128, (t + 1) * 128)
        S.dma("sp", lambda e, j=j, rs=rs: e.dma_start(out=xt[j][:], in_=x[rs, :]), writes=[("xt", j)])
        S.add(("dve", "pool")[j], lambda e, j=j: e.tensor_copy(out=bt[j][:], in_=xt[j][:]), reads=[("xt", j)], writes=[("bt", j)])
        outs.append(S.dma("sp", lambda e, j=j, rs=rs: e.dma_start(out=xb[rs, :], in_=bt[j][:]), reads=[("bt", j)]))
    S.emit(final_wait_ops=outs[-4:])
    return nc


import time as _time
import ml_dtypes as _mld
from concourse.bass_utils import run_bass_kernel_spmd

_BF = _mld.bfloat16
_NC_CACHE = {}
DEPTH = 2
LAMBDA_INIT = [0.8 - 0.6 * math.exp(-0.3 * l) for l in range(DEPTH)]
ALPHA = (2 * DEPTH) ** 0.25
_SPL = (768, 768, 1536, 1536, 16, 1280, 1280, 1280, 1280, 256, 256, 2048, 64, 32)
_OFF = [0]
for _s in _SPL:
    _OFF.append(_OFF[-1] + _s)
(O_GQ, O_GK, O_GV, O_GR, O_GLR, O_DQ, O_DK, O_DV, O_SQ, O_SK, O_SV, O_IQ, O_IK, O_IW, O_END) = _OFF
_LOG = []


def _get(key, fn):
    if key not in _NC_CACHE:
        _NC_CACHE[key] = fn()
    return _NC_CACHE[key]


def _run(name, nc, in_maps):
    t0 = _time.time()
    res = run_bass_kernel_spmd(nc, in_maps, core_ids=list(range(8)))
    _LOG.append((name, _time.time() - t0))
    print(f"[mk] launch {name}: {_time.time() - t0:.1f}s", flush=True)
    return res.results


def _c(a):
    return np.ascontiguousarray(a)


def _slot_blocks(r):
    return [4 * k + (r if k % 2 == 0 else 3 - r) for k in range(8)]


def _masks(r, neg):
    m = np.zeros((8, 128, 512), np.float32)
    for k, gq in enumerate(_slot_blocks(r)):
        keys = 4 * k * 128 + np.arange(512)
        qpos = gq * 128 + np.arange(128)
        m[k] = np.where(keys[None, :] <= qpos[:, None], 0.0, neg)
    return m


def _forward(inputs, dbg=None):
    x = np.asarray(inputs["x"], np.float32).reshape(8192, 4096)
    pos = np.asarray(inputs["positions"], np.int32).reshape(8192, 1)
    invf = (1.0 / (np.float32(500000.0) ** (np.arange(16, dtype=np.float32) * np.float32(2.0) / np.float32(32)))).astype(np.float32)[None]
    xb_T = None
    for l in range(DEPTH):
        w_in = np.asarray(inputs["w_in"][l], np.float32)
        Wp = np.zeros((4096, 8 * 1664), np.float32)
        Wp[:, :12400] = w_in
        if l == 0:
            ncc = _get(("cast",), lambda: build_cast(1024))
            res = _run("cast_x", ncc, [{"x": _c(x[c * 1024:(c + 1) * 1024])} for c in range(8)])
            xb_T = _c(np.concatenate([res[c]["xb"] for c in range(8)], axis=0).T)
            del res
        xT = xb_T
        nc = _get(("lin", 1664, False), lambda: build_lin(4096, 8192, 1664, False, BF16))
        res = _run(f"inproj{l}", nc, [{"xT": xT, "W": _c(Wp[:, c * 1664:(c + 1) * 1664])} for c in range(8)])
        hT = np.concatenate([res[c]["YT"] for c in range(8)], axis=0)[:12400]
        del Wp, res
        if dbg is not None:
            dbg[f"hT{l}"] = hT
        h16 = _c(np.concatenate([hT[O_DQ:O_DK], hT[O_DK:O_DV], hT[O_IQ:O_IK], hT[O_IK:O_IW]], axis=0).T)
        h32 = _c(np.concatenate([hT[O_SQ:O_SK], hT[O_SK:O_SV]], axis=0).T)
        nc = _get(("rope",), lambda: build_rope(1024))
        res = _run(f"rope{l}", nc, [{"pos": _c(pos[c * 1024:(c + 1) * 1024]), "invf": invf,
                                     "h16": _c(h16[c * 1024:(c + 1) * 1024]), "h32": _c(h32[c * 1024:(c + 1) * 1024])} for c in range(8)])
        r16 = np.concatenate([res[c]["o16"] for c in range(8)], axis=0)
        r32 = np.concatenate([res[c]["o32"] for c in range(8)], axis=0)
        dq, dk, iq, ik = r16[:, 0:1280], r16[:, 1280:2560], r16[:, 2560:4608], r16[:, 4608:4672]
        sq, sk = r32[:, 0:1280], r32[:, 1280:1536]
        del res, h16, h32
        if dbg is not None:
            dbg[f"dq{l}"] = dq; dbg[f"dk{l}"] = dk; dbg[f"iq{l}"] = iq; dbg[f"ik{l}"] = ik; dbg[f"sq{l}"] = sq; dbg[f"sk{l}"] = sk
        wgu = np.asarray(inputs["w_gate_up"][l], np.float32)
        bgt = np.asarray(inputs["b_gate"][l], np.float32)
        gng = np.asarray(inputs["gla_norm_g"][l], np.float32)[None]

        def chunked(a):
            return _c(a.reshape(64, 64, -1).transpose(1, 0, 2))
        maps = []
        for c in range(8):
            b, hd = c // 4, c % 4
            ts = slice(b * 4096, (b + 1) * 4096)
            maps.append({"qT": _c(hT[O_GQ + hd * 192:O_GQ + (hd + 1) * 192, ts]), "kT": _c(hT[O_GK + hd * 192:O_GK + (hd + 1) * 192, ts]),
                         "lrT": _c(hT[O_GLR:O_GLR + 16, ts]), "Wg": _c(wgu[:, hd * 192:(hd + 1) * 192]), "bg": _c(bgt[hd * 192:(hd + 1) * 192, None]),
                         "v": chunked(hT[O_GV + hd * 384:O_GV + (hd + 1) * 384, ts].T), "gr": chunked(hT[O_GR + hd * 384:O_GR + (hd + 1) * 384, ts].T),
                         "gn": gng})
        nc = _get(("gla",), lambda: build_gla(8))
        res = _run(f"gla{l}", nc, maps)
        cat = np.zeros((8192, 4096), _BF)
        for c in range(8):
            b, hd = c // 4, c % 4
            cat[b * 4096:(b + 1) * 4096, hd * 384:(hd + 1) * 384] = res[c]["o"].transpose(1, 0, 2).reshape(4096, 384)
        del res, maps
        lamv = np.stack([np.asarray(inputs[n][l], np.float32) for n in ("lambda_q1", "lambda_k1", "lambda_q2", "lambda_k2")])
        dng = np.asarray(inputs["diff_norm_g"][l], np.float32)[None]
        dmaps, smaps, qtoks = [], [], []
        for c in range(8):
            b, r = c // 4, c % 4
            ts = slice(b * 4096, (b + 1) * 4096)
            qtok = np.concatenate([np.arange(g * 128, (g + 1) * 128) for g in _slot_blocks(r)])
            qtoks.append(qtok)
            dq_b, dk_b = dq[ts], dk[ts]
            dv_b = hT[O_DV:O_SQ, ts].T
            dmaps.append({"qT": _c(dq_b[qtok].reshape(1024, 10, 128).transpose(1, 2, 0)),
                          "kT": _c(dk_b.reshape(4096, 10, 128).transpose(1, 2, 0)),
                          "v": _c(dv_b.reshape(32, 128, 10, 128).transpose(2, 1, 0, 3)),
                          "msk": _masks(r, -1.0e9), "lamv": lamv, "g": dng})
            sq_b, sk_b, iq_b, ik_b = sq[ts], sk[ts], iq[ts], ik[ts]
            sv_b = hT[O_SV:O_IQ, ts].T
            iw_b = hT[O_IW:O_END, ts].T
            ikT = ik_b.T
            smaps.append({"sqT": _c(sq_b[qtok].reshape(1024, 10, 128).transpose(1, 2, 0)),
                          "skT": _c(sk_b.reshape(4096, 2, 128).transpose(1, 2, 0)),
                          "sv": _c(sv_b.reshape(32, 128, 2, 128).transpose(2, 1, 0, 3)),
                          "iqT": _c(iq_b[qtok].reshape(1024, 16, 2, 64).transpose(1, 2, 3, 0).reshape(16, 128, 1024)),
                          "ikT2": _c(np.concatenate([ikT, ikT], axis=0)),
                          "iw": _c(iw_b[qtok]), "msk": _masks(r, -1.0e30)})
        nc = _get(("diff", l), lambda: build_diff(8, 4, 10, LAMBDA_INIT[l]))
        res = _run(f"diff{l}", nc, dmaps)
        for c in range(8):
            b = c // 4
            cat[b * 4096 + qtoks[c], 1536:2816] = res[c]["o"].reshape(1024, 1280)
        nc = _get(("dsa",), lambda: build_dsa(8, 4))
        res = _run(f"dsa{l}", nc, smaps)
        for c in range(8):
            b = c // 4
            cat[b * 4096 + qtoks[c], 2816:4096] = res[c]["o"].reshape(1024, 1280)
        del res, dmaps, smaps, hT
        if dbg is not None:
            dbg[f"cat{l}"] = cat
        w_out = np.asarray(inputs["w_out"][l], np.float32)
        catT = _c(cat.T)
        nc = _get(("lin", 512, False), lambda: build_lin(4096, 8192, 512, False, BF16))
        res = _run(f"outproj{l}", nc, [{"xT": catT, "W": _c(w_out[:, c * 512:(c + 1) * 512])} for c in range(8)])
        mix = _c(np.concatenate([res[c]["YT"] for c in range(8)], axis=0).T)
        del res, catT, cat
        if dbg is not None:
            dbg[f"mix{l}"] = mix
        nc = _get(("ln", 1), lambda: build_ln(1024, 1, True, ALPHA))
        g1 = np.asarray(inputs["ln1_g"][l], np.float32)[None]
        b1 = np.asarray(inputs["ln1_b"][l], np.float32)[None]
        res = _run(f"ln1_{l}", nc, [{"x": _c(x[c * 1024:(c + 1) * 1024]), "a0": _c(mix[c * 1024:(c + 1) * 1024]), "g": g1, "b": b1} for c in range(8)])
        x1 = np.concatenate([res[c]["y"] for c in range(8)], axis=0)
        x1b_T = _c(np.concatenate([res[c]["yb"] for c in range(8)], axis=0).T)
        del res, mix
        if dbg is not None:
            dbg[f"x1_{l}"] = x1
        if l % 2 == 0:
            j = l // 2
            w1, w3, w2 = (np.asarray(inputs[n][j], np.float32) for n in ("ffn_w1", "ffn_w3", "ffn_w2"))
            maps = []
            for c in range(8):
                fs = slice(c * 1376, (c + 1) * 1376)
                W1 = np.zeros((4096, 1408), np.float32); W1[:, :1376] = w1[:, fs]
                W3 = np.zeros((4096, 1408), np.float32); W3[:, :1376] = w3[:, fs]
                W2 = np.zeros((1408, 4096), np.float32); W2[:1376] = w2[fs]
                maps.append({"xT": x1b_T, "W1": W1, "W3": W3, "W2": W2})
            nc = _get(("ffn", 1408, False), lambda: build_ffn(8192, 1408, False))
            res = _run(f"ffn{l}", nc, maps)
        else:
            j = l // 2
            nc = _get(("router",), lambda: build_router(1024))
            x1T = _c(x1.T)
            rw = np.asarray(inputs["router_w"][j], np.float32)
            rb = np.asarray(inputs["router_b"][j], np.float32)[None]
            res = _run(f"router{l}", nc, [{"xT": _c(x1T[:, c * 1024:(c + 1) * 1024]), "Wr": rw, "rb": rb} for c in range(8)])
            gates = np.concatenate([res[c]["gates"] for c in range(8)], axis=0)
            if dbg is not None:
                dbg[f"gates{l}"] = gates
            del x1T
            maps = [{"xT": x1b_T, "W1": np.asarray(inputs["moe_w1"][j][e], np.float32), "W3": np.asarray(inputs["moe_w3"][j][e], np.float32),
                     "W2": np.asarray(inputs["moe_w2"][j][e], np.float32), "gate": _c(gates[:, e][None])} for e in range(8)]
            nc = _get(("ffn", 4096, True), lambda: build_ffn(8192, 4096, True))
            res = _run(f"moe{l}", nc, maps)
        parts = [_c(res[c]["yT"].T) for c in range(8)]
        del res, maps
        if dbg is not None:
            dbg[f"parts{l}"] = parts
        nc = _get(("ln", 8), lambda: build_ln(1024, 8, True, ALPHA))
        g2 = np.asarray(inputs["ln2_g"][l], np.float32)[None]
        b2 = np.asarray(inputs["ln2_b"][l], np.float32)[None]
        maps = []
        for c in range(8):
            m = {"x": _c(x1[c * 1024:(c + 1) * 1024]), "g": g2, "b": b2}
            for i in range(8):
                m[f"a{i}"] = _c(parts[i][c * 1024:(c + 1) * 1024])
            maps.append(m)
        res = _run(f"ln2_{l}", nc, maps)
        x = np.concatenate([res[c]["y"] for c in range(8)], axis=0)
        xb_T = _c(np.concatenate([res[c]["yb"] for c in range(8)], axis=0).T)
        del res, maps, parts
        if dbg is not None:
            dbg[f"x2_{l}"] = x
    return x.reshape(2, 4096, 4096).astype(np.float32)


def kernel(**inputs):
    return _forward(inputs)
```

```python
import contextlib
import numpy as np
import concourse.bass as bass
import concourse.mybir as mybir

F32 = mybir.dt.float32
BF16 = mybir.dt.bfloat16
I32 = mybir.dt.int32
U32 = mybir.dt.uint32
AF = mybir.ActivationFunctionType
ALU = mybir.AluOpType
AX = mybir.AxisListType

ENGS = ("pe", "act", "dve", "pool", "sp")
NDMASEM = 6


class Sched:
    def __init__(self, nc):
        self.nc = nc
        self.ops = []
        self.lastw = {}
        self.readers = {}
        self.es = contextlib.ExitStack()
        self._nt = 0

    def sb(self, shape, dt, name=None):
        self._nt += 1
        return self.es.enter_context(self.nc.sbuf_tensor(name or f"t{self._nt}", list(shape), dt))

    def ps(self, shape, dt=F32, name=None):
        self._nt += 1
        return self.es.enter_context(self.nc.psum_tensor(name or f"p{self._nt}", list(shape), dt))

    def add(self, eng, fn, reads=(), writes=(), dma=False):
        idx = len(self.ops)
        deps = set()
        for k in reads:
            w = self.lastw.get(k)
            if w is not None:
                deps.add(w)
        for k in writes:
            w = self.lastw.get(k)
            if w is not None:
                deps.add(w)
            for r in self.readers.get(k, {}).values():
                deps.add(r)
        deps.discard(idx)
        self.ops.append((eng, fn, sorted(deps), dma))
        for k in writes:
            self.lastw[k] = idx
            self.readers[k] = {}
        rk = ("dma", idx) if dma else eng
        for k in reads:
            self.readers.setdefault(k, {})[rk] = idx
        return idx

    def pe(self, fn, reads=(), writes=()):
        return self.add("pe", fn, reads, writes)

    def act(self, fn, reads=(), writes=()):
        return self.add("act", fn, reads, writes)

    def dve(self, fn, reads=(), writes=()):
        return self.add("dve", fn, reads, writes)

    def pool(self, fn, reads=(), writes=()):
        return self.add("pool", fn, reads, writes)

    def dma(self, q, fn, reads=(), writes=()):
        return self.add(q, fn, reads, writes, dma=True)

    def emit(self, final_wait_ops=()):
        nc = self.nc
        ops = self.ops
        n = len(ops)
        needed = [False] * n
        for (eng, fn, deps, dma) in ops:
            for d in deps:
                de, _, _, ddma = ops[d]
                if (not ddma) and de == "pe" and eng == "pe" and not dma:
                    continue
                needed[d] = True
        for d in final_wait_ops:
            needed[d] = True
        sem = {e: self.es.enter_context(nc.semaphore(f"s_{e}")) for e in ENGS if e != "sp"}
        dsem = {e: [self.es.enter_context(nc.semaphore(f"d_{e}{i}")) for i in range(NDMASEM)]
                for e in ENGS if e != "pe"}
        sig = [None] * n
        cnt = {e: 0 for e in ENGS}
        dcnt = {e: 0 for e in ENGS}
        dma_prev = {}
        dq = {e: [] for e in ENGS}
        for i, (eng, fn, deps, dma) in enumerate(ops):
            if dma:
                k = dcnt[eng]
                dcnt[eng] += 1
                sig[i] = (dsem[eng][k % NDMASEM], 16 * (k // NDMASEM + 1))
                if k >= NDMASEM:
                    dma_prev[i] = dq[eng][k - NDMASEM]
                dq[eng].append(i)
            elif needed[i]:
                cnt[eng] += 1
                sig[i] = (sem[eng], cnt[eng])
        per = {e: [] for e in ENGS}
        for i, o in enumerate(ops):
            per[o[0]].append(i)

        def run(engname, e):
            waited = {}
            for i in per[engname]:
                _, fn, deps, dma = ops[i]
                dl = list(deps)
                if dma and i in dma_prev:
                    dl.append(dma_prev[i])
                for d in dl:
                    de, _, _, ddma = ops[d]
                    if (not ddma) and de == "pe" and engname == "pe" and not dma:
                        continue
                    s, v = sig[d]
                    key = id(s)
                    if waited.get(key, 0) >= v:
                        continue
                    e.wait_ge(s, v)
                    waited[key] = v
                ins = fn(e)
                if dma:
                    s, v = sig[i]
                    ins.then_inc(s, 16)
                elif needed[i]:
                    s, v = sig[i]
                    ins.then_inc(s, 1)
            if engname == self.final_eng:
                for d in final_wait_ops:
                    s, v = sig[d]
                    e.wait_ge(s, v)

        self.final_eng = "sp"
        with nc.Block() as block:
            if per["sp"] or final_wait_ops:
                @block.sync
                def _(e):
                    run("sp", e)
            if per["pe"]:
                @block.tensor
                def _(e):
                    run("pe", e)
            if per["act"]:
                @block.scalar
                def _(e):
                    run("act", e)
            if per["dve"]:
                @block.vector
                def _(e):
                    run("dve", e)
            if per["pool"]:
                @block.gpsimd
                def _(e):
                    run("pool", e)
        self.es.close()


def _cast_eng(S, i):
    return ("dve", "pool")[i % 2]


def build_lin(K, T, Nc, x_f32, out_dt):
    nc = bass.Bass("TRN2", target_bir_lowering=False)
    KC, NT, TB = K // 128, Nc // 128, 512
    NB = T // TB
    xT = nc.dram_tensor("xT", [K, T], F32 if x_f32 else BF16, kind="ExternalInput").ap()
    W = nc.dram_tensor("W", [K, Nc], F32, kind="ExternalInput").ap()
    YT = nc.dram_tensor("YT", [Nc, T], out_dt, kind="ExternalOutput").ap()
    S = Sched(nc)
    Wb = S.sb([128, KC, Nc], BF16, "Wb")
    stg = [S.sb([128, Nc], F32, f"stg{i}") for i in range(2)]
    xb = [S.sb([128, KC, TB], BF16, f"xb{i}") for i in range(2)]
    ob = [S.sb([128, TB], out_dt, f"ob{i}") for i in range(4)]
    pb = [S.ps([128, TB], F32, f"pb{i}") for i in range(8)]
    Wv = W.rearrange("(c p) n -> p c n", p=128)
    xv = xT.rearrange("(c p) t -> p c t", p=128)
    for kc in range(KC):
        s = stg[kc % 2]
        S.dma("sp", lambda e, s=s, kc=kc: e.dma_start(out=s[:], in_=Wv[:, kc, :]), writes=[("stg", kc % 2)])
        S.add(_cast_eng(S, kc), lambda e, s=s, kc=kc: e.tensor_copy(out=Wb[:, kc, :], in_=s[:]),
              reads=[("stg", kc % 2)], writes=[("Wb", kc)])
    outs = []
    it = 0
    for tb in range(NB):
        xq = "pool" if x_f32 else "sp"
        b = xb[tb % 2]
        S.dma(xq, lambda e, b=b, tb=tb: e.dma_start(out=b[:], in_=xv[:, :, tb * TB:(tb + 1) * TB]),
              writes=[("xb", tb % 2)])
        for nt in range(NT):
            p = pb[it % 8]
            o = ob[it % 4]
            for kc in range(KC):
                S.pe(lambda e, p=p, b=b, kc=kc, nt=nt: e.matmul(p[:], lhsT=Wb[:, kc, nt * 128:(nt + 1) * 128],
                                                               rhs=b[:, kc, :], start=(kc == 0), stop=(kc == KC - 1)),
                     reads=[("xb", tb % 2), ("Wb", kc)], writes=[("pb", it % 8)])
            if it % 2 == 0:
                S.dve(lambda e, p=p, o=o: e.tensor_copy(out=o[:], in_=p[:]), reads=[("pb", it % 8)], writes=[("ob", it % 4)])
            else:
                S.act(lambda e, p=p, o=o: e.copy(out=o[:], in_=p[:]), reads=[("pb", it % 8)], writes=[("ob", it % 4)])
            outs.append(S.dma("sp", lambda e, o=o, nt=nt, tb=tb: e.dma_start(
                out=YT[nt * 128:(nt + 1) * 128, tb * TB:(tb + 1) * TB], in_=o[:]), reads=[("ob", it % 4)]))
            it += 1
    S.emit(final_wait_ops=outs[-8:])
    return nc


def build_ffn(T, F, gated):
    nc = bass.Bass("TRN2", target_bir_lowering=False)
    D = 4096
    KC, FT, SUBW, NSUB, NO = D // 128, F // 128, 512, 2, D // 128
    TB = SUBW * NSUB
    NB = T // TB
    xT = nc.dram_tensor("xT", [D, T], BF16, kind="ExternalInput").ap()
    W1 = nc.dram_tensor("W1", [D, F], F32, kind="ExternalInput").ap()
    W3 = nc.dram_tensor("W3", [D, F], F32, kind="ExternalInput").ap()
    W2 = nc.dram_tensor("W2", [F, D], F32, kind="ExternalInput").ap()
    if gated:
        gate = nc.dram_tensor("gate", [1, T], F32, kind="ExternalInput").ap()
    yT = nc.dram_tensor("yT", [D, T], BF16, kind="ExternalOutput").ap()
    W1s = nc.dram_tensor("W1s", [FT, 128, KC * 128], BF16).ap()
    W3s = nc.dram_tensor("W3s", [FT, 128, KC * 128], BF16).ap()
    W2s = nc.dram_tensor("W2s", [NO, 128, FT * 128], BF16).ap()
    S = Sched(nc)
    HW = 2048 if max(F, D) > 2048 else max(F, D)
    stg = S.sb([128, HW], F32, "stg")
    stb = [S.sb([128, HW], BF16, f"stb{i}") for i in range(2)]
    xb = S.sb([128, KC, TB], BF16, "xb")
    h1 = S.sb([128, FT, TB], BF16, "h1")
    w1t = [S.sb([128, KC * 128], BF16, f"w1t{i}") for i in range(2)]
    w3t = [S.sb([128, KC * 128], BF16, f"w3t{i}") for i in range(2)]
    w2t = [S.sb([128, FT * 128], BF16, f"w2t{i}") for i in range(2)]
    sg = [S.sb([128, SUBW], F32, f"sg{i}") for i in range(2)]
    ob = [S.sb([128, SUBW], BF16, f"ob{i}") for i in range(4)]
    gb = S.sb([128, TB], F32, "gb") if gated else None
    pb = [S.ps([128, SUBW], F32, f"pb{i}") for i in range(8)]
    n = 0
    for (Wsrc, Wdst, rows, cols, cch) in ((W1, W1s, D, F, KC), (W3, W3s, D, F, KC), (W2, W2s, F, D, FT)):
        Wv = Wsrc.rearrange("(c p) n -> p c n", p=128)
        Dv = Wdst.rearrange("n p (c j) -> p c n j", c=cch)
        for c in range(rows // 128):
            for c0 in range(0, cols, HW):
                cw = min(HW, cols - c0)
                sb_ = stb[n % 2]
                S.dma("sp", lambda e, Wv=Wv, c=c, c0=c0, cw=cw: e.dma_start(out=stg[:, 0:cw], in_=Wv[:, c, c0:c0 + cw]), writes=["stg"])
                S.add(_cast_eng(S, n), lambda e, sb_=sb_, cw=cw: e.tensor_copy(out=sb_[:, 0:cw], in_=stg[:, 0:cw]),
                      reads=["stg"], writes=[("stb", n % 2)])
                S.dma("act", lambda e, sb_=sb_, Dv=Dv, c=c, c0=c0, cw=cw: e.dma_start(
                    out=Dv[:, c, c0 // 128:(c0 + cw) // 128, :], in_=sb_[:, 0:cw].rearrange("p (n j) -> p n j", j=128)),
                    reads=[("stb", n % 2)], writes=[(id(Wdst.tensor), "scr")])
                n += 1
    keyW1, keyW3, keyW2 = (id(W1s.tensor), "scr"), (id(W3s.tensor), "scr"), (id(W2s.tensor), "scr")
    xv = xT.rearrange("(c p) t -> p c t", p=128)
    outs = []
    ip = iw = io = i2 = isg = 0
    for tb in range(NB):
        t0 = tb * TB
        for kh in range(2):
            ks = slice(kh * KC // 2, (kh + 1) * KC // 2)
            S.dma("sp", lambda e, ks=ks, t0=t0: e.dma_start(out=xb[:, ks, :], in_=xv[:, ks, t0:t0 + TB]), writes=[("xb", kh)])
        if gated:
            S.dma("sp", lambda e, t0=t0: e.dma_start(out=gb[:], in_=gate[0, t0:t0 + TB].partition_broadcast(128)), writes=["gb"])
        for ft in range(FT):
            a1, a3 = w1t[iw % 2], w3t[iw % 2]
            S.dma("sp", lambda e, a1=a1, ft=ft: e.dma_start(out=a1[:], in_=W1s[ft]), reads=[keyW1], writes=[("w1t", iw % 2)])
            S.dma("sp", lambda e, a3=a3, ft=ft: e.dma_start(out=a3[:], in_=W3s[ft]), reads=[keyW3], writes=[("w3t", iw % 2)])
            for su in range(NSUB):
                cs = slice(su * SUBW, (su + 1) * SUBW)
                pa, pg = pb[ip % 8], pb[(ip + 1) % 8]
                for kc in range(KC):
                    S.pe(lambda e, pa=pa, a1=a1, kc=kc, cs=cs: e.matmul(pa[:], lhsT=a1[:, kc * 128:(kc + 1) * 128], rhs=xb[:, kc, cs],
                                                                       start=(kc == 0), stop=(kc == KC - 1)),
                         reads=[("w1t", iw % 2), ("xb", kc * 2 // KC)], writes=[("pb", ip % 8)])
                for kc in range(KC):
                    S.pe(lambda e, pg=pg, a3=a3, kc=kc, cs=cs: e.matmul(pg[:], lhsT=a3[:, kc * 128:(kc + 1) * 128], rhs=xb[:, kc, cs],
                                                                       start=(kc == 0), stop=(kc == KC - 1)),
                         reads=[("w3t", iw % 2), ("xb", kc * 2 // KC)], writes=[("pb", (ip + 1) % 8)])
                s_ = sg[isg % 2]
                S.act(lambda e, s_=s_, pa=pa: e.activation(out=s_[:], in_=pa[:], func=AF.Silu),
                      reads=[("pb", ip % 8)], writes=[("sg", isg % 2)])
                S.dve(lambda e, s_=s_, pg=pg, ft=ft, cs=cs: e.tensor_tensor(out=h1[:, ft, cs], in0=s_[:], in1=pg[:], op=ALU.mult),
                      reads=[("sg", isg % 2), ("pb", (ip + 1) % 8)], writes=[("h1", ft, su)])
                ip += 2
                isg += 1
            iw += 1
        for no in range(NO):
            a2 = w2t[i2 % 2]
            S.dma("sp", lambda e, a2=a2, no=no: e.dma_start(out=a2[:], in_=W2s[no]), reads=[keyW2], writes=[("w2t", i2 % 2)])
            for su in range(NSUB):
                cs = slice(su * SUBW, (su + 1) * SUBW)
                po = pb[ip % 8]
                for fc in range(FT):
                    S.pe(lambda e, po=po, a2=a2, fc=fc, cs=cs: e.matmul(po[:], lhsT=a2[:, fc * 128:(fc + 1) * 128], rhs=h1[:, fc, cs],
                                                                       start=(fc == 0), stop=(fc == FT - 1)),
                         reads=[("w2t", i2 % 2), ("h1", fc, su)], writes=[("pb", ip % 8)])
                o = ob[io % 4]
                if gated:
                    S.dve(lambda e, o=o, po=po, cs=cs: e.tensor_tensor(out=o[:], in0=po[:], in1=gb[:, cs], op=ALU.mult),
                          reads=[("pb", ip % 8), "gb"], writes=[("ob", io % 4)])
                elif io % 2 == 0:
                    S.dve(lambda e, o=o, po=po: e.tensor_copy(out=o[:], in_=po[:]), reads=[("pb", ip % 8)], writes=[("ob", io % 4)])
                else:
                    S.act(lambda e, o=o, po=po: e.copy(out=o[:], in_=po[:]), reads=[("pb", ip % 8)], writes=[("ob", io % 4)])
                outs.append(S.dma("sp", lambda e, o=o, no=no, t0=t0, su=su: e.dma_start(
                    out=yT[no * 128:(no + 1) * 128, t0 + su * SUBW:t0 + (su + 1) * SUBW], in_=o[:]), reads=[("ob", io % 4)]))
                ip += 1
                io += 1
            i2 += 1
    S.emit(final_wait_ops=outs[-8:])
    return nc


def build_ln(Tc, n_add, add_bf16, alpha, eps=1e-5):
    nc = bass.Bass("TRN2", target_bir_lowering=False)
    D = 4096
    NTL = Tc // 128
    adt = BF16 if add_bf16 else F32
    x = nc.dram_tensor("x", [Tc, D], F32, kind="ExternalInput").ap()
    adds = [nc.dram_tensor(f"a{i}", [Tc, D], adt, kind="ExternalInput").ap() for i in range(n_add)]
    g = nc.dram_tensor("g", [1, D], F32, kind="ExternalInput").ap()
    bb = nc.dram_tensor("b", [1, D], F32, kind="ExternalInput").ap()
    y = nc.dram_tensor("y", [Tc, D], F32, kind="ExternalOutput").ap()
    yb = nc.dram_tensor("yb", [Tc, D], BF16, kind="ExternalOutput").ap()
    S = Sched(nc)
    gt = S.sb([128, D], F32, "gt")
    bt = S.sb([128, D], F32, "bt")
    xt = [S.sb([128, D], F32, f"xt{i}") for i in range(2)]
    at = [S.sb([128, D], adt, f"at{i}") for i in range(2)]
    yt = [S.sb([128, D], F32, f"yt{i}") for i in range(2)]
    ybt = [S.sb([128, D], BF16, f"ybt{i}") for i in range(2)]
    st = [S.sb([128, 8, 6], F32, f"st{i}") for i in range(2)]
    mv = [S.sb([128, 8], F32, f"mv{i}") for i in range(2)]
    S.dma("sp", lambda e: e.dma_start(out=gt[:], in_=g[0, :].partition_broadcast(128)), writes=["gt"])
    S.dma("sp", lambda e: e.dma_start(out=bt[:], in_=bb[0, :].partition_broadcast(128)), writes=["bt"])
    outs = []
    ia = 0
    for t in range(NTL):
        j = t % 2
        X, Y, YB, ST, MV = xt[j], yt[j], ybt[j], st[j], mv[j]
        rs = slice(t * 128, (t + 1) * 128)
        S.dma("sp", lambda e, X=X, rs=rs: e.dma_start(out=X[:], in_=x[rs, :]), writes=[("xt", j)])
        for i in range(n_add):
            A = at[ia % 2]
            S.dma("act" if i % 2 else "sp", lambda e, A=A, rs=rs, i=i: e.dma_start(out=A[:], in_=adds[i][rs, :]), writes=[("at", ia % 2)])
            if i == 0:
                S.dve(lambda e, X=X, A=A: e.scalar_tensor_tensor(out=X[:], in0=X[:], scalar=float(alpha), in1=A[:],
                                                                 op0=ALU.mult, op1=ALU.add),
                      reads=[("at", ia % 2), ("xt", j)], writes=[("xt", j)])
            else:
                S.dve(lambda e, X=X, A=A: e.tensor_tensor(out=X[:], in0=X[:], in1=A[:], op=ALU.add),
                      reads=[("at", ia % 2), ("xt", j)], writes=[("xt", j)])
            ia += 1
        for c in range(8):
            S.dve(lambda e, X=X, ST=ST, c=c: e.bn_stats(out=ST[:, c, :], in_=X[:, c * 512:(c + 1) * 512]),
                  reads=[("xt", j)], writes=[("st", j, c)])
        S.dve(lambda e, ST=ST, MV=MV: e.bn_aggr(out=MV[:, 0:2], in_=ST[:].rearrange("p c s -> p (c s)")),
              reads=[("st", j, c) for c in range(8)], writes=[("mv", j)])
        S.dve(lambda e, MV=MV: e.tensor_scalar(out=MV[:, 2:3], in0=MV[:, 1:2], scalar1=float(eps), scalar2=None, op0=ALU.add),
              reads=[("mv", j)], writes=[("mv", j)])
        S.act(lambda e, MV=MV: e.activation(out=MV[:, 3:4], in_=MV[:, 2:3], func=AF.Sqrt), reads=[("mv", j)], writes=[("mv", j)])
        S.dve(lambda e, MV=MV: e.reciprocal(out=MV[:, 4:5], in_=MV[:, 3:4]), reads=[("mv", j)], writes=[("mv", j)])
        S.dve(lambda e, MV=MV: e.scalar_tensor_tensor(out=MV[:, 5:6], in0=MV[:, 0:1], scalar=-1.0, in1=MV[:, 4:5],
                                                      op0=ALU.mult, op1=ALU.mult), reads=[("mv", j)], writes=[("mv", j)])
        S.act(lambda e, X=X, Y=Y, MV=MV: e.activation(out=Y[:], in_=X[:], func=AF.Identity, bias=MV[:, 5:6], scale=MV[:, 4:5]),
              reads=[("xt", j), ("mv", j)], writes=[("yt", j)])
        S.dve(lambda e, Y=Y: e.tensor_tensor(out=Y[:], in0=Y[:], in1=gt[:], op=ALU.mult), reads=[("yt", j), "gt"], writes=[("yt", j)])
        S.dve(lambda e, Y=Y: e.tensor_tensor(out=Y[:], in0=Y[:], in1=bt[:], op=ALU.add), reads=[("yt", j), "bt"], writes=[("yt", j)])
        S.pool(lambda e, Y=Y, YB=YB: e.tensor_copy(out=YB[:], in_=Y[:]), reads=[("yt", j)], writes=[("ybt", j)])
        outs.append(S.dma("sp", lambda e, Y=Y, rs=rs: e.dma_start(out=y[rs, :], in_=Y[:]), reads=[("yt", j)]))
        outs.append(S.dma("sp", lambda e, YB=YB, rs=rs: e.dma_start(out=yb[rs, :], in_=YB[:]), reads=[("ybt", j)]))
    S.emit(final_wait_ops=outs[-6:])
    return nc


def build_router(Tc):
    nc = bass.Bass("TRN2", target_bir_lowering=False)
    D, E = 4096, 8
    KC = D // 128
    NTL = Tc // 128
    xT = nc.dram_tensor("xT", [D, Tc], F32, kind="ExternalInput").ap()
    Wr = nc.dram_tensor("Wr", [D, E], F32, kind="ExternalInput").ap()
    rb = nc.dram_tensor("rb", [1, E], F32, kind="ExternalInput").ap()
    gates = nc.dram_tensor("gates", [Tc, E], F32, kind="ExternalOutput").ap()
    S = Sched(nc)
    wt = S.sb([128, KC, E], F32, "wt")
    rbt = S.sb([128, E], F32, "rbt")
    xt = [S.sb([128, KC, 128], F32, f"xt{i}") for i in range(2)]
    wk = [S.sb([128, 48], F32, f"wk{i}") for i in range(2)]
    pp = [S.ps([128, E], F32, f"pp{i}") for i in range(2)]
    S.dma("sp", lambda e: e.dma_start(out=wt[:], in_=Wr.rearrange("(c p) n -> p c n", p=128)), writes=["wt"])
    S.dma("sp", lambda e: e.dma_start(out=rbt[:], in_=rb[0, :].partition_broadcast(128)), writes=["rbt"])
    xv = xT.rearrange("(c p) t -> p c t", p=128)
    outs = []
    for t in range(NTL):
        j = t % 2
        X, P, Wk = xt[j], pp[j], wk[j]
        S.dma("sp", lambda e, X=X, t=t: e.dma_start(out=X[:], in_=xv[:, :, t * 128:(t + 1) * 128]), writes=[("xt", j)])
        for kc in range(KC):
            S.pe(lambda e, X=X, P=P, kc=kc: e.matmul(P[:], lhsT=X[:, kc, :], rhs=wt[:, kc, :], start=(kc == 0), stop=(kc == KC - 1)),
                 reads=[("xt", j), "wt"], writes=[("pp", j)])
        lg, m8, nm1, msk, ex, ssum = Wk[:, 0:8], Wk[:, 8:16], Wk[:, 16:17], Wk[:, 24:32], Wk[:, 32:40], Wk[:, 17:18]
        K = ("wk", j)
        S.dve(lambda e, P=P, lg=lg: e.tensor_tensor(out=lg, in0=P[:], in1=rbt[:], op=ALU.add), reads=[("pp", j), "rbt"], writes=[K])
        S.dve(lambda e, lg=lg, m8=m8: e.max(out=m8, in_=lg), reads=[K], writes=[K])
        S.dve(lambda e, m8=m8, nm1=nm1: e.tensor_scalar(out=nm1, in0=m8[:, 0:1], scalar1=-1.0, scalar2=None, op0=ALU.mult), reads=[K], writes=[K])
        S.dve(lambda e, lg=lg, m8=m8, msk=msk: e.tensor_scalar(out=msk, in0=lg, scalar1=m8[:, 1:2], scalar2=None, op0=ALU.is_ge),
              reads=[K], writes=[K])
        S.act(lambda e, lg=lg, ex=ex, nm1=nm1: e.activation(out=ex, in_=lg, func=AF.Exp, bias=nm1, scale=1.0), reads=[K], writes=[K])
        S.dve(lambda e, ex=ex, msk=msk: e.tensor_tensor(out=ex, in0=ex, in1=msk, op=ALU.mult), reads=[K], writes=[K])
        S.dve(lambda e, ex=ex, ssum=ssum: e.reduce_sum(out=ssum, in_=ex, axis=AX.X), reads=[K], writes=[K])
        S.dve(lambda e, ssum=ssum: e.reciprocal(out=ssum, in_=ssum), reads=[K], writes=[K])
        S.dve(lambda e, ex=ex, ssum=ssum: e.tensor_scalar(out=ex, in0=ex, scalar1=ssum, scalar2=None, op0=ALU.mult), reads=[K], writes=[K])
        outs.append(S.dma("sp", lambda e, ex=ex, t=t: e.dma_start(out=gates[t * 128:(t + 1) * 128, :], in_=ex), reads=[K]))
    S.emit(final_wait_ops=outs[-4:])
    return nc

import math
C1_2PI = 6.28125
C2_2PI = 2.0 * math.pi - 6.28125
MAGIC = 12582912.0


def build_rope(Tc):
    nc = bass.Bass("TRN2", target_bir_lowering=False)
    N16, N32 = 73, 12
    NTL = Tc // 128
    pos = nc.dram_tensor("pos", [Tc, 1], I32, kind="ExternalInput").ap()
    invf = nc.dram_tensor("invf", [1, 16], F32, kind="ExternalInput").ap()
    h16 = nc.dram_tensor("h16", [Tc, N16 * 64], BF16, kind="ExternalInput").ap()
    h32 = nc.dram_tensor("h32", [Tc, N32 * 128], BF16, kind="ExternalInput").ap()
    o16 = nc.dram_tensor("o16", [Tc, N16 * 64], BF16, kind="ExternalOutput").ap()
    o32 = nc.dram_tensor("o32", [Tc, N32 * 128], BF16, kind="ExternalOutput").ap()
    S = Sched(nc)
    ivt = S.sb([128, 16], F32, "ivt")
    S.dma("sp", lambda e: e.dma_start(out=ivt[:], in_=invf[0, :].partition_broadcast(128)), writes=["ivt"])
    A = [S.sb([128, N16 * 64], BF16, f"A{i}") for i in range(2)]
    Bt = [S.sb([128, N32 * 128], BF16, f"B{i}") for i in range(2)]
    pt = [S.sb([128, 1], I32, f"pt{i}") for i in range(2)]
    wk = [S.sb([128, 8, 16], F32, f"wk{i}") for i in range(2)]
    tmp = [[S.sb([128, N16 * 8], F32, f"tmp{i}_{k}") for k in range(4)] for i in range(2)]
    outs = []
    for t in range(NTL):
        j = t % 2
        rs = slice(t * 128, (t + 1) * 128)
        a, b, p, w, tm = A[j], Bt[j], pt[j], wk[j], tmp[j]
        S.dma("sp", lambda e, p=p, rs=rs: e.dma_start(out=p[:], in_=pos[rs, :]), writes=[("pt", j)])
        S.dma("sp", lambda e, a=a, rs=rs: e.dma_start(out=a[:], in_=h16[rs, :]), writes=[("A", j)])
        S.dma("act", lambda e, b=b, rs=rs: e.dma_start(out=b[:], in_=h32[rs, :]), writes=[("B", j)])
        K = ("wk", j)
        pf, ang, u, n_, r, ab, sn, cs = w[:, 0, 0:1], w[:, 1, :], w[:, 2, :], w[:, 3, :], w[:, 4, :], w[:, 5, :], w[:, 6, :], w[:, 7, :]
        S.dve(lambda e, pf=pf, p=p: e.tensor_copy(out=pf, in_=p[:]), reads=[("pt", j)], writes=[K])
        S.dve(lambda e, ang=ang, pf=pf: e.tensor_scalar(out=ang, in0=ivt[:], scalar1=pf, scalar2=None, op0=ALU.mult), reads=[K, "ivt"], writes=[K])
        S.dve(lambda e, u=u, ang=ang: e.tensor_scalar(out=u, in0=ang, scalar1=float(1.0 / (2 * math.pi)), scalar2=MAGIC, op0=ALU.mult, op1=ALU.add), reads=[K], writes=[K])
        S.dve(lambda e, u=u, n_=n_: e.tensor_scalar(out=n_, in0=u, scalar1=-MAGIC, scalar2=None, op0=ALU.add), reads=[K], writes=[K])
        S.dve(lambda e, r=r, n_=n_, ang=ang: e.scalar_tensor_tensor(out=r, in0=n_, scalar=-C1_2PI, in1=ang, op0=ALU.mult, op1=ALU.add), reads=[K], writes=[K])
        S.dve(lambda e, r=r, n_=n_: e.scalar_tensor_tensor(out=r, in0=n_, scalar=-C2_2PI, in1=r, op0=ALU.mult, op1=ALU.add), reads=[K], writes=[K])
        S.dve(lambda e, r=r: e.tensor_scalar(out=r, in0=r, scalar1=float(math.pi), scalar2=float(-math.pi), op0=ALU.min, op1=ALU.max), reads=[K], writes=[K])
        S.dve(lambda e, r=r, ab=ab: e.scalar_tensor_tensor(out=ab, in0=r, scalar=-1.0, in1=r, op0=ALU.mult, op1=ALU.max), reads=[K], writes=[K])
        S.dve(lambda e, ab=ab: e.tensor_scalar(out=ab, in0=ab, scalar1=-1.0, scalar2=float(math.pi / 2), op0=ALU.mult, op1=ALU.add), reads=[K], writes=[K])
        S.act(lambda e, sn=sn, r=r: e.activation(out=sn, in_=r, func=AF.Sin), reads=[K], writes=[K])
        S.act(lambda e, cs=cs, ab=ab: e.activation(out=cs, in_=ab, func=AF.Sin), reads=[K], writes=[K])
        for (buf, key, nh, hd, half, step) in ((a, ("A", j), N16, 64, 8, 2), (b, ("B", j), N32, 128, 16, 1)):
            hv = buf[:].rearrange("p (n d) -> p n d", d=hd)
            x1, x2 = hv[:, :, 0:half], hv[:, :, half:2 * half]
            cb = w[:, 7, 0:16:step].unsqueeze(1).broadcast_to([128, nh, half])
            sb_ = w[:, 6, 0:16:step].unsqueeze(1).broadcast_to([128, nh, half])
            T = [tm[k][:, 0:nh * half].rearrange("p (n d) -> p n d", d=half) for k in range(4)]
            TK = [("tmp", j, k) for k in range(4)]
            S.dve(lambda e, T=T, x1=x1, cb=cb: e.tensor_tensor(out=T[0], in0=x1, in1=cb, op=ALU.mult), reads=[key, K], writes=[TK[0]])
            S.dve(lambda e, T=T, x2=x2, sb_=sb_: e.tensor_tensor(out=T[1], in0=x2, in1=sb_, op=ALU.mult), reads=[key, K], writes=[TK[1]])
            S.dve(lambda e, T=T, x2=x2, cb=cb: e.tensor_tensor(out=T[2], in0=x2, in1=cb, op=ALU.mult), reads=[key, K], writes=[TK[2]])
            S.dve(lambda e, T=T, x1=x1, sb_=sb_: e.tensor_tensor(out=T[3], in0=x1, in1=sb_, op=ALU.mult), reads=[key, K], writes=[TK[3]])
            S.dve(lambda e, T=T, x1=x1: e.tensor_tensor(out=x1, in0=T[0], in1=T[1], op=ALU.subtract), reads=[TK[0], TK[1]], writes=[key])
            S.dve(lambda e, T=T, x2=x2: e.tensor_tensor(out=x2, in0=T[2], in1=T[3], op=ALU.add), reads=[TK[2], TK[3]], writes=[key])
        outs.append(S.dma("sp", lambda e, a=a, rs=rs: e.dma_start(out=o16[rs, :], in_=a[:]), reads=[("A", j)]))
        outs.append(S.dma("sp", lambda e, b=b, rs=rs: e.dma_start(out=o32[rs, :], in_=b[:]), reads=[("B", j)]))
    S.emit(final_wait_ops=outs[-6:])
    return nc


def build_gla(NSEG=8, eps=1e-5):
    nc = bass.Bass("TRN2", target_bir_lowering=False)
    DK, DV, C, SEG = 192, 384, 64, 512
    CPS = SEG // C
    Ttot = NSEG * SEG
    NCH = Ttot // C
    qT_d = nc.dram_tensor("qT", [DK, Ttot], BF16, kind="ExternalInput").ap()
    kT_d = nc.dram_tensor("kT", [DK, Ttot], BF16, kind="ExternalInput").ap()
    lrT_d = nc.dram_tensor("lrT", [16, Ttot], BF16, kind="ExternalInput").ap()
    Wg_d = nc.dram_tensor("Wg", [16, DK], F32, kind="ExternalInput").ap()
    bg_d = nc.dram_tensor("bg", [DK, 1], F32, kind="ExternalInput").ap()
    v_d = nc.dram_tensor("v", [C, NCH, DV], BF16, kind="ExternalInput").ap()
    gr_d = nc.dram_tensor("gr", [C, NCH, DV], BF16, kind="ExternalInput").ap()
    gn_d = nc.dram_tensor("gn", [1, DV], F32, kind="ExternalInput").ap()
    o_d = nc.dram_tensor("o", [C, NCH, DV], BF16, kind="ExternalOutput").ap()
    S = Sched(nc)
    DCS = ((0, 128), (128, 64))
    ones = S.sb([128, 128], F32, "ones")
    ident_f = S.sb([128, 128], F32, "ident_f")
    ident = S.sb([128, 128], BF16, "ident")
    umask = S.sb([64, 64], F32, "umask")
    rmask = S.sb([128, SEG], F32, "rmask")
    Wg32 = S.sb([16, DK], F32, "Wg32")
    Wg = S.sb([16, DK], BF16, "Wgb")
    nbg = [S.sb([sz, 1], F32, f"nbg{i}") for i, (o_, sz) in enumerate(DCS)]
    gn = S.sb([64, DV], F32, "gnb")
    S.pool(lambda e: e.memset(ones[:], 1.0), writes=["ones"])
    S.pool(lambda e: e.affine_select(out=ident_f[:], in_=ones[:], pattern=[[1, 128]], compare_op=ALU.is_equal, fill=0.0,
                                     base=0, channel_multiplier=-1), reads=["ones"], writes=["ident_f"])
    S.dve(lambda e: e.tensor_copy(out=ident[:], in_=ident_f[:]), reads=["ident_f"], writes=["ident"])
    S.pool(lambda e: e.affine_select(out=umask[:], in_=ones[0:64, 0:64], pattern=[[1, 64]], compare_op=ALU.is_ge, fill=0.0,
                                     base=0, channel_multiplier=-1), reads=["ones"], writes=["umask"])
    S.pool(lambda e: e.memset(rmask[:], 1.0), writes=["rmask"])
    S.pool(lambda e: e.memset(rmask[:, 0:SEG:C], 0.0), writes=["rmask"])
    S.dma("sp", lambda e: e.dma_start(out=Wg32[:], in_=Wg_d), writes=["Wg32"])
    S.dve(lambda e: e.tensor_copy(out=Wg[:], in_=Wg32[:]), reads=["Wg32"], writes=["Wg"])
    for i, (o_, sz) in enumerate(DCS):
        S.dma("sp", lambda e, i=i, o_=o_, sz=sz: e.dma_start(out=nbg[i][:], in_=bg_d[o_:o_ + sz, :]), writes=[("nbg", i)])
        S.dve(lambda e, i=i: e.tensor_scalar(out=nbg[i][:], in0=nbg[i][:], scalar1=-1.0, scalar2=None, op0=ALU.mult),
              reads=[("nbg", i)], writes=[("nbg", i)])
    S.dma("sp", lambda e: e.dma_start(out=gn[:], in_=gn_d[0, :].partition_broadcast(64)), writes=["gn"])
    st32 = [S.sb([sz, DV], F32, f"st32_{i}") for i, (o_, sz) in enumerate(DCS)]
    stbf = [S.sb([sz, DV], BF16, f"stbf_{i}") for i, (o_, sz) in enumerate(DCS)]
    def mk(shape, dt, nm):
        return [S.sb(shape, dt, f"{nm}{j}") for j in range(2)]
    qr = [mk([sz, SEG], BF16, f"qr{i}_") for i, (o_, sz) in enumerate(DCS)]
    kr = [mk([sz, SEG], BF16, f"kr{i}_") for i, (o_, sz) in enumerate(DCS)]
    qt = [mk([sz, SEG], BF16, f"qt{i}_") for i, (o_, sz) in enumerate(DCS)]
    kt = [mk([sz, SEG], BF16, f"kt{i}_") for i, (o_, sz) in enumerate(DCS)]
    kh = [mk([sz, SEG], BF16, f"kh{i}_") for i, (o_, sz) in enumerate(DCS)]
    gdec = [mk([sz, CPS], F32, f"gdec{i}_") for i, (o_, sz) in enumerate(DCS)]
    nbl = [mk([sz, CPS], F32, f"nbl{i}_") for i, (o_, sz) in enumerate(DCS)]
    lr = mk([16, SEG], BF16, "lr")
    vv = mk([C, CPS, DV], BF16, "vv")
    grr = mk([C, CPS, DV], BF16, "grr")
    sgr = mk([C, CPS, DV], BF16, "sgr")
    khat = mk([C, CPS, DK], BF16, "khat")
    sTb = mk([C, CPS, C], BF16, "sTb")
    ob = mk([C, CPS, DV], BF16, "ob")
    t_e = [S.sb([sz, SEG], F32, f"t_e{i}") for i, (o_, sz) in enumerate(DCS)]
    t_cs = [S.sb([sz, SEG], F32, f"t_cs{i}") for i, (o_, sz) in enumerate(DCS)]
    t_x = [S.sb([sz, SEG], F32, f"t_x{i}") for i, (o_, sz) in enumerate(DCS)]
    rs_t = [S.sb([C, 8], F32, f"rs{i}") for i in range(2)]
    junk = S.sb([C, DV], F32, "junk")
    pz = S.ps([128, SEG], F32, "pz")
    pS = S.ps([C, 512], F32, "pS")
    pT = S.ps([128, 512], BF16, "pT")
    pO = [S.ps([C, 512], F32, f"pO{i}") for i in range(2)]
    pD = [S.ps([128, 512], F32, f"pD{i}") for i in range(2)]
    outs = []
    INV16 = 1.0 / 16.0
    for sg in range(NSEG):
        j = sg % 2
        ts = slice(sg * SEG, (sg + 1) * SEG)
        cs0 = sg * CPS
        S.dma("sp", lambda e, j=j, ts=ts: e.dma_start(out=lr[j][:], in_=lrT_d[:, ts]), writes=[("lr", j)])
        for i, (o_, sz) in enumerate(DCS):
            S.dma("sp", lambda e, i=i, j=j, o_=o_, sz=sz, ts=ts: e.dma_start(out=qr[i][j][:], in_=qT_d[o_:o_ + sz, ts]), writes=[("qr", i, j)])
            S.dma("sp", lambda e, i=i, j=j, o_=o_, sz=sz, ts=ts: e.dma_start(out=kr[i][j][:], in_=kT_d[o_:o_ + sz, ts]), writes=[("kr", i, j)])
        S.dma("act", lambda e, j=j, cs0=cs0: e.dma_start(out=vv[j][:], in_=v_d[:, cs0:cs0 + CPS, :]), writes=[("vv", j)])
        S.dma("act", lambda e, j=j, cs0=cs0: e.dma_start(out=grr[j][:], in_=gr_d[:, cs0:cs0 + CPS, :]), writes=[("grr", j)])
        S.act(lambda e, j=j: e.activation(out=sgr[j][:], in_=grr[j][:], func=AF.Silu), reads=[("grr", j)], writes=[("sgr", j)])
        S.pool(lambda e, j=j: e.tensor_tensor(out=sgr[j][:], in0=sgr[j][:], in1=gn[:].unsqueeze(1).broadcast_to([C, CPS, DV]), op=ALU.mult),
               reads=[("sgr", j), "gn"], writes=[("sgr", j)])
        for i, (o_, sz) in enumerate(DCS):
            E, CSM, X = t_e[i], t_cs[i], t_x[i]
            kE, kC, kX = ("t_e", i), ("t_cs", i), ("t_x", i)
            S.pe(lambda e, i=i, j=j, o_=o_, sz=sz: e.matmul(pz[0:sz, :], lhsT=Wg[:, o_:o_ + sz], rhs=lr[j][:], start=True, stop=True),
                 reads=["Wg", ("lr", j)], writes=["pz"])
            S.act(lambda e, i=i, sz=sz, E=E: e.activation(out=E[:], in_=pz[0:sz, :], func=AF.Exp, bias=nbg[i][:, 0:1], scale=-1.0),
                  reads=["pz", ("nbg", i)], writes=[kE])
            S.act(lambda e, E=E: e.activation(out=E[:], in_=E[:], func=AF.Ln, bias=1.0, scale=1.0), reads=[kE], writes=[kE])
            S.dve(lambda e, E=E, CSM=CSM, sz=sz: e.tensor_tensor_scan(out=CSM[:], data0=rmask[0:sz, :], data1=E[:], initial=0.0,
                                                                       op0=ALU.mult, op1=ALU.add), reads=[kE, "rmask"], writes=[kC])
            S.dve(lambda e, i=i, j=j, CSM=CSM: e.tensor_scalar(out=nbl[i][j][:], in0=CSM[:, C - 1:SEG:C], scalar1=-INV16, scalar2=None, op0=ALU.mult),
                  reads=[kC], writes=[("nbl", i, j)])
            S.act(lambda e, i=i, j=j: e.activation(out=gdec[i][j][:], in_=nbl[i][j][:], func=AF.Exp), reads=[("nbl", i, j)], writes=[("gdec", i, j)])
            S.act(lambda e, CSM=CSM, X=X: e.activation(out=X[:], in_=CSM[:], func=AF.Exp, scale=-INV16), reads=[kC], writes=[kX])
            S.dve(lambda e, i=i, j=j, X=X: e.scalar_tensor_tensor(out=qt[i][j][:], in0=X[:], scalar=float(DK ** -0.5), in1=qr[i][j][:],
                                                                   op0=ALU.mult, op1=ALU.mult), reads=[kX, ("qr", i, j)], writes=[("qt", i, j)])
            S.act(lambda e, CSM=CSM, X=X: e.activation(out=X[:], in_=CSM[:], func=AF.Exp, scale=INV16), reads=[kC], writes=[kX])
            S.dve(lambda e, i=i, j=j, X=X: e.tensor_tensor(out=kt[i][j][:], in0=X[:], in1=kr[i][j][:], op=ALU.mult),
                  reads=[kX, ("kr", i, j)], writes=[("kt", i, j)])
            for c in range(CPS):
                S.act(lambda e, i=i, j=j, c=c, CSM=CSM, X=X: e.activation(out=X[:, c * C:(c + 1) * C], in_=CSM[:, c * C:(c + 1) * C], func=AF.Exp,
                                                                           bias=nbl[i][j][:, c:c + 1], scale=INV16),
                      reads=[kC, ("nbl", i, j)], writes=[kX])
            S.dve(lambda e, i=i, j=j, X=X: e.tensor_tensor(out=kh[i][j][:], in0=X[:], in1=kr[i][j][:], op=ALU.mult),
                  reads=[kX, ("kr", i, j)], writes=[("kh", i, j)])
        for c in range(CPS):
            cs = slice(c * C, (c + 1) * C)
            S.pe(lambda e, j=j, cs=cs: e.transpose(out=pT[0:C, 0:128], in_=kh[0][j][:, cs], identity=ident[:]),
                 reads=[("kh", 0, j), "ident"], writes=["pT"])
            S.pe(lambda e, j=j, cs=cs: e.transpose(out=pT[0:C, 128:192], in_=kh[1][j][:, cs], identity=ident[0:64, 0:64]),
                 reads=[("kh", 1, j), "ident"], writes=["pT"])
            S.act(lambda e, j=j, c=c: e.copy(out=khat[j][:, c, :], in_=pT[0:C, 0:DK]), reads=["pT"], writes=[("khat", j, c)])
            S.pe(lambda e, j=j, cs=cs: e.matmul(pS[:, 0:C], lhsT=kt[0][j][:, cs], rhs=qt[0][j][:, cs], start=True, stop=False),
                 reads=[("kt", 0, j), ("qt", 0, j)], writes=["pS"])
            S.pe(lambda e, j=j, cs=cs: e.matmul(pS[:, 0:C], lhsT=kt[1][j][:, cs], rhs=qt[1][j][:, cs], start=False, stop=True),
                 reads=[("kt", 1, j), ("qt", 1, j)], writes=["pS"])
            S.dve(lambda e, j=j, c=c: e.tensor_tensor(out=sTb[j][:, c, :], in0=pS[:, 0:C], in1=umask[:], op=ALU.mult),
                  reads=["pS", "umask"], writes=[("sTb", j, c)])
        for c in range(CPS):
            gc = cs0 + c
            cs = slice(c * C, (c + 1) * C)
            po = pO[gc % 2]
            kpo = ("pO", gc % 2)
            first = (gc == 0)
            S.pe(lambda e, j=j, c=c, po=po, first=first: e.matmul(po[:, 0:DV], lhsT=sTb[j][:, c, :], rhs=vv[j][:, c, :], start=True, stop=first),
                 reads=[("sTb", j, c), ("vv", j)], writes=[kpo])
            if not first:
                S.pe(lambda e, j=j, cs=cs, po=po: e.matmul(po[:, 0:DV], lhsT=qt[0][j][:, cs], rhs=stbf[0][:], start=False, stop=False),
                     reads=[("qt", 0, j), ("stbf", 0)], writes=[kpo])
                S.pe(lambda e, j=j, cs=cs, po=po: e.matmul(po[:, 0:DV], lhsT=qt[1][j][:, cs], rhs=stbf[1][:], start=False, stop=True),
                     reads=[("qt", 1, j), ("stbf", 1)], writes=[kpo])
            for i, (o_, sz) in enumerate(DCS):
                pd = pD[i]
                S.pe(lambda e, i=i, j=j, c=c, o_=o_, sz=sz, pd=pd: e.matmul(pd[0:sz, 0:DV], lhsT=khat[j][:, c, o_:o_ + sz], rhs=vv[j][:, c, :],
                                                                             start=True, stop=True),
                     reads=[("khat", j, c), ("vv", j)], writes=[("pD", i)])
                if first:
                    S.dve(lambda e, i=i, sz=sz, pd=pd: e.tensor_copy(out=st32[i][:], in_=pd[0:sz, 0:DV]), reads=[("pD", i)], writes=[("st32", i)])
                else:
                    S.dve(lambda e, i=i, j=j, c=c, sz=sz, pd=pd: e.scalar_tensor_tensor(out=st32[i][:], in0=st32[i][:], scalar=gdec[i][j][:, c:c + 1],
                                                                                         in1=pd[0:sz, 0:DV], op0=ALU.mult, op1=ALU.add),
                          reads=[("pD", i), ("st32", i), ("gdec", i, j)], writes=[("st32", i)])
                S.act(lambda e, i=i: e.copy(out=stbf[i][:], in_=st32[i][:]), reads=[("st32", i)], writes=[("stbf", i)])
            R = rs_t[gc % 2]
            kR = ("rs", gc % 2)
            S.act(lambda e, po=po, R=R: e.activation(out=junk[:], in_=po[:, 0:DV], func=AF.Square, accum_out=R[:, 0:1]),
                  reads=[kpo], writes=[kR, "junk"])
            S.dve(lambda e, R=R: e.tensor_scalar(out=R[:, 1:2], in0=R[:, 0:1], scalar1=float(1.0 / DV), scalar2=float(eps), op0=ALU.mult, op1=ALU.add),
                  reads=[kR], writes=[kR])
            S.act(lambda e, R=R: e.activation(out=R[:, 2:3], in_=R[:, 1:2], func=AF.Sqrt), reads=[kR], writes=[kR])
            S.dve(lambda e, R=R: e.reciprocal(out=R[:, 3:4], in_=R[:, 2:3]), reads=[kR], writes=[kR])
            S.dve(lambda e, j=j, c=c, po=po, R=R: e.scalar_tensor_tensor(out=ob[j][:, c, :], in0=po[:, 0:DV], scalar=R[:, 3:4], in1=sgr[j][:, c, :],
                                                                          op0=ALU.mult, op1=ALU.mult), reads=[kpo, kR, ("sgr", j)], writes=[("ob", j)])
        outs.append(S.dma("sp", lambda e, j=j, cs0=cs0: e.dma_start(out=o_d[:, cs0:cs0 + CPS, :], in_=ob[j][:]), reads=[("ob", j)]))
    S.emit(final_wait_ops=outs[-3:])
    return nc

NEG = -1.0e9


def _softmax_pv_consts(S):
    ones = S.sb([128, 128], F32, "c_ones")
    ident_f = S.sb([128, 128], F32, "c_identf")
    ident = S.sb([128, 128], BF16, "c_ident")
    S.pool(lambda e: e.memset(ones[:], 1.0), writes=["c_ones"])
    S.pool(lambda e: e.affine_select(out=ident_f[:], in_=ones[:], pattern=[[1, 128]], compare_op=ALU.is_equal, fill=0.0,
                                     base=0, channel_multiplier=-1), reads=["c_ones"], writes=["c_identf"])
    S.dve(lambda e: e.tensor_copy(out=ident[:], in_=ident_f[:]), reads=["c_identf"], writes=["c_ident"])
    return ident


def build_diff(NSLOT, SB, NH, lam_init, eps=1e-5):
    nc = bass.Bass("TRN2", target_bir_lowering=False)
    D, DV = 64, 128
    TK = NSLOT * SB * 128
    TQ = NSLOT * 128
    NKBT = TK // 128
    qT_d = nc.dram_tensor("qT", [NH, 128, TQ], BF16, kind="ExternalInput").ap()
    kT_d = nc.dram_tensor("kT", [NH, 128, TK], BF16, kind="ExternalInput").ap()
    v_d = nc.dram_tensor("v", [NH, 128, NKBT, DV], BF16, kind="ExternalInput").ap()
    msk_d = nc.dram_tensor("msk", [NSLOT, 128, SB * 128], F32, kind="ExternalInput").ap()
    lam_d = nc.dram_tensor("lamv", [4, D], F32, kind="ExternalInput").ap()
    g_d = nc.dram_tensor("g", [1, DV], F32, kind="ExternalInput").ap()
    o_d = nc.dram_tensor("o", [TQ, NH, DV], BF16, kind="ExternalOutput").ap()
    S = Sched(nc)
    ident = _softmax_pv_consts(S)
    lv = S.sb([128, 4, D], F32, "lv")
    lw = S.sb([128, 8], F32, "lw")
    junk = S.sb([128, DV], F32, "junk")
    gt = S.sb([128, DV], F32, "gt")
    for i in range(4):
        S.dma("sp", lambda e, i=i: e.dma_start(out=lv[:, i, :], in_=lam_d[i, :].partition_broadcast(128)), writes=[("lv", i)])
    S.dma("sp", lambda e: e.dma_start(out=gt[:], in_=g_d[0, :].partition_broadcast(128)), writes=["gt"])
    S.dve(lambda e: e.tensor_scalar(out=gt[:], in0=gt[:], scalar1=float(1.0 - lam_init), scalar2=None, op0=ALU.mult), reads=["gt"], writes=["gt"])
    for i in range(2):
        S.dve(lambda e, i=i: e.tensor_tensor(out=junk[:, 0:D], in0=lv[:, 2 * i, :], in1=lv[:, 2 * i + 1, :], op=ALU.mult),
              reads=[("lv", 2 * i), ("lv", 2 * i + 1)], writes=["junk"])
        S.dve(lambda e, i=i: e.reduce_sum(out=lw[:, i:i + 1], in_=junk[:, 0:D], axis=AX.X), reads=["junk"], writes=["lw"])
    S.act(lambda e: e.activation(out=lw[:, 2:4], in_=lw[:, 0:2], func=AF.Exp), reads=["lw"], writes=["lw"])
    S.dve(lambda e: e.tensor_tensor(out=lw[:, 4:5], in0=lw[:, 3:4], in1=lw[:, 2:3], op=ALU.subtract), reads=["lw"], writes=["lw"])
    S.dve(lambda e: e.tensor_scalar(out=lw[:, 5:6], in0=lw[:, 4:5], scalar1=float(-lam_init), scalar2=None, op0=ALU.add), reads=["lw"], writes=["lw"])
    nlam = lw[:, 5:6]
    mk_ = S.sb([128, NSLOT, SB * 128], F32, "mk_sb")
    S.dma("sp", lambda e: e.dma_start(out=mk_[:], in_=msk_d.rearrange("s p k -> p s k")), writes=["mk"])
    qt = [S.sb([128, TQ], BF16, f"qt{i}") for i in range(2)]
    kt = [S.sb([128, TK], BF16, f"kt{i}") for i in range(2)]
    vt = [S.sb([128, NKBT, DV], BF16, f"vt{i}") for i in range(2)]
    Sb = [[S.sb([128, TK], F32, f"S{c}_{i}") for i in range(2)] for c in range(2)]
    Ab = [S.sb([128, TK], BF16, f"A{i}") for i in range(2)]
    AT = [S.sb([128, NKBT, 128], BF16, f"AT{i}") for i in range(2)]
    st = [S.sb([128, 64], F32, f"stt{i}") for i in range(2)]
    ob = [S.sb([128, DV], BF16, f"ob{i}") for i in range(4)]
    pS = [S.ps([128, 512], F32, f"pS{i}") for i in range(4)]
    pT = [S.ps([128, 8, 128], BF16, f"pT{i}") for i in range(2)]
    pO = [S.ps([128, 512], F32, f"pO{i}") for i in range(2)]
    outs = []
    cnt = {"ips": 0, "ipt": 0}

    def load_head(h):
        hj = h % 2
        S.dma("sp", lambda e, h=h, hj=hj: e.dma_start(out=qt[hj][:], in_=qT_d[h]), writes=[("qt", hj)])
        S.dma("sp", lambda e, h=h, hj=hj: e.dma_start(out=kt[hj][:], in_=kT_d[h]), writes=[("kt", hj)])
        S.dma("act", lambda e, h=h, hj=hj: e.dma_start(out=vt[hj][:], in_=v_d[h]), writes=[("vt", hj)])

    def stage_a(h, k, it):
        hj, j = h % 2, it % 2
        L = SB * (k + 1) * 128
        L0 = L - SB * 128
        ST = st[j]
        kST = ("st", j)
        qs = slice(k * 128, (k + 1) * 128)
        for c in range(2):
            Sc = Sb[c][j]
            kS = ("S", c, j)
            ps_ = slice(64 * c, 64 * c + 64)
            col = 0
            nmx = 0
            while col < L:
                w = min(512, L - col) if col >= L0 else min(512, L0 - col)
                ips = cnt["ips"]
                p = pS[ips % 4]
                kp = ("pS", ips % 4)
                S.pe(lambda e, p=p, hj=hj, ps_=ps_, qs=qs, col=col, w=w: e.matmul(p[:, 0:w], lhsT=qt[hj][ps_, qs], rhs=kt[hj][ps_, col:col + w],
                                                                              start=True, stop=True),
                     reads=[("qt", hj), ("kt", hj)], writes=[kp])
                mcol = ST[:, 16 * c + nmx % 16: 16 * c + nmx % 16 + 1]
                if col >= L0:
                    S.dve(lambda e, p=p, Sc=Sc, col=col, w=w, k=k, L0=L0: e.tensor_tensor(
                        out=Sc[:, col:col + w], in0=p[:, 0:w], in1=mk_[:, k, col - L0:col - L0 + w], op=ALU.add),
                        reads=[kp, "mk"], writes=[kS])
                    S.dve(lambda e, Sc=Sc, col=col, w=w, mcol=mcol: e.reduce_max(out=mcol, in_=Sc[:, col:col + w], axis=AX.X),
                          reads=[kS], writes=[(kST, c, nmx)])
                else:
                    S.dve(lambda e, p=p, Sc=Sc, col=col, w=w, mcol=mcol: e.tensor_scalar(
                        out=Sc[:, col:col + w], in0=p[:, 0:w], scalar1=1.0, scalar2=-3.0e38, op0=ALU.mult, op1=ALU.max, accum_out=mcol),
                        reads=[kp], writes=[kS, (kST, c, nmx)])
                cnt["ips"] += 1
                nmx += 1
                col += w
            nm = nmx
            S.dve(lambda e, ST=ST, c=c, nm=nm: e.reduce_max(out=ST[:, 32 + c:33 + c], in_=ST[:, 16 * c:16 * c + nm], axis=AX.X),
                  reads=[(kST, c, i_) for i_ in range(nm)], writes=[(kST, "m", c)])
            S.dve(lambda e, ST=ST, c=c: e.tensor_scalar(out=ST[:, 34 + c:35 + c], in0=ST[:, 32 + c:33 + c], scalar1=-0.125, scalar2=None, op0=ALU.mult),
                  reads=[(kST, "m", c)], writes=[(kST, "nmb", c)])
            S.act(lambda e, Sc=Sc, ST=ST, c=c, L=L: e.activation(out=Sc[:, 0:L], in_=Sc[:, 0:L], func=AF.Exp, bias=ST[:, 34 + c:35 + c], scale=0.125,
                                                                 accum_out=ST[:, 36 + c:37 + c]),
                  reads=[kS, (kST, "nmb", c)], writes=[kS, (kST, "l", c)])

    def stage_b(h, k, it):
        hj, j = h % 2, it % 2
        L = SB * (k + 1) * 128
        NKB = L // 128
        ST = st[j]
        kST = ("st", j)
        qs = slice(k * 128, (k + 1) * 128)
        S.dve(lambda e, ST=ST: e.reciprocal(out=ST[:, 38:40], in_=ST[:, 36:38]), reads=[(kST, "l", 0), (kST, "l", 1)], writes=[(kST, "r")])
        S.dve(lambda e, ST=ST: e.tensor_scalar(out=ST[:, 40:41], in0=ST[:, 39:40], scalar1=nlam, scalar2=None, op0=ALU.mult),
              reads=[(kST, "r"), "lw"], writes=[(kST, "r2")])
        A = Ab[j]
        kA = ("A", j)
        S.act(lambda e, A=A, j=j, ST=ST, L=L: e.activation(out=A[:, 0:L], in_=Sb[0][j][:, 0:L], func=AF.Copy, scale=ST[:, 38:39]),
              reads=[("S", 0, j), (kST, "r")], writes=[kA])
        S.dve(lambda e, A=A, j=j, ST=ST, L=L: e.scalar_tensor_tensor(out=A[:, 0:L], in0=Sb[1][j][:, 0:L], scalar=ST[:, 40:41], in1=A[:, 0:L],
                                                                      op0=ALU.mult, op1=ALU.add),
              reads=[("S", 1, j), (kST, "r2"), kA], writes=[kA])
        ATj = AT[j]
        kb = 0
        while kb < NKB:
            g = min(4, NKB - kb)
            ipt = cnt["ipt"]
            pt = pT[ipt % 2]
            kpt = ("pT", ipt % 2)
            for u in range(g):
                S.pe(lambda e, pt=pt, A=A, kb=kb, u=u: e.transpose(out=pt[:, u, :], in_=A[:, (kb + u) * 128:(kb + u + 1) * 128], identity=ident[:]),
                     reads=[kA, "c_ident"], writes=[kpt])
            if ipt % 2 == 0:
                S.act(lambda e, pt=pt, ATj=ATj, kb=kb, g=g: e.copy(out=ATj[:, kb:kb + g, :], in_=pt[:, 0:g, :]), reads=[kpt], writes=[("AT", j, kb)])
            else:
                S.dve(lambda e, pt=pt, ATj=ATj, kb=kb, g=g: e.tensor_copy(out=ATj[:, kb:kb + g, :], in_=pt[:, 0:g, :]), reads=[kpt], writes=[("AT", j, kb)])
            cnt["ipt"] += 1
            kb += g
        po = pO[it % 2]
        kpo = ("pO", it % 2)
        for kb in range(NKB):
            S.pe(lambda e, po=po, ATj=ATj, hj=hj, kb=kb, NKB=NKB: e.matmul(po[:, 0:DV], lhsT=ATj[:, kb, :], rhs=vt[hj][:, kb, :],
                                                                           start=(kb == 0), stop=(kb == NKB - 1)),
                 reads=[("AT", j, (kb // 4) * 4), ("vt", hj)], writes=[kpo])
        S.act(lambda e, po=po, ST=ST: e.activation(out=junk[:], in_=po[:, 0:DV], func=AF.Square, accum_out=ST[:, 42:43]),
              reads=[kpo], writes=[(kST, "ss"), "junk"])
        S.dve(lambda e, ST=ST: e.tensor_scalar(out=ST[:, 43:44], in0=ST[:, 42:43], scalar1=float(1.0 / DV), scalar2=float(eps), op0=ALU.mult, op1=ALU.add),
              reads=[(kST, "ss")], writes=[(kST, "ms")])
        S.act(lambda e, ST=ST: e.activation(out=ST[:, 44:45], in_=ST[:, 43:44], func=AF.Sqrt), reads=[(kST, "ms")], writes=[(kST, "sq")])
        S.dve(lambda e, ST=ST: e.reciprocal(out=ST[:, 45:46], in_=ST[:, 44:45]), reads=[(kST, "sq")], writes=[(kST, "rs")])
        o = ob[it % 4]
        S.dve(lambda e, o=o, po=po, ST=ST: e.scalar_tensor_tensor(out=o[:], in0=po[:, 0:DV], scalar=ST[:, 45:46], in1=gt[:], op0=ALU.mult, op1=ALU.mult),
              reads=[kpo, (kST, "rs"), "gt"], writes=[("ob", it % 4)])
        outs.append(S.dma("sp", lambda e, o=o, qs=qs, h=h: e.dma_start(out=o_d[qs, h, :], in_=o[:]), reads=[("ob", it % 4)]))

    work = [(h, k) for h in range(NH) for k in range(NSLOT)]
    for i, (h, k) in enumerate(work):
        if k == 0:
            load_head(h)
        stage_a(h, k, i)
        if i > 0:
            stage_b(work[i - 1][0], work[i - 1][1], i - 1)
    stage_b(work[-1][0], work[-1][1], len(work) - 1)
    S.emit(final_wait_ops=outs[-6:])
    return nc

NEGBIG = -1.0e30


def build_dsa(NSLOT, SB, NH=10, NG=2, NIH=32, KSEL=256):
    nc = bass.Bass("TRN2", target_bir_lowering=False)
    DH, DI = 128, 64
    TK = NSLOT * SB * 128
    TQ = NSLOT * 128
    NKBT = TK // 128
    R = NH // NG
    NPR = NIH // 2
    NRND = KSEL // 8
    scale = float(DH ** -0.5)
    sq_d = nc.dram_tensor("sqT", [NH, DH, TQ], BF16, kind="ExternalInput").ap()
    sk_d = nc.dram_tensor("skT", [NG, DH, TK], BF16, kind="ExternalInput").ap()
    sv_d = nc.dram_tensor("sv", [NG, 128, NKBT, DH], BF16, kind="ExternalInput").ap()
    iq_d = nc.dram_tensor("iqT", [NPR, 128, TQ], BF16, kind="ExternalInput").ap()
    ik_d = nc.dram_tensor("ikT2", [128, TK], BF16, kind="ExternalInput").ap()
    iw_d = nc.dram_tensor("iw", [TQ, NIH], BF16, kind="ExternalInput").ap()
    msk_d = nc.dram_tensor("msk", [NSLOT, 128, SB * 128], F32, kind="ExternalInput").ap()
    o_d = nc.dram_tensor("o", [TQ, NH, DH], BF16, kind="ExternalOutput").ap()
    S = Sched(nc)
    ident = _softmax_pv_consts(S)
    sk = S.sb([128, NG, TK], BF16, "sk_sb")
    sv = S.sb([128, NG, NKBT, DH], BF16, "sv_sb")
    ik = S.sb([128, TK], BF16, "ik_sb")
    iwb = S.sb([128, NSLOT, NIH], BF16, "iwb")
    iw = S.sb([128, NSLOT, NIH], F32, "iw32")
    mk_ = S.sb([128, NSLOT, SB * 128], F32, "mk_sb")
    for g in range(NG):
        S.dma("sp", lambda e, g=g: e.dma_start(out=sk[:, g, :], in_=sk_d[g]), writes=[("sk", g)])
        S.dma("act", lambda e, g=g: e.dma_start(out=sv[:, g, :, :], in_=sv_d[g]), writes=[("sv", g)])
    S.dma("sp", lambda e: e.dma_start(out=ik[:], in_=ik_d), writes=["ik"])
    S.dma("sp", lambda e: e.dma_start(out=iwb[:], in_=iw_d.rearrange("(s p) h -> p s h", p=128)), writes=["iwb"])
    S.dve(lambda e: e.tensor_copy(out=iw[:], in_=iwb[:]), reads=["iwb"], writes=["iw"])
    S.dma("sp", lambda e: e.dma_start(out=mk_[:], in_=msk_d.rearrange("s p k -> p s k")), writes=["mk"])
    iq = [S.sb([128, NPR, 128], BF16, f"iq{i}") for i in range(2)]
    sq = [S.sb([128, NH, 128], BF16, f"sq{i}") for i in range(2)]
    score = S.sb([128, TK], F32, "score")
    Wk = S.sb([128, TK], F32, "Wk")
    sel = S.sb([128, TK], BF16, "sel")
    Rb = [S.sb([128, 512], F32, f"Rb{i}") for i in range(4)]
    Sb = [S.sb([128, TK], F32, f"S{i}") for i in range(2)]
    Pb = [S.sb([128, TK], BF16, f"P{i}") for i in range(2)]
    AT = [S.sb([128, NKBT, 128], BF16, f"AT{i}") for i in range(2)]
    st = [S.sb([128, 32], F32, f"stt{i}") for i in range(2)]
    m8 = S.sb([128, 8], F32, "m8")
    thr = S.sb([128, 1], F32, "thr")
    ob = [S.sb([128, DH], BF16, f"ob{i}") for i in range(4)]
    pS = [S.ps([128, 512], F32, f"pS{i}") for i in range(4)]
    pT = [S.ps([128, 8, 128], BF16, f"pT{i}") for i in range(2)]
    pO = [S.ps([128, 512], F32, f"pO{i}") for i in range(2)]
    outs = []
    ips = ir = ipt = it = 0
    for k in range(NSLOT):
        kj = k % 2
        L = SB * (k + 1) * 128
        NKB = L // 128
        L0 = L - SB * 128
        qs = slice(k * 128, (k + 1) * 128)
        S.dma("sp", lambda e, kj=kj, qs=qs: e.dma_start(out=iq[kj][:], in_=iq_d[:, :, qs].rearrange("n p q -> p n q")), writes=[("iq", kj)])
        S.dma("sp", lambda e, kj=kj, qs=qs: e.dma_start(out=sq[kj][:], in_=sq_d[:, :, qs].rearrange("n p q -> p n q")), writes=[("sq", kj)])
        chunks = []
        col = 0
        while col < L:
            w = min(512, L - col)
            chunks.append((col, w))
            col += w
        for hh in range(NIH):
            pr, mem = hh // 2, hh % 2
            ps_ = slice(64 * mem, 64 * mem + 64)
            for (col, w) in chunks:
                p = pS[ips % 4]
                kp = ("pS", ips % 4)
                rb = Rb[ir % 4]
                krb = ("Rb", ir % 4)
                S.pe(lambda e, p=p, kj=kj, pr=pr, ps_=ps_, col=col, w=w: e.matmul(p[:, 0:w], lhsT=iq[kj][ps_, pr, :], rhs=ik[ps_, col:col + w],
                                                                                  start=True, stop=True),
                     reads=[("iq", kj), "ik"], writes=[kp])
                S.act(lambda e, p=p, rb=rb, w=w: e.activation(out=rb[:, 0:w], in_=p[:, 0:w], func=AF.Relu), reads=[kp], writes=[krb])
                wcol = iw[:, k, hh:hh + 1]
                if hh == 0:
                    S.dve(lambda e, rb=rb, col=col, w=w, wcol=wcol: e.tensor_scalar(out=score[:, col:col + w], in0=rb[:, 0:w], scalar1=wcol, scalar2=None,
                                                                                    op0=ALU.mult), reads=[krb, "iw"], writes=[("score", col)])
                else:
                    S.dve(lambda e, rb=rb, col=col, w=w, wcol=wcol: e.scalar_tensor_tensor(out=score[:, col:col + w], in0=rb[:, 0:w], scalar=wcol,
                                                                                           in1=score[:, col:col + w], op0=ALU.mult, op1=ALU.add),
                          reads=[krb, "iw", ("score", col)], writes=[("score", col)])
                ips += 1
                ir += 1
        allsc = [("score", c_) for (c_, w_) in chunks]
        S.dve(lambda e, k=k, L=L, L0=L0: e.tensor_tensor(out=score[:, L0:L], in0=score[:, L0:L], in1=mk_[:, k, :], op=ALU.add),
              reads=allsc + ["mk"], writes=allsc)
        S.act(lambda e, L=L: e.copy(out=Wk[:, 0:L], in_=score[:, 0:L]), reads=allsc, writes=["Wk"])
        for r in range(NRND):
            S.dve(lambda e, L=L: e.max(out=m8[:], in_=Wk[:, 0:L]), reads=["Wk"], writes=["m8"])
            if r < NRND - 1:
                S.dve(lambda e, L=L: e.match_replace(out=Wk[:, 0:L], in_to_replace=m8[:], in_values=Wk[:, 0:L], imm_value=NEGBIG),
                      reads=["Wk", "m8"], writes=["Wk"])
        S.dve(lambda e: e.tensor_scalar(out=thr[:], in0=m8[:, 7:8], scalar1=-1.0e29, scalar2=None, op0=ALU.max), reads=["m8"], writes=["thr"])
        S.dve(lambda e, L=L: e.tensor_scalar(out=sel[:, 0:L], in0=score[:, 0:L], scalar1=thr[:, 0:1], scalar2=None, op0=ALU.is_ge),
              reads=allsc + ["thr"], writes=["sel"])
        cnt = {"ips": ips, "ipt": ipt}

        def stage_a(h, it, k=k, kj=kj, L=L, chunks=chunks):
            g = h // R
            j = it % 2
            ST = st[j]
            kST = ("st", j)
            Sc, P = Sb[j], Pb[j]
            kS, kP = ("S", j), ("P", j)
            for ci, (col, w) in enumerate(chunks):
                p = pS[cnt["ips"] % 4]
                kp = ("pS", cnt["ips"] % 4)
                S.pe(lambda e, p=p, kj=kj, h=h, g=g, col=col, w=w: e.matmul(p[:, 0:w], lhsT=sq[kj][:, h, :], rhs=sk[:, g, col:col + w], start=True, stop=True),
                     reads=[("sq", kj), ("sk", g)], writes=[kp])
                S.dve(lambda e, p=p, Sc=Sc, col=col, w=w, ST=ST, ci=ci: e.tensor_scalar(out=Sc[:, col:col + w], in0=p[:, 0:w], scalar1=1.0, scalar2=-3.0e38,
                                                                                        op0=ALU.mult, op1=ALU.max, accum_out=ST[:, ci:ci + 1]),
                      reads=[kp], writes=[kS, (kST, ci)])
                cnt["ips"] += 1
            nch = len(chunks)
            S.dve(lambda e, ST=ST, nch=nch: e.reduce_max(out=ST[:, 16:17], in_=ST[:, 0:nch], axis=AX.X), reads=[(kST, i_) for i_ in range(nch)], writes=[(kST, "m")])
            S.dve(lambda e, ST=ST: e.tensor_scalar(out=ST[:, 17:18], in0=ST[:, 16:17], scalar1=-scale, scalar2=None, op0=ALU.mult), reads=[(kST, "m")], writes=[(kST, "nmb")])
            S.act(lambda e, Sc=Sc, P=P, ST=ST, L=L: e.activation(out=P[:, 0:L], in_=Sc[:, 0:L], func=AF.Exp, bias=ST[:, 17:18], scale=scale),
                  reads=[kS, (kST, "nmb")], writes=[kP])

        def stage_b(h, it, k=k, kj=kj, L=L, NKB=NKB, qs=qs):
            g = h // R
            j = it % 2
            ST = st[j]
            kST = ("st", j)
            P = Pb[j]
            kP = ("P", j)
            S.dve(lambda e, P=P, ST=ST, L=L: e.scalar_tensor_tensor(out=P[:, 0:L], in0=P[:, 0:L], scalar=1.0, in1=sel[:, 0:L], op0=ALU.mult, op1=ALU.mult,
                                                                    accum_out=ST[:, 18:19]), reads=[kP, "sel"], writes=[kP, (kST, "l")])
            S.dve(lambda e, ST=ST: e.reciprocal(out=ST[:, 19:20], in_=ST[:, 18:19]), reads=[(kST, "l")], writes=[(kST, "r")])
            ATj = AT[j]
            kb = 0
            while kb < NKB:
                gsz = min(4, NKB - kb)
                pt = pT[cnt["ipt"] % 2]
                kpt = ("pT", cnt["ipt"] % 2)
                for u in range(gsz):
                    S.pe(lambda e, pt=pt, P=P, kb=kb, u=u: e.transpose(out=pt[:, u, :], in_=P[:, (kb + u) * 128:(kb + u + 1) * 128], identity=ident[:]),
                         reads=[kP, "c_ident"], writes=[kpt])
                S.act(lambda e, pt=pt, ATj=ATj, kb=kb, gsz=gsz: e.copy(out=ATj[:, kb:kb + gsz, :], in_=pt[:, 0:gsz, :]), reads=[kpt], writes=[("AT", j, kb)])
                cnt["ipt"] += 1
                kb += gsz
            po = pO[it % 2]
            kpo = ("pO", it % 2)
            for kb in range(NKB):
                S.pe(lambda e, po=po, ATj=ATj, g=g, kb=kb, NKB=NKB: e.matmul(po[:, 0:DH], lhsT=ATj[:, kb, :], rhs=sv[:, g, kb, :],
                                                                             start=(kb == 0), stop=(kb == NKB - 1)),
                     reads=[("AT", j, (kb // 4) * 4), ("sv", g)], writes=[kpo])
            o = ob[it % 4]
            S.dve(lambda e, o=o, po=po, ST=ST: e.tensor_scalar(out=o[:], in0=po[:, 0:DH], scalar1=ST[:, 19:20], scalar2=None, op0=ALU.mult),
                  reads=[kpo, (kST, "r")], writes=[("ob", it % 4)])
            outs.append(S.dma("sp", lambda e, o=o, qs=qs, h=h: e.dma_start(out=o_d[qs, h, :], in_=o[:]), reads=[("ob", it % 4)]))

        for h in range(NH):
            stage_a(h, it + h)
            if h > 0:
                stage_b(h - 1, it + h - 1)
        stage_b(NH - 1, it + NH - 1)
        it += NH
        ips, ipt = cnt["ips"], cnt["ipt"]
    S.emit(final_wait_ops=outs[-6:])
    return nc


def build_cast(Tc):
    nc = bass.Bass("TRN2", target_bir_lowering=False)
    D = 4096
    x = nc.dram_tensor("x", [Tc, D], F32, kind="ExternalInput").ap()
    xb = nc.dram_tensor("xb", [Tc, D], BF16, kind="ExternalOutput").ap()
    S = Sched(nc)
    xt = [S.sb([128, D], F32, f"xt{i}") for i in range(2)]
    bt = [S.sb([128, D], BF16, f"bt{i}") for i in range(2)]
    outs = []
    for t in range(Tc // 128):
        j = t % 2
        rs = slice(t * 128, (t + 1) * 128)
        S.dma("sp", lambda e, j=j, rs=rs: e.dma_start(out=xt[j][:], in_=x[rs, :]), writes=[("xt", j)])
        S.add(("dve", "pool")[j], lambda e, j=j: e.tensor_copy(out=bt[j][:], in_=xt[j][:]), reads=[("xt", j)], writes=[("bt", j)])
        outs.append(S.dma("sp", lambda e, j=j, rs=rs: e.dma_start(out=xb[rs, :], in_=bt[j][:]), reads=[("bt", j)]))
    S.emit(final_wait_ops=outs[-4:])
    return nc


import time as _time
import ml_dtypes as _mld
from concourse.bass_utils import run_bass_kernel_spmd

_BF = _mld.bfloat16
_NC_CACHE = {}
DEPTH = 2
LAMBDA_INIT = [0.8 - 0.6 * math.exp(-0.3 * l) for l in range(DEPTH)]
ALPHA = (2 * DEPTH) ** 0.25
_SPL = (768, 768, 1536, 1536, 16, 1280, 1280, 1280, 1280, 256, 256, 2048, 64, 32)
_OFF = [0]
for _s in _SPL:
    _OFF.append(_OFF[-1] + _s)
(O_GQ, O_GK, O_GV, O_GR, O_GLR, O_DQ, O_DK, O_DV, O_SQ, O_SK, O_SV, O_IQ, O_IK, O_IW, O_END) = _OFF
_LOG = []


def _get(key, fn):
    if key not in _NC_CACHE:
        _NC_CACHE[key] = fn()
    return _NC_CACHE[key]


def _run(name, nc, in_maps):
    t0 = _time.time()
    res = run_bass_kernel_spmd(nc, in_maps, core_ids=list(range(8)))
    _LOG.append((name, _time.time() - t0))
    print(f"[mk] launch {name}: {_time.time() - t0:.1f}s", flush=True)
    return res.results


def _c(a):
    return np.ascontiguousarray(a)


def _slot_blocks(r):
    return [4 * k + (r if k % 2 == 0 else 3 - r) for k in range(8)]


def _masks(r, neg):
    m = np.zeros((8, 128, 512), np.float32)
    for k, gq in enumerate(_slot_blocks(r)):
        keys = 4 * k * 128 + np.arange(512)
        qpos = gq * 128 + np.arange(128)
        m[k] = np.where(keys[None, :] <= qpos[:, None], 0.0, neg)
    return m


def _forward(inputs, dbg=None):
    x = np.asarray(inputs["x"], np.float32).reshape(8192, 4096)
    pos = np.asarray(inputs["positions"], np.int32).reshape(8192, 1)
    invf = (1.0 / (np.float32(500000.0) ** (np.arange(16, dtype=np.float32) * np.float32(2.0) / np.float32(32)))).astype(np.float32)[None]
    xb_T = None
    for l in range(DEPTH):
        w_in = np.asarray(inputs["w_in"][l], np.float32)
        Wp = np.zeros((4096, 8 * 1664), np.float32)
        Wp[:, :12400] = w_in
        if l == 0:
            ncc = _get(("cast",), lambda: build_cast(1024))
            res = _run("cast_x", ncc, [{"x": _c(x[c * 1024:(c + 1) * 1024])} for c in range(8)])
            xb_T = _c(np.concatenate([res[c]["xb"] for c in range(8)], axis=0).T)
            del res
        xT = xb_T
        nc = _get(("lin", 1664, False), lambda: build_lin(4096, 8192, 1664, False, BF16))
        res = _run(f"inproj{l}", nc, [{"xT": xT, "W": _c(Wp[:, c * 1664:(c + 1) * 1664])} for c in range(8)])
        hT = np.concatenate([res[c]["YT"] for c in range(8)], axis=0)[:12400]
        del Wp, res
        if dbg is not None:
            dbg[f"hT{l}"] = hT
        h16 = _c(np.concatenate([hT[O_DQ:O_DK], hT[O_DK:O_DV], hT[O_IQ:O_IK], hT[O_IK:O_IW]], axis=0).T)
        h32 = _c(np.concatenate([hT[O_SQ:O_SK], hT[O_SK:O_SV]], axis=0).T)
        nc = _get(("rope",), lambda: build_rope(1024))
        res = _run(f"rope{l}", nc, [{"pos": _c(pos[c * 1024:(c + 1) * 1024]), "invf": invf,
                                     "h16": _c(h16[c * 1024:(c + 1) * 1024]), "h32": _c(h32[c * 1024:(c + 1) * 1024])} for c in range(8)])
        r16 = np.concatenate([res[c]["o16"] for c in range(8)], axis=0)
        r32 = np.concatenate([res[c]["o32"] for c in range(8)], axis=0)
        dq, dk, iq, ik = r16[:, 0:1280], r16[:, 1280:2560], r16[:, 2560:4608], r16[:, 4608:4672]
        sq, sk = r32[:, 0:1280], r32[:, 1280:1536]
        del res, h16, h32
        if dbg is not None:
            dbg[f"dq{l}"] = dq; dbg[f"dk{l}"] = dk; dbg[f"iq{l}"] = iq; dbg[f"ik{l}"] = ik; dbg[f"sq{l}"] = sq; dbg[f"sk{l}"] = sk
        wgu = np.asarray(inputs["w_gate_up"][l], np.float32)
        bgt = np.asarray(inputs["b_gate"][l], np.float32)
        gng = np.asarray(inputs["gla_norm_g"][l], np.float32)[None]

        def chunked(a):
            return _c(a.reshape(64, 64, -1).transpose(1, 0, 2))
        maps = []
        for c in range(8):
            b, hd = c // 4, c % 4
            ts = slice(b * 4096, (b + 1) * 4096)
            maps.append({"qT": _c(hT[O_GQ + hd * 192:O_GQ + (hd + 1) * 192, ts]), "kT": _c(hT[O_GK + hd * 192:O_GK + (hd + 1) * 192, ts]),
                         "lrT": _c(hT[O_GLR:O_GLR + 16, ts]), "Wg": _c(wgu[:, hd * 192:(hd + 1) * 192]), "bg": _c(bgt[hd * 192:(hd + 1) * 192, None]),
                         "v": chunked(hT[O_GV + hd * 384:O_GV + (hd + 1) * 384, ts].T), "gr": chunked(hT[O_GR + hd * 384:O_GR + (hd + 1) * 384, ts].T),
                         "gn": gng})
        nc = _get(("gla",), lambda: build_gla(8))
        res = _run(f"gla{l}", nc, maps)
        cat = np.zeros((8192, 4096), _BF)
        for c in range(8):
            b, hd = c // 4, c % 4
            cat[b * 4096:(b + 1) * 4096, hd * 384:(hd + 1) * 384] = res[c]["o"].transpose(1, 0, 2).reshape(4096, 384)
        del res, maps
        lamv = np.stack([np.asarray(inputs[n][l], np.float32) for n in ("lambda_q1", "lambda_k1", "lambda_q2", "lambda_k2")])
        dng = np.asarray(inputs["diff_norm_g"][l], np.float32)[None]
        dmaps, smaps, qtoks = [], [], []
        for c in range(8):
            b, r = c // 4, c % 4
            ts = slice(b * 4096, (b + 1) * 4096)
            qtok = np.concatenate([np.arange(g * 128, (g + 1) * 128) for g in _slot_blocks(r)])
            qtoks.append(qtok)
            dq_b, dk_b = dq[ts], dk[ts]
            dv_b = hT[O_DV:O_SQ, ts].T
            dmaps.append({"qT": _c(dq_b[qtok].reshape(1024, 10, 128).transpose(1, 2, 0)),
                          "kT": _c(dk_b.reshape(4096, 10, 128).transpose(1, 2, 0)),
                          "v": _c(dv_b.reshape(32, 128, 10, 128).transpose(2, 1, 0, 3)),
                          "msk": _masks(r, -1.0e9), "lamv": lamv, "g": dng})
            sq_b, sk_b, iq_b, ik_b = sq[ts], sk[ts], iq[ts], ik[ts]
            sv_b = hT[O_SV:O_IQ, ts].T
            iw_b = hT[O_IW:O_END, ts].T
            ikT = ik_b.T
            smaps.append({"sqT": _c(sq_b[qtok].reshape(1024, 10, 128).transpose(1, 2, 0)),
                          "skT": _c(sk_b.reshape(4096, 2, 128).transpose(1, 2, 0)),
                          "sv": _c(sv_b.reshape(32, 128, 2, 128).transpose(2, 1, 0, 3)),
                          "iqT": _c(iq_b[qtok].reshape(1024, 16, 2, 64).transpose(1, 2, 3, 0).reshape(16, 128, 1024)),
                          "ikT2": _c(np.concatenate([ikT, ikT], axis=0)),
                          "iw": _c(iw_b[qtok]), "msk": _masks(r, -1.0e30)})
        nc = _get(("diff", l), lambda: build_diff(8, 4, 10, LAMBDA_INIT[l]))
        res = _run(f"diff{l}", nc, dmaps)
        for c in range(8):
            b = c // 4
            cat[b * 4096 + qtoks[c], 1536:2816] = res[c]["o"].reshape(1024, 1280)
        nc = _get(("dsa",), lambda: build_dsa(8, 4))
        res = _run(f"dsa{l}", nc, smaps)
        for c in range(8):
            b = c // 4
            cat[b * 4096 + qtoks[c], 2816:4096] = res[c]["o"].reshape(1024, 1280)
        del res, dmaps, smaps, hT
        if dbg is not None:
            dbg[f"cat{l}"] = cat
        w_out = np.asarray(inputs["w_out"][l], np.float32)
        catT = _c(cat.T)
        nc = _get(("lin", 512, False), lambda: build_lin(4096, 8192, 512, False, BF16))
        res = _run(f"outproj{l}", nc, [{"xT": catT, "W": _c(w_out[:, c * 512:(c + 1) * 512])} for c in range(8)])
        mix = _c(np.concatenate([res[c]["YT"] for c in range(8)], axis=0).T)
        del res, catT, cat
        if dbg is not None:
            dbg[f"mix{l}"] = mix
        nc = _get(("ln", 1), lambda: build_ln(1024, 1, True, ALPHA))
        g1 = np.asarray(inputs["ln1_g"][l], np.float32)[None]
        b1 = np.asarray(inputs["ln1_b"][l], np.float32)[None]
        res = _run(f"ln1_{l}", nc, [{"x": _c(x[c * 1024:(c + 1) * 1024]), "a0": _c(mix[c * 1024:(c + 1) * 1024]), "g": g1, "b": b1} for c in range(8)])
        x1 = np.concatenate([res[c]["y"] for c in range(8)], axis=0)
        x1b_T = _c(np.concatenate([res[c]["yb"] for c in range(8)], axis=0).T)
        del res, mix
        if dbg is not None:
            dbg[f"x1_{l}"] = x1
        if l % 2 == 0:
            j = l // 2
            w1, w3, w2 = (np.asarray(inputs[n][j], np.float32) for n in ("ffn_w1", "ffn_w3", "ffn_w2"))
            maps = []
            for c in range(8):
                fs = slice(c * 1376, (c + 1) * 1376)
                W1 = np.zeros((4096, 1408), np.float32); W1[:, :1376] = w1[:, fs]
                W3 = np.zeros((4096, 1408), np.float32); W3[:, :1376] = w3[:, fs]
                W2 = np.zeros((1408, 4096), np.float32); W2[:1376] = w2[fs]
                maps.append({"xT": x1b_T, "W1": W1, "W3": W3, "W2": W2})
            nc = _get(("ffn", 1408, False), lambda: build_ffn(8192, 1408, False))
            res = _run(f"ffn{l}", nc, maps)
        else:
            j = l // 2
            nc = _get(("router",), lambda: build_router(1024))
            x1T = _c(x1.T)
            rw = np.asarray(inputs["router_w"][j], np.float32)
            rb = np.asarray(inputs["router_b"][j], np.float32)[None]
            res = _run(f"router{l}", nc, [{"xT": _c(x1T[:, c * 1024:(c + 1) * 1024]), "Wr": rw, "rb": rb} for c in range(8)])
            gates = np.concatenate([res[c]["gates"] for c in range(8)], axis=0)
            if dbg is not None:
                dbg[f"gates{l}"] = gates
            del x1T
            maps = [{"xT": x1b_T, "W1": np.asarray(inputs["moe_w1"][j][e], np.float32), "W3": np.asarray(inputs["moe_w3"][j][e], np.float32),
                     "W2": np.asarray(inputs["moe_w2"][j][e], np.float32), "gate": _c(gates[:, e][None])} for e in range(8)]
            nc = _get(("ffn", 4096, True), lambda: build_ffn(8192, 4096, True))
            res = _run(f"moe{l}", nc, maps)
        parts = [_c(res[c]["yT"].T) for c in range(8)]
        del res, maps
        if dbg is not None:
            dbg[f"parts{l}"] = parts
        nc = _get(("ln", 8), lambda: build_ln(1024, 8, True, ALPHA))
        g2 = np.asarray(inputs["ln2_g"][l], np.float32)[None]
        b2 = np.asarray(inputs["ln2_b"][l], np.float32)[None]
        maps = []
        for c in range(8):
            m = {"x": _c(x1[c * 1024:(c + 1) * 1024]), "g": g2, "b": b2}
            for i in range(8):
                m[f"a{i}"] = _c(parts[i][c * 1024:(c + 1) * 1024])
            maps.append(m)
        res = _run(f"ln2_{l}", nc, maps)
        x = np.concatenate([res[c]["y"] for c in range(8)], axis=0)
        xb_T = _c(np.concatenate([res[c]["yb"] for c in range(8)], axis=0).T)
        del res, maps, parts
        if dbg is not None:
            dbg[f"x2_{l}"] = x
    return x.reshape(2, 4096, 4096).astype(np.float32)


def kernel(**inputs):
    return _forward(inputs)
```
